# Optimizing a Trainium2 kernel written in Bass

```python
import math
import jax
import jax.numpy as jnp
from jax import lax
import numpy as np

D_MODEL = 1024
BATCH = 8
SEQ = 4096
DEPTH = 1

N_HEADS = 8
N_KV_HEADS = 2
HEAD_DIM = 64
WINDOW = 128
BLOCK = 128
ATTN_Q = N_HEADS * HEAD_DIM
ATTN_KV = N_KV_HEADS * HEAD_DIM
N_BUCKETS = 32
MAX_EXACT = N_BUCKETS // 2
MAX_DISTANCE = 128
GM_WIDTH = 512
GM_GROUPS = 4
GM_GROUP_DIM = GM_WIDTH // GM_GROUPS
GM_CHUNK = 128
IN_WIDTH = ATTN_Q + 2 * ATTN_KV + 2 * GM_WIDTH + 2 * D_MODEL
N_GROUPS = 4
EXPERTS_PER_GROUP = 8
N_EXPERTS = N_GROUPS * EXPERTS_PER_GROUP
TOP_K = 2
D_EXPERT = 512
MOE_BLOCK = 128

EPS = 1e-6
NEG = -1e30

kernel_name = 'hybrid_swa_gmlp_hmoe_adaln'


def rms_norm(x, g):
    xf = x.astype(jnp.float32)
    y = xf * lax.rsqrt(jnp.mean(xf * xf, axis=-1, keepdims=True) + EPS)
    return (y * g.astype(jnp.float32)).astype(x.dtype)


def layer_norm(x, g, b):
    xf = x.astype(jnp.float32)
    mu = jnp.mean(xf, axis=-1, keepdims=True)
    var = jnp.mean(jnp.square(xf - mu), axis=-1, keepdims=True)
    y = (xf - mu) * lax.rsqrt(var + EPS)
    return (y * g.astype(jnp.float32) + b.astype(jnp.float32)).astype(x.dtype)


def t5_bucket(dist):
    n = jnp.maximum(dist, 0)
    nf = jnp.maximum(n, 1).astype(jnp.float32)
    large = MAX_EXACT + (jnp.log(nf / MAX_EXACT) / math.log(MAX_DISTANCE / MAX_EXACT)
                         * (N_BUCKETS - MAX_EXACT)).astype(jnp.int32)
    large = jnp.minimum(large, N_BUCKETS - 1)
    return jnp.where(n < MAX_EXACT, n, large)


def band(t):
    prev = jnp.concatenate([jnp.zeros_like(t[:, :1]), t[:, :-1]], axis=1)
    return jnp.concatenate([prev, t], axis=2)


def sliding_window_attention(q, k, v, positions, sinks, rel_bias):
    B, S, _ = q.shape
    nb = S // BLOCK
    grp = N_HEADS // N_KV_HEADS
    q = q.reshape(B, nb, BLOCK, N_KV_HEADS, grp, HEAD_DIM)
    kb = band(k.reshape(B, nb, BLOCK, N_KV_HEADS, HEAD_DIM))
    vb = band(v.reshape(B, nb, BLOCK, N_KV_HEADS, HEAD_DIM))
    pq = positions.reshape(B, nb, BLOCK)
    pk = band(pq)
    bucket = t5_bucket(pq[..., :, None] - pk[..., None, :])
    bias = rel_bias[bucket].reshape(B, nb, BLOCK, 2 * BLOCK, N_KV_HEADS, grp)
    bias = bias.transpose(0, 1, 4, 5, 2, 3).astype(jnp.float32)
    qi = jnp.arange(BLOCK)[:, None] + BLOCK
    kj = jnp.arange(2 * BLOCK)[None, :]
    local = (kj <= qi) & (qi - kj < WINDOW)
    first = (jnp.arange(nb)[:, None, None] > 0) | (kj[None] >= BLOCK)
    mask = (local[None] & first)[None, :, None, None]
    scale = HEAD_DIM ** -0.5
    s = jnp.einsum('bnqhgd,bnshd->bnhgqs', q, kb).astype(jnp.float32)
    s = jnp.where(mask, s * scale + bias, NEG)
    sink = sinks.astype(jnp.float32).reshape(N_KV_HEADS, grp)[None, None, :, :, None, None]
    m = jnp.maximum(jnp.max(s, axis=-1, keepdims=True), sink)
    p = jnp.exp(s - m)
    p = p / (jnp.sum(p, axis=-1, keepdims=True) + jnp.exp(sink - m))
    o = jnp.einsum('bnhgqs,bnshd->bnqhgd', p.astype(v.dtype), vb)
    return o.reshape(B, S, ATTN_Q)


def spatial_gating(u, v, ln_g, ln_b, w_s, b_s):
    B, S, _ = v.shape
    nc = S // GM_CHUNK
    vn = layer_norm(v, ln_g, ln_b).reshape(B, nc, GM_CHUNK, GM_GROUPS, GM_GROUP_DIM)
    w = w_s * jnp.tril(jnp.ones((GM_CHUNK, GM_CHUNK), w_s.dtype))
    sv = jnp.einsum('gts,bnsgc->bntgc', w, vn) + b_s.T[:, :, None]
    return u * sv.reshape(B, S, GM_WIDTH)


def hierarchical_moe(h, w_rg, b_rg, w_re, b_re, w_gate, w_up, w_down):
    B, S, D = h.shape
    T = B * S
    hf = h.reshape(T, D)
    lg = (hf @ w_rg).astype(jnp.float32) + b_rg.astype(jnp.float32)
    g_idx = jnp.argmax(lg, axis=-1)
    p_g = jnp.take_along_axis(jax.nn.softmax(lg, axis=-1), g_idx[:, None], axis=-1)[:, 0]
    le = ((hf @ w_re).astype(jnp.float32) + b_re.astype(jnp.float32)).reshape(T, N_GROUPS, EXPERTS_PER_GROUP)
    le = jnp.take_along_axis(le, g_idx[:, None, None], axis=1)[:, 0]
    top_p, top_i = lax.top_k(jax.nn.softmax(le, axis=-1), TOP_K)
    top_p = top_p / jnp.sum(top_p, axis=-1, keepdims=True)
    weights = p_g[:, None] * top_p
    expert_id = g_idx[:, None] * EXPERTS_PER_GROUP + top_i
    A = T * TOP_K
    e_flat = expert_id.reshape(A)
    tok_flat = jnp.repeat(jnp.arange(T, dtype=jnp.int32), TOP_K)
    w_flat = weights.reshape(A)
    order = jnp.argsort(e_flat)
    e_sorted, tok_sorted, w_sorted = e_flat[order], tok_flat[order], w_flat[order]
    counts = jnp.bincount(e_flat, length=N_EXPERTS)
    starts = jnp.cumsum(counts) - counts
    padded = (counts + MOE_BLOCK - 1) // MOE_BLOCK * MOE_BLOCK
    pends = jnp.cumsum(padded)
    pstarts = pends - padded
    dest = pstarts[e_sorted] + (jnp.arange(A) - starts[e_sorted])
    R = (A + MOE_BLOCK - 1) // MOE_BLOCK * MOE_BLOCK + N_EXPERTS * MOE_BLOCK
    NB = R // MOE_BLOCK
    row_tok = jnp.zeros((R,), jnp.int32).at[dest].set(tok_sorted)
    row_w = jnp.zeros((R,), jnp.float32).at[dest].set(w_sorted)
    block_e = jnp.minimum(jnp.searchsorted(pends, jnp.arange(NB) * MOE_BLOCK, side='right'), N_EXPERTS - 1)
    xr = hf[row_tok].reshape(NB, MOE_BLOCK, D)

    def expert_block(args):
        xb, e = args
        return (jax.nn.silu(xb @ w_gate[e]) * (xb @ w_up[e])) @ w_down[e]

    yr = lax.map(expert_block, (xr, block_e)).reshape(R, D)
    out = jax.ops.segment_sum(yr.astype(jnp.float32) * row_w[:, None], row_tok, num_segments=T)
    return out.astype(h.dtype).reshape(B, S, D)


def setup_inputs(seed: int = 0) -> dict:
    key = jax.random.key(seed)
    ks = jax.random.split(key, 26)

    def nrm(k, shape, scale):
        return jax.random.normal(k, shape, jnp.float32) * scale

    L = DEPTH
    return {
        'x': nrm(ks[0], (BATCH, SEQ, D_MODEL), 1.0),
        'c': nrm(ks[1], (BATCH, D_MODEL), 1.0),
        'positions': jnp.broadcast_to(jnp.arange(SEQ, dtype=jnp.int32), (BATCH, SEQ)),
        'rel_bias': nrm(ks[2], (N_BUCKETS, N_HEADS), 0.5),
        'w_ada': nrm(ks[3], (L, D_MODEL, 6 * D_MODEL), D_MODEL ** -0.5),
        'b_ada': nrm(ks[4], (L, 6 * D_MODEL), 0.02),
        'norm1_g': 1.0 + nrm(ks[5], (L, D_MODEL), 0.02),
        'w_in': nrm(ks[6], (L, D_MODEL, IN_WIDTH), D_MODEL ** -0.5),
        'sinks': nrm(ks[7], (L, N_HEADS), 0.5),
        'gm_ln_g': 1.0 + nrm(ks[8], (L, GM_WIDTH), 0.02),
        'gm_ln_b': nrm(ks[9], (L, GM_WIDTH), 0.02),
        'gm_w_s': nrm(ks[10], (L, GM_GROUPS, GM_CHUNK, GM_CHUNK), GM_CHUNK ** -0.5),
        'gm_b_s': 1.0 + nrm(ks[11], (L, GM_GROUPS, GM_CHUNK), 0.02),
        'p_a': nrm(ks[12], (L, ATTN_Q, D_MODEL), ATTN_Q ** -0.5),
        'p_b': nrm(ks[13], (L, GM_WIDTH, D_MODEL), GM_WIDTH ** -0.5),
        'w_o': nrm(ks[14], (L, D_MODEL, D_MODEL), D_MODEL ** -0.5),
        'norm2_g': 1.0 + nrm(ks[15], (L, D_MODEL), 0.02),
        'w_router_g': nrm(ks[16], (L, D_MODEL, N_GROUPS), D_MODEL ** -0.5),
        'b_router_g': nrm(ks[17], (L, N_GROUPS), 0.01),
        'w_router_e': nrm(ks[18], (L, D_MODEL, N_EXPERTS), D_MODEL ** -0.5),
        'b_router_e': nrm(ks[19], (L, N_EXPERTS), 0.01),
        'w_gate': nrm(ks[20], (L, N_EXPERTS, D_MODEL, D_EXPERT), D_MODEL ** -0.5),
        'w_up': nrm(ks[21], (L, N_EXPERTS, D_MODEL, D_EXPERT), D_MODEL ** -0.5),
        'w_down': nrm(ks[22], (L, N_EXPERTS, D_EXPERT, D_MODEL), D_EXPERT ** -0.5),
        'final_g': 1.0 + nrm(ks[23], (D_MODEL,), 0.02),
    }


def reference(x, c, positions, rel_bias, w_ada, b_ada, norm1_g, w_in, sinks, gm_ln_g, gm_ln_b,
              gm_w_s, gm_b_s, p_a, p_b, w_o, norm2_g, w_router_g, b_router_g, w_router_e,
              b_router_e, w_gate, w_up, w_down, final_g):
    splits = np.cumsum([ATTN_Q, ATTN_KV, ATTN_KV, GM_WIDTH, GM_WIDTH, D_MODEL]).tolist()
    cs = jax.nn.silu(c)
    for l in range(DEPTH):
        mod = cs @ w_ada[l] + b_ada[l]
        sh1, sc1, g1, sh2, sc2, g2 = [m[:, None, :] for m in jnp.split(mod, 6, axis=-1)]
        h = rms_norm(x, norm1_g[l]) * (1.0 + sc1) + sh1
        q, k, v, gu, gv, ga, gb = jnp.split(h @ w_in[l], splits, axis=-1)
        y_a = sliding_window_attention(q, k, v, positions, sinks[l], rel_bias)
        y_b = spatial_gating(jax.nn.gelu(gu), jax.nn.gelu(gv), gm_ln_g[l], gm_ln_b[l], gm_w_s[l], gm_b_s[l])
        merged = jax.nn.sigmoid(ga) * (y_a @ p_a[l]) + jax.nn.sigmoid(gb) * (y_b @ p_b[l])
        x = x + g1 * (merged @ w_o[l])
        h = rms_norm(x, norm2_g[l]) * (1.0 + sc2) + sh2
        x = x + g2 * hierarchical_moe(h, w_router_g[l], b_router_g[l], w_router_e[l], b_router_e[l],
                                      w_gate[l], w_up[l], w_down[l])
    return rms_norm(x, final_g)
```

```python
import numpy as np
import ml_dtypes
from contextlib import ExitStack
import concourse.bass as bass
import concourse.mybir as mybir
from concourse.bass_utils import run_bass_kernel_spmd

F32 = mybir.dt.float32
BF16 = mybir.dt.bfloat16
I32 = mybir.dt.int32
AF = mybir.ActivationFunctionType
ALU = mybir.AluOpType
AX = mybir.AxisListType

D = 1024
S = 4096
NBLK = 32
ST = 512
NST = 8
BPS = 4
NE = 32
NB = 96
NLANE = 6
LB = NB // NLANE
R = NB * 128
INW = 3968
C_Q, C_K, C_V, C_GU, C_GV, C_GA, C_GB = 0, 512, 768, 896, 1408, 1920, 2944
EPS = 1e-6
GELU_FN = AF.Gelu_apprx_tanh


class Buf:
    __slots__ = ("w", "r", "ro")

    def __init__(self):
        self.w = []
        self.r = []
        self.ro = False


class Q:
    def __init__(self, name, sems, dma_sems):
        self.name = name
        self.spare = list(sems)
        self.sem = self.spare.pop()
        self.cnt = 0
        self.waited = {}
        self.prog = []
        self.dma = [[sm, 0] for sm in dma_sems]
        self.dma_i = 0


class Sched:
    def __init__(self, nc, es):
        self.nc = nc
        sems = [es.enter_context(nc.semaphore(f"s{i}")) for i in range(96)]
        it = iter(sems)
        take = lambda n: [next(it) for _ in range(n)]
        self.q = {
            "tensor": Q("tensor", take(4), []),
            "vector": Q("vector", take(3), []),
            "scalar": Q("scalar", take(3), take(4)),
            "gpsimd": Q("gpsimd", take(3), take(24)),
            "sync": Q("sync", take(2), take(24)),
        }
        self.all_dma = []

    def _wait(self, q, tok):
        sem, val = tok
        k = id(sem)
        if q.waited.get(k, 0) >= val:
            return
        q.waited[k] = val
        q.prog.append(("w", sem, val))

    def _deps(self, q, reads, writes, skip_self, join=False):
        for b in reads:
            for t in b.w:
                if not (skip_self and t[0] is q.sem):
                    self._wait(q, t)
        for b in writes:
            if not join:
                for t in b.w:
                    if not (skip_self and t[0] is q.sem):
                        self._wait(q, t)
            for t in b.r:
                if not (skip_self and t[0] is q.sem):
                    self._wait(q, t)

    def op(self, qn, fn, reads=(), writes=()):
        q = self.q[qn]
        if q.cnt >= 24000:
            q.sem = q.spare.pop()
            q.cnt = 0
        self._deps(q, reads, writes, qn == "tensor")
        q.cnt += 1
        tok = (q.sem, q.cnt)
        q.prog.append(("i", fn, q.sem, 1))
        for b in writes:
            b.w = [tok]
            b.r = []
        for b in reads:
            if not b.ro and b not in writes:
                b.r.append(tok)
        return tok

    def dma(self, qn, fn, reads=(), writes=(), join=False):
        q = self.q[qn]
        self._deps(q, reads, writes, False, join=join)
        slot = q.dma[q.dma_i % len(q.dma)]
        q.dma_i += 1
        if slot[1] > 0:
            self._wait(q, (slot[0], slot[1]))
        slot[1] += 16
        tok = (slot[0], slot[1])
        q.prog.append(("i", fn, slot[0], 16))
        for b in writes:
            if join:
                b.w = b.w + [tok]
            else:
                b.w = [tok]
                b.r = []
        for b in reads:
            if not b.ro and b not in writes:
                b.r.append(tok)
        self.all_dma.append(tok)
        return tok

    def drain(self, qn="sync"):
        q = self.q[qn]
        last = {}
        for sem, val in self.all_dma:
            last[id(sem)] = (sem, max(val, last.get(id(sem), (sem, 0))[1]))
        for tok in last.values():
            self._wait(q, tok)
        self.all_dma = []

    def reg(self, e, val):
        if val not in self.regcache:
            self.regcache[val] = e.to_reg(val)
        return self.regcache[val]

    def emit(self):
        self.regcache = {}
        with self.nc.Block() as blk:
            for name in ["tensor", "vector", "scalar", "gpsimd", "sync"]:
                q = self.q[name]
                items = q.prog
                q.prog = []

                def body(e, items=items):
                    for it in items:
                        if it[0] == "w":
                            e.wait_ge(it[1], it[2])
                        else:
                            it[1](e).then_inc(it[2], it[3])

                getattr(blk, name)(body)


def bc(ap, shape):
    return ap.broadcast_to(list(shape))


def build_nc(stage="full", dbg=False):
    nc = bass.Bass("TRN2", target_bir_lowering=False)

    def din(name, shape, dt=F32):
        return nc.dram_tensor(name, list(shape), dt, kind="ExternalInput").ap()

    x_d = din("x", [S, D])
    c_d = din("c", [128, 8])
    posq_d = din("posq", [128, 128], I32)
    posk_d = din("posk", [128, 2], I32)
    rb_d = din("relb", [128, 256])
    wada_d = din("w_ada", [128, 6, 8, 1024])
    badaf_d = din("b_ada_f", [128, 6, 8])
    badar_d = din("b_ada_r", [128, 6, 1024])
    n1g_d = din("n1g", [128, 8])
    n2g_d = din("n2g", [128, 1024])
    fg_d = din("fg", [128, 1024])
    win_d = din("w_in", [128, 8, INW])
    sinks_d = din("sinks", [128, 8])
    lng_d = din("lng", [128, 512])
    lnb_d = din("lnb", [128, 512])
    wst_d = din("wst", [128, 4, 128])
    bs_d = din("bs", [1, 512])
    pa_d = din("p_a", [128, 4, 1024])
    pb_d = din("p_b", [128, 4, 1024])
    wo_d = din("w_o", [128, 8, 1024])
    wr_d = din("w_r", [128, 8, 36])
    br_d = din("b_r", [1, 36])
    wg_d = [din(f"w_gate{i}", [NE * 128, 2048]) for i in range(2)]
    wu_d = [din(f"w_up{i}", [NE * 128, 2048]) for i in range(2)]
    wd_d = [din(f"w_down{i}", [NE * 128, 2048]) for i in range(2)]
    zr_d = din("zrows", [1536, D], BF16)
    out_d = nc.dram_tensor("out", [S, D], F32, kind="ExternalOutput").ap()
    h2_d = nc.dram_tensor("h2s", [S, D], BF16, kind="Internal").ap()
    x1_d = nc.dram_tensor("x1s", [S, D], F32, kind="Internal").ap()
    ys_d = nc.dram_tensor("yss", [R, D], BF16, kind="Internal").ap()
    st_d = nc.dram_tensor("sts", [R, 2], I32, kind="Internal").ap()
    g2s_d = nc.dram_tensor("g2s", [128, 1024], F32, kind="Internal").ap()
    xs_d = nc.dram_tensor("xss", [R, D], BF16, kind="Internal").ap()
    dbg_d = {}
    if dbg:
        dbg_d["lg"] = nc.dram_tensor("dbg_lg", [128, NBLK * 36], F32, kind="ExternalOutput").ap()
        dbg_d["x1"] = nc.dram_tensor("dbg_x1", [S, D], F32, kind="ExternalOutput").ap()
        dbg_d["rt"] = nc.dram_tensor("dbg_rt", [128, 512], F32, kind="ExternalOutput").ap()

    es = ExitStack()
    with es:
        sc = Sched(nc, es)
        T = lambda *a, **k: sc.op("tensor", *a, **k)
        V = lambda *a, **k: sc.op("vector", *a, **k)
        A = lambda *a, **k: sc.op("scalar", *a, **k)
        P = lambda *a, **k: sc.op("gpsimd", *a, **k)

        def sb(name, shape, dt, stack=es):
            t = stack.enter_context(nc.sbuf_tensor(name, list(shape), dt))
            return t, Buf()

        X1D_b = [Buf() for _ in range(NBLK)]
        H2D_b = [Buf() for _ in range(NBLK)]
        YS_b = [Buf() for _ in range(NB)]
        XS_b = [Buf() for _ in range(2 * NBLK)]
        XZ_b = [Buf() for _ in range(NST)]
        banks = []
        for i in range(8):
            t = es.enter_context(nc.psum_tensor(f"bank{i}", [128, 512], F32))
            banks.append((t, Buf()))

        def bank_bf(i):
            return banks[i][0][:, :].bitcast(BF16)

        IDB, IDB_b = sb("idb", [128, 128], BF16)
        IDF, IDF_b = sb("idf", [128, 128], F32)
        ONEF, ONEF_b = sb("onef", [128, 128], F32)
        G2D_b = Buf()
        LG, LG_b = sb("lg", [128, NBLK, 36], F32)
        NEGH, NEGH_b = sb("negh", [128, 8], F32)
        W12, W12_b = sb("w12", [128, 2, 32], F32)
        DEST, DEST_b = sb("dest", [128, 2, 32], I32)
        SLOT, SLOT_b = sb("slot", [128, NB], I32)
        IDXW, IDXW_b = sb("idxw", [128, NB], I32)

        V(lambda e: e.memset(ONEF[:, :], 1.0), writes=[ONEF_b])
        V(lambda e: e.memset(NEGH[:, :], -0.5), writes=[NEGH_b])
        P(lambda e: e.affine_select(out=IDF[:, :], in_=ONEF[:, :], pattern=[[-1, 128]],
                                    compare_op=ALU.is_equal, fill=0.0, base=0, channel_multiplier=1),
          reads=[ONEF_b], writes=[IDF_b])
        V(lambda e: e.tensor_copy(out=IDB[:, :], in_=IDF[:, :]), reads=[IDF_b], writes=[IDB_b])

        pa_es = ExitStack()
        with pa_es:
            def sa(name, shape, dt):
                return sb(name, shape, dt, stack=pa_es)

            WIN, WIN_b = sa("win", [128, 8, INW], BF16)
            PAW, PAW_b = sa("paw", [128, 4, 1024], BF16)
            PBW, PBW_b = sa("pbw", [128, 4, 1024], BF16)
            WOW, WOW_b = sa("wow", [128, 8, 1024], BF16)
            KT, KT_b = sa("kt", [128, 2, 2, 640], BF16)
            VA, VA_b = sa("va", [128, 5, 2, 65], BF16)
            BMH, BMH_b = sa("bmh", [128, 2, 8, 128], BF16)
            BML, BML_b = sa("bml", [128, 2, 8, 128], BF16)
            G1H, G1H_b = sa("g1h", [128, 1024], F32)
            A2, A2_b = sa("a2", [128, 1024], F32)
            SH2, SH2_b = sa("sh2", [128, 1024], F32)
            LNG, LNG_b = sa("lngt", [128, 512], F32)
            LNB, LNB_b = sa("lnbt", [128, 512], F32)
            WT, WT_b = sa("wt", [128, 4, 128], BF16)
            BS, BS_b = sa("bst", [1, 512], F32)
            WR, WR_b = sa("wr", [128, 8, 36], F32)
            BR, BR_b = sa("brt", [1, 36], F32)
            ES_, ES_b = sa("es", [128, 8], F32)
            A1, A1_b = sa("a1", [128, 8], F32)
            SH1, SH1_b = sa("sh1", [128, 8], F32)
            SM, SM_b = sa("small", [128, 64], F32)
            smb = [Buf() for _ in range(8)]
            p0_es = ExitStack()
            p0_es.__enter__()
            sa0 = lambda name, shape, dt: sb(name, shape, dt, stack=p0_es)


            thr = _t5_thresholds()
            RBT, RBT_b = sa0("rbt", [128, 32, 8], F32)
            PQ, PQ_b = sa0("pq", [128, 128], I32)
            PK, PK_b = sa0("pk", [128, 2], I32)
            PKF, PKF_b = sa0("pkf", [128, 2], F32)
            DD, DD_b = sa0("dd", [128, 2, 128], F32)
            IND, IND_b = sa0("ind", [128, 2, 128], F32)
            sc.dma("sync", lambda e: e.dma_start(out=RBT[:, :, :], in_=rb_d.rearrange("p (k h) -> p k h", h=8)), writes=[RBT_b])
            sc.dma("sync", lambda e: e.dma_start(out=PQ[:, :], in_=posq_d[:, :]), writes=[PQ_b])
            sc.dma("sync", lambda e: e.dma_start(out=PK[:, :], in_=posk_d[:, :]), writes=[PK_b])
            V(lambda e: e.tensor_copy(out=PKF[:, :], in_=PK[:, :]), reads=[PK_b], writes=[PKF_b])
            V(lambda e: e.tensor_copy(out=DD[:, 0, :], in_=PQ[:, :]), reads=[PQ_b], writes=[DD_b])
            V(lambda e: e.tensor_copy(out=DD[:, 1, :], in_=PQ[:, :]), reads=[PQ_b], writes=[DD_b])
            for hf in range(2):
                V(lambda e, hf=hf: e.tensor_scalar(DD[:, hf, :], DD[:, hf, :], PKF[:, hf:hf + 1], 0.0, ALU.subtract, ALU.max),
                  reads=[DD_b, PKF_b], writes=[DD_b])
            DLT, DLT_b = sa0("dlt", [128, 32, 8], F32)
            V(lambda e: e.tensor_copy(out=DLT[:, 0:1, :], in_=RBT[:, 0:1, :]), reads=[RBT_b], writes=[DLT_b])
            V(lambda e: e.tensor_tensor(out=DLT[:, 1:32, :], in0=RBT[:, 1:32, :], in1=RBT[:, 0:31, :], op=ALU.subtract),
              reads=[RBT_b], writes=[DLT_b])
            BA, _ = sa0("ba", [128, 2, 8, 128], F32)
            BAh = [Buf() for _ in range(8)]
            IND2, IND2_b = sa0("ind2", [128, 2, 128], F32)
            inds = [(IND, IND_b), (IND2, IND2_b)]
            for h in range(8):
                V(lambda e, h=h: e.tensor_copy(out=BA[:, :, h, :], in_=bc(DLT[:, 0, h:h + 1].unsqueeze(1), [128, 2, 128])),
                  reads=[DLT_b], writes=[BAh[h]])
            def t5_chunk(k0, k1):
                for k in range(k0, k1):
                    it_, itb = inds[k % 2]
                    V(lambda e, k=k, it_=it_: e.tensor_scalar(it_[:, :, :], DD[:, :, :], float(thr[k]), None, ALU.is_ge),
                      reads=[DD_b], writes=[itb])
                    for h in range(8):
                        V(lambda e, k=k, h=h, it_=it_: e.scalar_tensor_tensor(
                            out=BA[:, :, h, :], in0=it_[:, :, :], scalar=DLT[:, k, h:h + 1],
                            in1=BA[:, :, h, :], op0=ALU.mult, op1=ALU.add),
                            reads=[itb, DLT_b, BAh[h]], writes=[BAh[h]])
            t5_bounds = [1, 7, 12, 17, 22, 27, 32]
            def cast_load(dst_ap, src_ap, buf, join=False):
                sc.dma("gpsimd", lambda e: e.dma_start(out=dst_ap, in_=src_ap), writes=[buf], join=join)

            CL, CL_b = sa0("cl", [128, 8], F32)
            CS, CS_b = sa0("cs", [128, 8], BF16)
            CSR, CSR_b = sa0("csr", [128, 8, 128], BF16)
            WAD = [sa0(f"wad{i}", [128, 8, 1024], BF16) for i in range(2)]
            sc.dma("sync", lambda e: e.dma_start(out=CL[:, :], in_=c_d[:, :]), writes=[CL_b])
            A(lambda e: e.activation(out=CL[:, :], in_=CL[:, :], func=AF.Silu), reads=[CL_b], writes=[CL_b])
            V(lambda e: e.tensor_copy(out=CS[:, :], in_=CL[:, :]), reads=[CL_b], writes=[CS_b])
            V(lambda e: e.tensor_copy(out=CSR[:, :, :], in_=bc(CL[:, :].unsqueeze(2), [128, 8, 128])),
              reads=[CL_b], writes=[CSR_b])
            BF_, BF_b = sa0("badaf", [128, 6, 8], F32)
            sc.dma("sync", lambda e: e.dma_start(out=BF_[:, :, :], in_=badaf_d[:, :, :]), writes=[BF_b])
            N1G, N1G_b = sa0("n1gt", [128, 8], F32)
            sc.dma("sync", lambda e: e.dma_start(out=N1G[:, :], in_=n1g_d[:, :]), writes=[N1G_b])
            sc.dma("sync", lambda e: e.dma_start(out=G1H[:, :], in_=badar_d[:, 2, :]), writes=[G1H_b])
            sc.dma("sync", lambda e: e.dma_start(out=SH2[:, :], in_=badar_d[:, 3, :]), writes=[SH2_b])
            sc.dma("sync", lambda e: e.dma_start(out=A2[:, :], in_=badar_d[:, 4, :]), writes=[A2_b])
            G2, G2_b = sa0("g2t", [128, 1024], F32)
            sc.dma("sync", lambda e: e.dma_start(out=G2[:, :], in_=badar_d[:, 5, :]), writes=[G2_b])
            N2G, N2G_b = sa0("n2gt", [128, 1024], F32)
            sc.dma("sync", lambda e: e.dma_start(out=N2G[:, :], in_=n2g_d[:, :]), writes=[N2G_b])

            for v in range(6):
                t5_chunk(t5_bounds[v], t5_bounds[v + 1])
                wt_, wb_ = WAD[v % 2]
                for kk in range(4):
                    cast_load(wt_[:, 2 * kk:2 * kk + 2, :], wada_d[:, v, 2 * kk:2 * kk + 2, :], wb_, join=(kk > 0))
                if v < 2:
                    bk, bkb = banks[v]
                    for cc in range(8):
                        for kc in range(8):
                            T(lambda e, cc=cc, kc=kc, wt_=wt_, bk=bk: e.matmul(
                                out=bk[:, cc:cc + 1], lhsT=wt_[:, kc, cc * 128:(cc + 1) * 128],
                                rhs=CS[:, kc:kc + 1], start=(kc == 0), stop=(kc == 7)),
                              reads=[wb_, CS_b], writes=[bkb])
                    if v == 0:
                        V(lambda e, bk=bk: e.tensor_tensor(out=SH1[:, :], in0=bk[:, 0:8], in1=BF_[:, 0, :], op=ALU.add),
                          reads=[bkb, BF_b], writes=[SH1_b])
                    else:
                        V(lambda e, bk=bk: e.tensor_tensor(out=A1[:, :], in0=bk[:, 0:8], in1=BF_[:, 1, :], op=ALU.add),
                          reads=[bkb, BF_b], writes=[A1_b])
                        V(lambda e: e.scalar_tensor_tensor(out=A1[:, :], in0=A1[:, :], scalar=1.0, in1=N1G[:, :],
                                                           op0=ALU.add, op1=ALU.mult),
                          reads=[A1_b, N1G_b], writes=[A1_b])
                else:
                    dst, dstb = {2: (G1H, G1H_b), 3: (SH2, SH2_b), 4: (A2, A2_b), 5: (G2, G2_b)}[v]
                    for hf in range(2):
                        bk, bkb = banks[2 + hf]
                        for kc in range(8):
                            T(lambda e, kc=kc, hf=hf, wt_=wt_, bk=bk: e.matmul(
                                out=bk[:, :], lhsT=CSR[:, kc, :], rhs=wt_[:, kc, hf * 512:(hf + 1) * 512],
                                start=(kc == 0), stop=(kc == 7)),
                              reads=[wb_, CSR_b], writes=[bkb])
                        V(lambda e, hf=hf, bk=bk, dst=dst: e.tensor_tensor(
                            out=dst[:, hf * 512:(hf + 1) * 512], in0=bk[:, :], in1=dst[:, hf * 512:(hf + 1) * 512], op=ALU.add),
                          reads=[bkb, dstb], writes=[dstb])
                    if v == 2:
                        V(lambda e: e.tensor_scalar(G1H[:, :], G1H[:, :], 0.5, None, ALU.mult), reads=[G1H_b], writes=[G1H_b])
                    if v == 4:
                        V(lambda e: e.scalar_tensor_tensor(out=A2[:, :], in0=A2[:, :], scalar=1.0, in1=N2G[:, :],
                                                           op0=ALU.add, op1=ALU.mult),
                          reads=[A2_b, N2G_b], writes=[A2_b])

            for kc in range(8):
                for hh in range(2):
                    cast_load(WIN[:, kc, hh * 1984:(hh + 1) * 1984], win_d[:, kc, hh * 1984:(hh + 1) * 1984], WIN_b, join=(kc + hh > 0))
            for kc in range(4):
                cast_load(PAW[:, kc, :], pa_d[:, kc, :], PAW_b, join=(kc > 0))
                cast_load(PBW[:, kc, :], pb_d[:, kc, :], PBW_b, join=(kc > 0))
            for kc in range(8):
                cast_load(WOW[:, kc, :], wo_d[:, kc, :], WOW_b, join=(kc > 0))
            cast_load(WT[:, :, :], wst_d[:, :, :], WT_b)
            P(lambda e: e.affine_select(out=WT[:, :, :], in_=WT[:, :, :], pattern=[[0, 4], [1, 128]],
                                        compare_op=ALU.is_ge, fill=0.0, base=0, channel_multiplier=-1),
              reads=[WT_b], writes=[WT_b])
            for (dap, dstb, sap) in [(LNG[:, :], LNG_b, lng_d[:, :]), (LNB[:, :], LNB_b, lnb_d[:, :]), (BS[:, :], BS_b, bs_d[:, :]),
                                     (WR[:, :, :], WR_b, wr_d[:, :, :]), (BR[:, :], BR_b, br_d[:, :]), (ES_[:, :], ES_b, sinks_d[:, :])]:
                sc.dma("sync", lambda e, dap=dap, sap=sap: e.dma_start(out=dap, in_=sap), writes=[dstb])
            A(lambda e: e.activation(out=ES_[:, :], in_=ES_[:, :], func=AF.Exp), reads=[ES_b], writes=[ES_b])
            V(lambda e: e.memset(VA[:, :, :, :], 1.0), writes=[VA_b])
            V(lambda e: e.memset(KT[:, :, :, :], 0.0), writes=[KT_b])

            P(lambda e: e.affine_select(out=BA[:, 0, :, :], in_=BA[:, 0, :, :], pattern=[[0, 8], [-1, 128]],
                                        compare_op=ALU.is_gt, fill=-30000.0, base=0, channel_multiplier=1),
              reads=BAh, writes=BAh)
            P(lambda e: e.affine_select(out=BA[:, 1, :, :], in_=BA[:, 1, :, :], pattern=[[0, 8], [1, 128]],
                                        compare_op=ALU.is_ge, fill=-30000.0, base=0, channel_multiplier=-1),
              reads=BAh, writes=BAh)
            V(lambda e: e.tensor_copy(out=BMH[:, :, :, :], in_=BA[:, :, :, :]), reads=BAh, writes=[BMH_b])
            V(lambda e: e.tensor_tensor(out=BA[:, :, :, :], in0=BA[:, :, :, :], in1=BMH[:, :, :, :], op=ALU.subtract),
              reads=BAh + [BMH_b], writes=BAh)
            V(lambda e: e.tensor_copy(out=BML[:, :, :, :], in_=BA[:, :, :, :]), reads=BAh, writes=[BML_b])

            for b_ in (WIN_b, PAW_b, PBW_b, WOW_b):
                pass

            sc.dma("sync", lambda e: e.dma_start(out=g2s_d[:, :], in_=G2[:, :]), reads=[G2_b], writes=[G2D_b])
            sc.drain("sync")
            sc.emit()
            p0_es.__exit__(None, None, None)
            XT, _ = sa("xt", [128, 4, 1024], F32)
            xb = [Buf() for _ in range(4)]
            XN = [sa(f"xn{i}", [128, 1024], BF16) for i in range(2)]
            HT, _ = sa("ht", [128, 8, ST], BF16)
            HTk = [Buf() for _ in range(8)]
            QT, QT_b = sa("qt", [128, 4, ST], BF16)
            UT, UT_b = sa("ut", [128, 4, ST], BF16)
            YA, YA_b = sa("ya", [128, 512], BF16)
            YAT, YAT_b = sa("yat", [128, 4, ST], BF16)
            YBT, YBT_b = sa("ybt", [128, 4, ST], BF16)
            MT, MT_b = sa("mt", [128, 8, ST], BF16)
            RA, RA_b = sa("ra", [128, 512], F32)
            RB, RB_b = sa("rb", [128, 512], F32)
            RC, RC_b = sa("rc", [128, 512], F32)
            RD, RD_b = sa("rd", [128, 1024], F32)
            RE, RE_b = sa("re", [128, 1024], BF16)
            VNS = [sa(f"vn{i}", [128, 512], BF16) for i in range(2)]
            H2T, H2T_b = sa("h2t", [128, 8, 128], F32)
            H2Tb_b = Buf()
            LGT, LGT_b = sa("lgt", [128, 128], F32)
            PT_bf = RD[:, :].bitcast(BF16)
            n_st = NST if stage != "A1" else 1
            re_half = [Buf(), Buf()]
            pj = [0]

            def next_proj_bank():
                pj[0] ^= 1
                return banks[pj[0]]

            for s in range(n_st):
                for b in range(BPS):
                    n = s * BPS + b
                    sc.dma("sync", lambda e, b=b, n=n: e.dma_start(out=XT[:, b, :], in_=x_d[n * 128:(n + 1) * 128, :]),
                           writes=[xb[b]])
                sc.dma("scalar", lambda e, s=s: e.dma_start(out=xs_d[s * 1536:(s + 1) * 1536, :], in_=zr_d[:, :]), writes=[XZ_b[s]])
                SSQ = SM[:, 0:4]
                RSTD = SM[:, 4:8]
                V(lambda e: e.memset(SSQ, 0.0), writes=[smb[0]])
                for b in range(BPS):
                    xn_t, xn_b = XN[b % 2]
                    A(lambda e, b=b, xn_t=xn_t: e.activation(out=xn_t[:, :], in_=XT[:, b, :], func=AF.Square,
                                                             accum_out=SM[:, b:b + 1]),
                      reads=[xb[b]], writes=[xn_b, smb[0]])
                V(lambda e: e.tensor_scalar(SM[:, 8:12], SSQ, 1.0 / D, EPS, ALU.mult, ALU.add), reads=[smb[0]], writes=[smb[1]])
                P(lambda e: e.tensor_tensor(out=RSTD, in0=SM[:, 8:12], in1=NEGH[:, 0:4], op=ALU.pow),
                  reads=[smb[1], NEGH_b], writes=[smb[2]])
                for b in range(BPS):
                    xn_t, xn_b = XN[b % 2]
                    A(lambda e, b=b, xn_t=xn_t: e.activation(out=xn_t[:, :], in_=XT[:, b, :], func=AF.Copy,
                                                             scale=SM[:, 4 + b:5 + b]),
                      reads=[xb[b], smb[2]], writes=[xn_b])
                    for c in range(8):
                        bi = 2 + c // 2
                        T(lambda e, b=b, c=c, bi=bi, xn_t=xn_t: e.transpose(
                            out=bank_bf(bi)[:, (c % 2) * 512 + b * 128:(c % 2) * 512 + (b + 1) * 128],
                            in_=xn_t[:, c * 128:(c + 1) * 128], identity=IDB[:, :]),
                          reads=[xn_b, IDB_b], writes=[banks[bi][1]])
                for c in range(8):
                    bi = 2 + c // 2
                    V(lambda e, c=c, bi=bi: e.tensor_scalar(HT[:, c, :], bank_bf(bi)[:, (c % 2) * 512:(c % 2 + 1) * 512],
                                                            A1[:, c:c + 1], SH1[:, c:c + 1], ALU.mult, ALU.add),
                      reads=[banks[bi][1], A1_b, SH1_b], writes=[HTk[c]])

                def proj_fm(col, evac):
                    bk, bkb = next_proj_bank()
                    for kc in range(8):
                        T(lambda e, kc=kc, bk=bk: e.matmul(out=bk[:, :], lhsT=WIN[:, kc, col:col + 128], rhs=HT[:, kc, :],
                                                           start=(kc == 0), stop=(kc == 7)),
                          reads=[WIN_b, HTk[kc]], writes=[bkb])
                    evac(bk, bkb)

                for i in range(4):
                    proj_fm(C_Q + i * 128, lambda bk, bkb, i=i: V(
                        lambda e: e.tensor_scalar(QT[:, i, :], bk[:, :], 0.125, None, ALU.mult), reads=[bkb], writes=[QT_b]))
                def k_evac(bk, bkb, i):
                    V(lambda e: e.tensor_copy(out=KT[0:64, i, 0, 128:640], in_=bk[0:64, :]), reads=[bkb], writes=[KT_b])
                    V(lambda e: e.tensor_copy(out=KT[64:128, i, 1, 128:640], in_=bk[64:128, :]), reads=[bkb], writes=[KT_b])

                for i in range(2):
                    proj_fm(C_K + i * 128, lambda bk, bkb, i=i: k_evac(bk, bkb, i))
                for i in range(4):
                    proj_fm(C_GU + i * 128, lambda bk, bkb, i=i: A(
                        lambda e: e.activation(out=UT[:, i, :], in_=bk[:, :], func=GELU_FN), reads=[bkb], writes=[UT_b]))

                def tm_front(b):
                    blk = slice(b * 128, (b + 1) * 128)
                    vn_t, vn_b = VNS[b % 2]
                    bk, bkb = next_proj_bank()
                    for kc in range(8):
                        T(lambda e, kc=kc, bk=bk, blk=blk: e.matmul(out=bk[:, 0:128], lhsT=HT[:, kc, blk],
                                                                    rhs=WIN[:, kc, C_V:C_V + 128], start=(kc == 0), stop=(kc == 7)),
                          reads=[WIN_b, HTk[kc]], writes=[bkb])
                    V(lambda e, b=b, bk=bk: e.tensor_copy(out=VA[:, 1 + b, :, 0:64],
                                                          in_=bk[:, 0:128].rearrange("p (k d) -> p k d", d=64)),
                      reads=[bkb], writes=[VA_b])
                    bk, bkb = next_proj_bank()
                    for kc in range(8):
                        T(lambda e, kc=kc, bk=bk, blk=blk: e.matmul(out=bk[:, :], lhsT=HT[:, kc, blk],
                                                                    rhs=WIN[:, kc, C_GV:C_GV + 512], start=(kc == 0), stop=(kc == 7)),
                          reads=[WIN_b, HTk[kc]], writes=[bkb])
                    A(lambda e, bk=bk: e.activation(out=RB[:, :], in_=bk[:, :], func=GELU_FN), reads=[bkb], writes=[RB_b])
                    V(lambda e: e.bn_stats(out=SM[:, 16:22], in_=RB[:, :]), reads=[RB_b], writes=[smb[3]])
                    V(lambda e: e.bn_aggr(out=SM[:, 22:24], in_=SM[:, 16:22]), reads=[smb[3]], writes=[smb[4]])
                    V(lambda e: e.tensor_scalar(SM[:, 24:25], SM[:, 23:24], EPS, None, ALU.add), reads=[smb[4]], writes=[smb[5]])
                    P(lambda e: e.tensor_tensor(out=SM[:, 25:26], in0=SM[:, 24:25], in1=NEGH[:, 0:1], op=ALU.pow),
                      reads=[smb[5], NEGH_b], writes=[smb[6]])
                    V(lambda e: e.tensor_scalar(RC[:, :], RB[:, :], SM[:, 22:23], SM[:, 25:26], ALU.subtract, ALU.mult),
                      reads=[RB_b, smb[4], smb[6]], writes=[RC_b])
                    P(lambda e: e.tensor_tensor(out=RC[:, :], in0=RC[:, :], in1=LNG[:, :], op=ALU.mult),
                      reads=[RC_b, LNG_b], writes=[RC_b])
                    P(lambda e, vn_t=vn_t: e.tensor_tensor(out=vn_t[:, :], in0=RC[:, :], in1=LNB[:, :], op=ALU.add),
                      reads=[RC_b, LNB_b], writes=[vn_b])

                def tm_spatial(b):
                    blk = slice(b * 128, (b + 1) * 128)
                    vn_t, vn_b = VNS[b % 2]
                    bk, bkb = next_proj_bank()
                    for g in range(4):
                        gs = slice(g * 128, (g + 1) * 128)
                        T(lambda e, bk=bk, gs=gs, g=g, vn_t=vn_t: e.matmul(out=bk[:, gs], lhsT=vn_t[:, gs], rhs=WT[:, g, :], start=True, stop=False),
                          reads=[vn_b, WT_b], writes=[bkb])
                        T(lambda e, bk=bk, gs=gs: e.matmul(out=bk[:, gs], lhsT=ONEF[0:1, 0:128], rhs=BS[0:1, gs], start=False, stop=True),
                          reads=[ONEF_b, BS_b], writes=[bkb])
                    V(lambda e, bk=bk, blk=blk: e.tensor_tensor(out=YBT[:, :, blk], in0=bk[:, :].rearrange("p (g t) -> p g t", t=128),
                                                                in1=UT[:, :, blk], op=ALU.mult),
                      reads=[bkb, UT_b], writes=[YBT_b])

                def at_info(b):
                    n = s * BPS + b
                    return n, slice(b * 128, (b + 1) * 128), ([1] if n == 0 else [0, 1])

                def at_S(b):
                    n, blk, halves = at_info(b)
                    for hf in halves:
                        kcols = slice((b + hf) * 128, (b + hf + 1) * 128)
                        for h in range(8):
                            par, kvh = h % 2, h // 4
                            bk, bkb = banks[2 + hf * 2 + h // 4]
                            oc = slice((h % 4) * 128, (h % 4 + 1) * 128)
                            T(lambda e, bk=bk, oc=oc, hf=hf, h=h: e.matmul(out=bk[:, oc], lhsT=IDB[:, :], rhs=BMH[:, hf, h, :], start=True, stop=False),
                              reads=[IDB_b, BMH_b], writes=[bkb])
                            T(lambda e, bk=bk, oc=oc, hf=hf, h=h: e.matmul(out=bk[:, oc], lhsT=IDB[:, :], rhs=BML[:, hf, h, :], start=False, stop=False),
                              reads=[IDB_b, BML_b], writes=[bkb])
                            T(lambda e, bk=bk, oc=oc, h=h, kvh=kvh, par=par, kcols=kcols, blk=blk: e.matmul(
                                out=bk[:, oc], lhsT=KT[:, kvh, par, kcols], rhs=QT[:, h // 2, blk], start=False, stop=True),
                              reads=[KT_b, QT_b], writes=[bkb])

                def at_softmax(b):
                    n, blk, halves = at_info(b)
                    for hf in halves:
                        for g2 in range(2):
                            bk, bkb = banks[2 + hf * 2 + g2]
                            pcol = (hf * 2 + g2) * 512
                            A(lambda e, bk=bk, pcol=pcol: e.activation(out=PT_bf[:, pcol:pcol + 512], in_=bk[:, :], func=AF.Exp),
                              reads=[bkb], writes=[RD_b])

                def at_PV(b):
                    n, blk, halves = at_info(b)
                    for h in range(8):
                        kvh = h // 4
                        bk, bkb = banks[6 + h // 4]
                        for hf in halves:
                            pcol = (hf * 2 + h // 4) * 512 + (h % 4) * 128
                            T(lambda e, bk=bk, h=h, kvh=kvh, hf=hf, pcol=pcol, b=b, halves=halves: e.matmul(
                                out=bk[:, (h % 4) * 65:(h % 4 + 1) * 65], lhsT=PT_bf[:, pcol:pcol + 128],
                                rhs=VA[:, b + hf, kvh, :], start=(hf == halves[0]), stop=(hf == 1)),
                              reads=[RD_b, VA_b], writes=[bkb])

                def at_norm(b):
                    n, blk, halves = at_info(b)
                    for grp in range(2):
                        bk, bkb = banks[6 + grp]
                        obv = bk[:, 0:260].rearrange("p (h d) -> p h d", d=65)
                        V(lambda e, obv=obv, grp=grp: e.tensor_tensor(out=SM[:, 32 + grp * 4:36 + grp * 4], in0=obv[:, :, 64],
                                                                      in1=ES_[:, grp * 4:(grp + 1) * 4], op=ALU.add),
                          reads=[bkb, ES_b], writes=[smb[7]])
                        V(lambda e, grp=grp: e.reciprocal(out=SM[:, 40 + grp * 4:44 + grp * 4], in_=SM[:, 32 + grp * 4:36 + grp * 4]),
                          reads=[smb[7]], writes=[smb[7]])
                        V(lambda e, obv=obv, grp=grp: e.tensor_tensor(
                            out=YA[:, grp * 256:(grp + 1) * 256].rearrange("p (h d) -> p h d", d=64), in0=obv[:, :, 0:64],
                            in1=bc(SM[:, 40 + grp * 4:44 + grp * 4].unsqueeze(2), [128, 4, 64]), op=ALU.mult),
                          reads=[bkb, smb[7]], writes=[YA_b])
                    bk, bkb = next_proj_bank()
                    bi = pj[0]
                    for c in range(4):
                        T(lambda e, c=c, bi=bi: e.transpose(out=bank_bf(bi)[:, c * 128:(c + 1) * 128], in_=YA[:, c * 128:(c + 1) * 128],
                                                            identity=IDB[:, :]),
                          reads=[YA_b, IDB_b], writes=[bkb])
                    V(lambda e, bi=bi, blk=blk: e.tensor_copy(out=YAT[:, :, blk],
                                                              in_=bank_bf(bi)[:, 0:512].rearrange("p (c t) -> p c t", t=128)),
                      reads=[bkb], writes=[YAT_b])

                tm_front(0)
                at_S(0)
                at_softmax(0)
                for b in range(BPS):
                    if b + 1 < BPS:
                        tm_front(b + 1)
                        at_S(b + 1)
                    tm_spatial(b)
                    at_PV(b)
                    if b + 1 < BPS:
                        at_softmax(b + 1)
                    at_norm(b)

                for cc in range(8):
                    cs_ = slice(cc * 128, (cc + 1) * 128)
                    bGA, bGB, bPA, bPB = (2, 3, 4, 5) if cc % 2 == 0 else (6, 7, 0, 1)
                    for kc in range(8):
                        T(lambda e, kc=kc, cc=cc, bGA=bGA: e.matmul(out=banks[bGA][0][:, :], lhsT=WIN[:, kc, C_GA + cc * 128:C_GA + (cc + 1) * 128],
                                                                    rhs=HT[:, kc, :], start=(kc == 0), stop=(kc == 7)),
                          reads=[WIN_b, HTk[kc]], writes=[banks[bGA][1]])
                    for kc in range(8):
                        T(lambda e, kc=kc, cc=cc, bGB=bGB: e.matmul(out=banks[bGB][0][:, :], lhsT=WIN[:, kc, C_GB + cc * 128:C_GB + (cc + 1) * 128],
                                                                    rhs=HT[:, kc, :], start=(kc == 0), stop=(kc == 7)),
                          reads=[WIN_b, HTk[kc]], writes=[banks[bGB][1]])
                    for kc in range(4):
                        T(lambda e, kc=kc, cs_=cs_, bPA=bPA: e.matmul(out=banks[bPA][0][:, :], lhsT=PAW[:, kc, cs_], rhs=YAT[:, kc, :],
                                                                      start=(kc == 0), stop=(kc == 3)),
                          reads=[PAW_b, YAT_b], writes=[banks[bPA][1]])
                    for kc in range(4):
                        T(lambda e, kc=kc, cs_=cs_, bPB=bPB: e.matmul(out=banks[bPB][0][:, :], lhsT=PBW[:, kc, cs_], rhs=YBT[:, kc, :],
                                                                      start=(kc == 0), stop=(kc == 3)),
                          reads=[PBW_b, YBT_b], writes=[banks[bPB][1]])
                    sa_t = RE[:, (cc % 2) * 512:(cc % 2) * 512 + 512]
                    sa_b = re_half[cc % 2]
                    A(lambda e, bGA=bGA, sa_t=sa_t: e.activation(out=sa_t, in_=banks[bGA][0][:, :], func=AF.Tanh, scale=0.5),
                      reads=[banks[bGA][1]], writes=[sa_b, RE_b])
                    V(lambda e, bPA=bPA, sa_t=sa_t: e.scalar_tensor_tensor(out=RA[:, :], in0=sa_t, scalar=1.0, in1=banks[bPA][0][:, :], op0=ALU.add, op1=ALU.mult),
                      reads=[sa_b, banks[bPA][1]], writes=[RA_b])
                    A(lambda e, bGB=bGB, sa_t=sa_t: e.activation(out=sa_t, in_=banks[bGB][0][:, :], func=AF.Tanh, scale=0.5),
                      reads=[banks[bGB][1]], writes=[sa_b, RE_b])
                    V(lambda e, bPB=bPB, sa_t=sa_t: e.scalar_tensor_tensor(out=RB[:, :], in0=sa_t, scalar=1.0, in1=banks[bPB][0][:, :], op0=ALU.add, op1=ALU.mult),
                      reads=[sa_b, banks[bPB][1]], writes=[RB_b])
                    P(lambda e, cc=cc: e.tensor_tensor(out=MT[:, cc, :], in0=RA[:, :], in1=RB[:, :], op=ALU.add),
                      reads=[RA_b, RB_b], writes=[MT_b])

                RD2 = QT[:, :, :].rearrange("p a t -> p (a t)").bitcast(F32)
                RE2 = XN[0][0]
                h2bufs = [(RD, RD_b, RE, RE_b, [RE_b, re_half[0], re_half[1]]), (RD2, QT_b, RE2, XN[0][1], [XN[0][1]])]

                def wo_WO(b):
                    blk = slice(b * 128, (b + 1) * 128)
                    for hf in range(2):
                        hs = slice(hf * 512, (hf + 1) * 512)
                        bk, bkb = next_proj_bank()
                        for kc in range(8):
                            T(lambda e, kc=kc, bk=bk, blk=blk, hs=hs: e.matmul(out=bk[:, :], lhsT=MT[:, kc, blk], rhs=WOW[:, kc, hs],
                                                                               start=(kc == 0), stop=(kc == 7)),
                              reads=[MT_b, WOW_b], writes=[bkb])
                        V(lambda e, bk=bk, hs=hs: e.tensor_tensor(out=RC[:, :], in0=bk[:, :], in1=G1H[:, hs], op=ALU.mult),
                          reads=[bkb, G1H_b], writes=[RC_b])
                        V(lambda e, b=b, hs=hs: e.tensor_tensor(out=XT[:, b, hs], in0=XT[:, b, hs], in1=RC[:, :], op=ALU.add),
                          reads=[RC_b, xb[b]], writes=[xb[b]])

                def wo_N2(b):
                    n = s * BPS + b
                    rd, rdb, re_, reb, rew = h2bufs[b % 2]
                    sc.dma("sync", lambda e, b=b, n=n: e.dma_start(out=x1_d[n * 128:(n + 1) * 128, :], in_=XT[:, b, :]), reads=[xb[b]], writes=[X1D_b[n]])
                    if dbg:
                        sc.dma("sync", lambda e, b=b, n=n: e.dma_start(out=dbg_d["x1"][n * 128:(n + 1) * 128, :], in_=XT[:, b, :]),
                               reads=[xb[b]])
                    V(lambda e: e.memset(SM[:, 48:49], 0.0), writes=[smb[0]])
                    A(lambda e, b=b, rd=rd: e.activation(out=rd[:, :], in_=XT[:, b, :], func=AF.Square, accum_out=SM[:, 48:49]),
                      reads=[xb[b]], writes=[rdb, smb[0]])
                    V(lambda e: e.tensor_scalar(SM[:, 49:50], SM[:, 48:49], 1.0 / D, EPS, ALU.mult, ALU.add), reads=[smb[0]], writes=[smb[1]])
                    P(lambda e: e.tensor_tensor(out=SM[:, 50:51], in0=SM[:, 49:50], in1=NEGH[:, 0:1], op=ALU.pow),
                      reads=[smb[1], NEGH_b], writes=[smb[2]])
                    V(lambda e, b=b, rd=rd: e.scalar_tensor_tensor(out=rd[:, :], in0=XT[:, b, :], scalar=SM[:, 50:51], in1=A2[:, :],
                                                                   op0=ALU.mult, op1=ALU.mult),
                      reads=[xb[b], smb[2], A2_b], writes=[rdb])
                    V(lambda e, rd=rd: e.tensor_tensor(out=rd[:, :], in0=rd[:, :], in1=SH2[:, :], op=ALU.add), reads=[rdb, SH2_b], writes=[rdb])
                    A(lambda e, rd=rd, re_=re_: e.activation(out=re_[:, :], in_=rd[:, :], func=AF.Copy), reads=[rdb], writes=rew)
                    sc.dma("sync", lambda e, n=n, re_=re_: e.dma_start(out=h2_d[n * 128:(n + 1) * 128, :], in_=re_[:, :]), reads=[reb], writes=[H2D_b[n]])

                def wo_TR(b):
                    n = s * BPS + b
                    rd, rdb, re_, reb, rew = h2bufs[b % 2]
                    for c in range(8):
                        bi = 6 + c // 4
                        T(lambda e, c=c, bi=bi, rd=rd: e.transpose(out=banks[bi][0][:, (c % 4) * 128:(c % 4 + 1) * 128],
                                                                   in_=rd[:, c * 128:(c + 1) * 128], identity=IDF[:, :]),
                          reads=[rdb, IDF_b], writes=[banks[bi][1]])
                    A(lambda e: e.activation(out=H2T[:, 0:4, :], in_=banks[6][0][:, :].rearrange("p (c t) -> p c t", t=128), func=AF.Copy),
                      reads=[banks[6][1]], writes=[H2T_b])
                    V(lambda e: e.tensor_copy(out=H2T[:, 4:8, :], in_=banks[7][0][:, :].rearrange("p (c t) -> p c t", t=128)),
                      reads=[banks[7][1]], writes=[H2Tb_b])
                    bk, bkb = next_proj_bank()
                    for c in range(8):
                        T(lambda e, c=c, bk=bk: e.matmul(out=bk[0:36, 0:128], lhsT=WR[:, c, :], rhs=H2T[:, c, :], start=(c == 0), stop=False),
                          reads=[H2T_b if c < 4 else H2Tb_b, WR_b], writes=[bkb])
                    T(lambda e, bk=bk: e.matmul(out=bk[0:36, 0:128], lhsT=BR[0:1, 0:36], rhs=ONEF[0:1, 0:128], start=False, stop=True),
                      reads=[ONEF_b, BR_b], writes=[bkb])
                    A(lambda e, bk=bk: e.activation(out=LGT[0:36, :], in_=bk[0:36, 0:128], func=AF.Copy), reads=[bkb], writes=[LGT_b])

                def wo_LG(b):
                    n = s * BPS + b
                    bk, bkb = next_proj_bank()
                    T(lambda e, bk=bk: e.transpose(out=bk[:, 0:36], in_=LGT[0:36, :], identity=IDF[0:36, 0:36]),
                      reads=[LGT_b, IDF_b], writes=[bkb])
                    V(lambda e, bk=bk, n=n: e.tensor_copy(out=LG[:, n, :], in_=bk[:, 0:36]), reads=[bkb], writes=[LG_b])

                wo_WO(0)
                wo_N2(0)
                for b in range(BPS):
                    if b + 1 < BPS:
                        wo_WO(b + 1)
                    if b > 0:
                        wo_LG(b - 1)
                    wo_TR(b)
                    if b + 1 < BPS:
                        wo_N2(b + 1)
                wo_LG(BPS - 1)
                P(lambda e: e.tensor_copy(out=KT[:, :, :, 0:128], in_=KT[:, :, :, 512:640]), reads=[KT_b], writes=[KT_b])
                P(lambda e: e.tensor_copy(out=VA[:, 0, :, :], in_=VA[:, 4, :, :]), reads=[VA_b], writes=[VA_b])

            if dbg:
                sc.dma("sync", lambda e: e.dma_start(out=dbg_d["lg"][:, :], in_=LG[:, :, :].rearrange("p t k -> p (t k)")), reads=[LG_b])
            sc.drain("sync")
            sc.emit()

        if stage in ("A", "A1"):
            return nc, dbg_d

        pr_es = ExitStack()
        with pr_es:
            sr = lambda name, shape, dt: sb(name, shape, dt, stack=pr_es)
            GM, GM_b = sr("gm", [128, 32], F32)
            OHG, OHG_b = sr("ohg", [128, 32, 4], F32)
            T4, T4_b = sr("t4", [128, 32, 4], F32)
            PEN, PEN_b = sr("pen", [128, 32, 4], F32)
            PG, PG_b = sr("pg", [128, 32], F32)
            LEM, LEM_b = sr("lem", [128, 32, 32], F32)
            LEM2, LEM2_b = sr("lem2", [128, 32, 32], F32)
            OH1, OH1_b = sr("oh1", [128, 32, 32], F32)
            OH2, OH2_b = sr("oh2", [128, 32, 32], F32)
            M1, M1_b = sr("m1", [128, 32], F32)
            M2, M2_b = sr("m2", [128, 32], F32)
            DL, DL_b = sr("dl", [128, 32], F32)
            MM, MM_b = sr("mm", [128, 32, 32], F32)
            US, US_b = sr("us", [128, 128], F32)
            TOT, TOT_b = sr("tot", [128, 32, 32], F32)
            CA, CA_b = sr("ca", [128, 32, 32], F32)
            CB, CB_b = sr("cb", [128, 32, 32], F32)
            RANK, RANK_b = sr("rank", [128, 32, 32], F32)
            THRI, THRI_b = sr("thri", [128, 32], I32)
            THRF, THRF_b = sr("thrf", [128, 32], F32)
            NBK, NBK_b = sr("nbk", [128, 32], F32)
            C1, C1_b = sr("c1", [128, 32], F32)
            C2, C2_b = sr("c2", [128, 32], F32)
            PST, PST_b = sr("pst", [128, 32], F32)
            JVI, JVI_b = sr("jvi", [128, NB], I32)
            JVF, JVF_b = sr("jvf", [128, NB], F32)
            CMP2, CMP2_b = sr("cmp2", [128, NB, 32], F32)
            EJ, EJ_b = sr("ej", [128, NB], F32)
            DF, DF_b = sr("df", [128, 2, 32], F32)
            TOKID, TOKID_b = sr("tokid", [128, 32, 2], I32)
            ZERO, ZERO_b = sr("zero", [128, 256], I32)
            SLJ, SLJ_b = sr("slj", [128, 256], I32)
            SLJF, SLJF_b = sr("sljf", [128, 128], F32)
            PIDI, PIDI_b = sr("pidi", [128, 1], I32)
            PIDF, PIDF_b = sr("pidf", [128, 1], F32)
            IWF, IWF_b = sr("iwf", [128, NB], F32)

            lgv = LG[:, :, 0:4]
            lev = LG[:, :, 4:36]
            V(lambda e: e.tensor_reduce(out=GM[:, :], in_=lgv, axis=AX.X, op=ALU.max), reads=[LG_b], writes=[GM_b])
            V(lambda e: e.tensor_tensor(out=OHG[:, :, :], in0=lgv, in1=bc(GM[:, :].unsqueeze(2), [128, 32, 4]), op=ALU.is_equal),
              reads=[LG_b, GM_b], writes=[OHG_b])
            V(lambda e: e.tensor_tensor(out=T4[:, :, :], in0=lgv, in1=bc(GM[:, :].unsqueeze(2), [128, 32, 4]), op=ALU.subtract),
              reads=[LG_b, GM_b], writes=[T4_b])
            A(lambda e: e.activation(out=T4[:, :, :], in_=T4[:, :, :], func=AF.Exp), reads=[T4_b], writes=[T4_b])
            V(lambda e: e.tensor_reduce(out=PG[:, :], in_=T4[:, :, :], axis=AX.X, op=ALU.add), reads=[T4_b], writes=[PG_b])
            V(lambda e: e.reciprocal(out=PG[:, :], in_=PG[:, :]), reads=[PG_b], writes=[PG_b])
            V(lambda e: e.tensor_scalar(PEN[:, :, :], OHG[:, :, :], 1e30, -1e30, ALU.mult, ALU.add), reads=[OHG_b], writes=[PEN_b])
            V(lambda e: e.tensor_tensor(out=LEM[:, :, :].rearrange("p t (g i) -> p t g i", i=8),
                                        in0=lev.rearrange("p t (g i) -> p t g i", i=8),
                                        in1=bc(PEN[:, :, :].unsqueeze(3), [128, 32, 4, 8]), op=ALU.add),
              reads=[LG_b, PEN_b], writes=[LEM_b])
            V(lambda e: e.tensor_reduce(out=M1[:, :], in_=LEM[:, :, :], axis=AX.X, op=ALU.max), reads=[LEM_b], writes=[M1_b])
            V(lambda e: e.tensor_tensor(out=OH1[:, :, :], in0=LEM[:, :, :], in1=bc(M1[:, :].unsqueeze(2), [128, 32, 32]), op=ALU.is_equal),
              reads=[LEM_b, M1_b], writes=[OH1_b])
            V(lambda e: e.scalar_tensor_tensor(out=LEM2[:, :, :], in0=OH1[:, :, :], scalar=-1e30, in1=LEM[:, :, :],
                                               op0=ALU.mult, op1=ALU.add),
              reads=[OH1_b, LEM_b], writes=[LEM2_b])
            V(lambda e: e.tensor_reduce(out=M2[:, :], in_=LEM2[:, :, :], axis=AX.X, op=ALU.max), reads=[LEM2_b], writes=[M2_b])
            V(lambda e: e.tensor_tensor(out=OH2[:, :, :], in0=LEM2[:, :, :], in1=bc(M2[:, :].unsqueeze(2), [128, 32, 32]), op=ALU.is_equal),
              reads=[LEM2_b, M2_b], writes=[OH2_b])
            V(lambda e: e.tensor_tensor(out=DL[:, :], in0=M2[:, :], in1=M1[:, :], op=ALU.subtract), reads=[M1_b, M2_b], writes=[DL_b])
            A(lambda e: e.activation(out=DL[:, :], in_=DL[:, :], func=AF.Exp), reads=[DL_b], writes=[DL_b])
            V(lambda e: e.tensor_scalar(DL[:, :], DL[:, :], 1.0, None, ALU.add), reads=[DL_b], writes=[DL_b])
            V(lambda e: e.reciprocal(out=DL[:, :], in_=DL[:, :]), reads=[DL_b], writes=[DL_b])
            V(lambda e: e.tensor_tensor(out=W12[:, 0, :], in0=PG[:, :], in1=DL[:, :], op=ALU.mult), reads=[PG_b, DL_b], writes=[W12_b])
            V(lambda e: e.tensor_tensor(out=W12[:, 1, :], in0=PG[:, :], in1=W12[:, 0, :], op=ALU.subtract), reads=[PG_b, W12_b], writes=[W12_b])
            V(lambda e: e.tensor_tensor(out=MM[:, :, :], in0=OH1[:, :, :], in1=OH2[:, :, :], op=ALU.add), reads=[OH1_b, OH2_b], writes=[MM_b])
            P(lambda e: e.affine_select(out=US[:, :], in_=ONEF[:, :], pattern=[[1, 128]], compare_op=ALU.is_gt, fill=0.0,
                                        base=0, channel_multiplier=-1), reads=[ONEF_b], writes=[US_b])
            MMf = MM[:, :, :].rearrange("p t e -> p (t e)")
            for hf in range(2):
                T(lambda e, hf=hf: e.matmul(out=banks[hf][0][:, :], lhsT=US[:, :], rhs=MMf[:, hf * 512:(hf + 1) * 512], start=True, stop=True),
                  reads=[US_b, MM_b], writes=[banks[hf][1]])
                T(lambda e, hf=hf: e.matmul(out=banks[2 + hf][0][:, :], lhsT=ONEF[:, :], rhs=MMf[:, hf * 512:(hf + 1) * 512], start=True, stop=True),
                  reads=[ONEF_b, MM_b], writes=[banks[2 + hf][1]])
            TOTf = TOT[:, :, :].rearrange("p t e -> p (t e)")
            for hf in range(2):
                V(lambda e, hf=hf: e.tensor_copy(out=TOTf[:, hf * 512:(hf + 1) * 512], in_=banks[2 + hf][0][:, :]),
                  reads=[banks[2 + hf][1]], writes=[TOT_b])
            chain = [(TOT, TOT_b), (CA, CA_b), (CB, CB_b), (CA, CA_b), (CB, CB_b), (CA, CA_b)]
            for i, sh_ in enumerate([1, 2, 4, 8, 16]):
                (src, srcb), (dst, dstb) = chain[i], chain[i + 1]
                V(lambda e, src=src, dst=dst, sh_=sh_: e.tensor_copy(out=dst[:, 0:sh_, :], in_=src[:, 0:sh_, :]), reads=[srcb], writes=[dstb])
                V(lambda e, src=src, dst=dst, sh_=sh_: e.tensor_tensor(out=dst[:, sh_:32, :], in0=src[:, sh_:32, :], in1=src[:, 0:32 - sh_, :], op=ALU.add),
                  reads=[srcb], writes=[dstb])
            V(lambda e: e.tensor_tensor(out=CB[:, :, :], in0=CA[:, :, :], in1=TOT[:, :, :], op=ALU.subtract), reads=[CA_b, TOT_b], writes=[CB_b])
            CBf = CB[:, :, :].rearrange("p t e -> p (t e)")
            RANKf = RANK[:, :, :].rearrange("p t e -> p (t e)")
            for hf in range(2):
                V(lambda e, hf=hf: e.tensor_tensor(out=RANKf[:, hf * 512:(hf + 1) * 512], in0=banks[hf][0][:, :],
                                                   in1=CBf[:, hf * 512:(hf + 1) * 512], op=ALU.add),
                  reads=[banks[hf][1], CB_b], writes=[RANK_b])
            CNT = CA[:, 31, :]
            P(lambda e: e.iota(THRI[:, :], pattern=[[128, 32]], base=1, channel_multiplier=0), writes=[THRI_b])
            V(lambda e: e.tensor_copy(out=THRF[:, :], in_=THRI[:, :]), reads=[THRI_b], writes=[THRF_b])
            V(lambda e: e.tensor_tensor(out=LEM[:, :, :], in0=bc(CNT.unsqueeze(2), [128, 32, 32]),
                                        in1=bc(THRF[:, :].unsqueeze(1), [128, 32, 32]), op=ALU.is_ge),
              reads=[CA_b, THRF_b], writes=[LEM_b])
            V(lambda e: e.tensor_reduce(out=NBK[:, :], in_=LEM[:, :, :], axis=AX.X, op=ALU.add), reads=[LEM_b], writes=[NBK_b])
            chain2 = [(NBK, NBK_b), (C1, C1_b), (C2, C2_b), (C1, C1_b), (C2, C2_b), (C1, C1_b)]
            for i, sh_ in enumerate([1, 2, 4, 8, 16]):
                (src, srcb), (dst, dstb) = chain2[i], chain2[i + 1]
                V(lambda e, src=src, dst=dst, sh_=sh_: e.tensor_copy(out=dst[:, 0:sh_], in_=src[:, 0:sh_]), reads=[srcb], writes=[dstb])
                V(lambda e, src=src, dst=dst, sh_=sh_: e.tensor_tensor(out=dst[:, sh_:32], in0=src[:, sh_:32], in1=src[:, 0:32 - sh_], op=ALU.add),
                  reads=[srcb], writes=[dstb])
            V(lambda e: e.tensor_tensor(out=PST[:, :], in0=C1[:, :], in1=NBK[:, :], op=ALU.subtract), reads=[C1_b, NBK_b], writes=[PST_b])
            P(lambda e: e.iota(JVI[:, :], pattern=[[1, NB]], base=0, channel_multiplier=0), writes=[JVI_b])
            V(lambda e: e.tensor_copy(out=JVF[:, :], in_=JVI[:, :]), reads=[JVI_b], writes=[JVF_b])
            V(lambda e: e.tensor_tensor(out=CMP2[:, :, :], in0=bc(C1[:, :].unsqueeze(1), [128, NB, 32]),
                                        in1=bc(JVF[:, :].unsqueeze(2), [128, NB, 32]), op=ALU.is_le),
              reads=[C1_b, JVF_b], writes=[CMP2_b])
            V(lambda e: e.tensor_reduce(out=EJ[:, :], in_=CMP2[:, :, :], axis=AX.X, op=ALU.add), reads=[CMP2_b], writes=[EJ_b])
            V(lambda e: e.tensor_scalar(EJ[:, :], EJ[:, :], 31.0, None, ALU.min), reads=[EJ_b], writes=[EJ_b])
            V(lambda e: e.scalar_tensor_tensor(out=LEM2[:, :, :], in0=bc(PST[:, :].unsqueeze(1), [128, 32, 32]), scalar=128.0,
                                               in1=RANK[:, :, :], op0=ALU.mult, op1=ALU.add),
              reads=[PST_b, RANK_b], writes=[LEM2_b])
            for k, (OH, OHb) in enumerate([(OH1, OH1_b), (OH2, OH2_b)]):
                V(lambda e, OH=OH: e.tensor_tensor(out=LEM[:, :, :], in0=OH[:, :, :], in1=LEM2[:, :, :], op=ALU.mult),
                  reads=[OHb, LEM2_b], writes=[LEM_b])
                V(lambda e, k=k: e.tensor_reduce(out=DF[:, k, :], in_=LEM[:, :, :], axis=AX.X, op=ALU.add), reads=[LEM_b], writes=[DF_b])
            V(lambda e: e.tensor_copy(out=DEST[:, :, :], in_=DF[:, :, :]), reads=[DF_b], writes=[DEST_b])
            HX = [sr(f"hx{i}", [128, 1024], BF16) for i in range(4)]
            for t in range(NBLK):
                hx, hxb = HX[t % 4]
                sc.dma("sync", lambda e, t=t, hx=hx: e.dma_start(out=hx[:, :], in_=h2_d[t * 128:(t + 1) * 128, :]), reads=[H2D_b[t]], writes=[hxb])
                for k in range(2):
                    sc.dma("gpsimd", lambda e, k=k, t=t, hx=hx: e.indirect_dma_start(
                        out=xs_d[:, :], out_offset=bass.IndirectOffsetOnAxis(ap=DEST[:, k, t:t + 1], axis=0),
                        in_=hx[:, :], in_offset=None, bounds_check=sc.reg(e, R - 1), oob_is_err=False),
                        reads=[DEST_b, hxb] + XZ_b, writes=[XS_b[2 * t + k]])
            P(lambda e: e.iota(PIDI[:, :], pattern=[[0, 1]], base=0, channel_multiplier=1), writes=[PIDI_b])
            V(lambda e: e.tensor_copy(out=PIDF[:, :], in_=PIDI[:, :]), reads=[PIDI_b], writes=[PIDF_b])
            V(lambda e: e.tensor_scalar(IWF[:, :], EJ[:, :], 128.0, PIDF[:, 0:1], ALU.mult, ALU.add), reads=[EJ_b, PIDF_b], writes=[IWF_b])
            EQ, EQ_b = sr("eq", [128, NB], F32)
            V(lambda e: e.memset(EQ[:, :], 0.0), writes=[EQ_b])
            V(lambda e: e.tensor_tensor(out=EQ[:, 1:NB], in0=EJ[:, 1:NB], in1=EJ[:, 0:NB - 1], op=ALU.is_equal), reads=[EJ_b], writes=[EQ_b])
            V(lambda e: e.memset(EQ[:, :].rearrange("p (l k) -> p l k", k=LB)[:, :, 0:1], 0.0), writes=[EQ_b])
            V(lambda e: e.scalar_tensor_tensor(out=IWF[:, :], in0=EQ[:, :], scalar=1.0e6, in1=IWF[:, :], op0=ALU.mult, op1=ALU.add),
              reads=[EQ_b, IWF_b], writes=[IWF_b])
            V(lambda e: e.tensor_copy(out=IDXW[:, :], in_=IWF[:, :]), reads=[IWF_b], writes=[IDXW_b])
            if dbg:
                sc.dma("sync", lambda e: e.dma_start(out=dbg_d["rt"][:, 0:NB], in_=IWF[:, :]), reads=[IWF_b])
                sc.dma("sync", lambda e: e.dma_start(out=dbg_d["rt"][:, 256:320], in_=DF[:, :, :].rearrange("p k t -> p (k t)")), reads=[DF_b])
                sc.dma("sync", lambda e: e.dma_start(out=dbg_d["rt"][:, 320:384], in_=W12[:, :, :].rearrange("p k t -> p (k t)")), reads=[W12_b])
                sc.dma("sync", lambda e: e.dma_start(out=dbg_d["rt"][:, 384:416], in_=CA[:, 31, :]), reads=[CA_b])
            sc.drain("sync")
            sc.emit()

        if stage == "R":
            return nc, dbg_d

        pb_es = ExitStack()
        with pb_es:
            sB = lambda name, shape, dt: sb(name, shape, dt, stack=pb_es)
            WG = [sB(f"wg{i}", [128, 8, 512], BF16) for i in range(NLANE)]
            WU = [sB(f"wu{i}", [128, 8, 512], BF16) for i in range(NLANE)]
            WD = [sB(f"wd{i}", [128, 4, 1024], BF16) for i in range(NLANE)]
            NXG = 8
            XG = [sB(f"xg{i}", [128, 1024], BF16) for i in range(NXG)]
            XTt = [sB(f"xtt{i}", [128, 8, 128], BF16) for i in range(2)]
            SG, SG_b = sB("sg", [128, 512], F32)
            ACTT = [sB(f"actt{i}", [128, 4, 128], BF16) for i in range(2)]
            ACTM = [sB(f"actm{i}", [128, 512], BF16) for i in range(2)]
            YO = [sB(f"yo{i}", [128, 1024], BF16) for i in range(2)]
            order = [l * LB + k for k in range(LB) for l in range(NLANE)]
            nsteps = len(order)

            def loads(i):
                j = order[i]
                lane = j // LB
                xg, xgb = XG[i % NXG]
                sc.dma("sync", lambda e, j=j, xg=xg: e.dma_start(out=xg[:, :], in_=xs_d[j * 128:(j + 1) * 128, :]),
                       reads=XS_b, writes=[xgb])
                for (Wt, wdl) in ((WG, wg_d), (WU, wu_d), (WD, wd_d)):
                    wt_, wtb = Wt[lane]
                    wflat = wt_[:, :, :].rearrange("p a b -> p (a b)")
                    for hh in range(2):
                        sc.dma("gpsimd", lambda e, j=j, hh=hh, wflat=wflat, wdl=wdl: e.indirect_dma_start(
                            out=wflat[:, hh * 2048:(hh + 1) * 2048], out_offset=None, in_=wdl[hh][:, :],
                            in_offset=bass.IndirectOffsetOnAxis(ap=IDXW[:, j:j + 1], axis=0), bounds_check=sc.reg(e, NE * 128 - 1), oob_is_err=False),
                            reads=[IDXW_b], writes=[wtb], join=(hh > 0))

            def stepT(i):
                xg, xgb = XG[i % NXG]
                tb = i % 2
                xt_, xtb = XTt[i % 2]
                for c in range(8):
                    T(lambda e, c=c, tb=tb, xg=xg: e.transpose(out=bank_bf(tb)[:, c * 128:(c + 1) * 128], in_=xg[:, c:1024:8], identity=IDB[:, :]),
                      reads=[xgb, IDB_b], writes=[banks[tb][1]])
                V(lambda e, tb=tb, xt_=xt_: e.tensor_copy(out=xt_[:, :, :].rearrange("p c t -> p (c t)"), in_=bank_bf(tb)[:, 0:1024]),
                  reads=[banks[tb][1]], writes=[xtb])

            def stepGU(i):
                j = order[i]
                lane = j // LB
                xt_, xtb = XTt[i % 2]
                gb_, ub_ = (2, 3) if i % 2 == 0 else (6, 7)
                for (Wt, bi) in ((WG, gb_), (WU, ub_)):
                    wt_, wtb = Wt[lane]
                    for c in range(8):
                        T(lambda e, wt_=wt_, bi=bi, c=c, xt_=xt_: e.matmul(
                            out=banks[bi][0][:, :], lhsT=xt_[:, c, :], rhs=wt_[:, c, :], start=(c == 0), stop=(c == 7)),
                          reads=[wtb, xtb], writes=[banks[bi][1]])
                at_, atb = ACTT[i % 2]
                am_, amb = ACTM[i % 2]
                A(lambda e, gb_=gb_: e.activation(out=SG[:, :], in_=banks[gb_][0][:, :], func=AF.Silu), reads=[banks[gb_][1]], writes=[SG_b])
                V(lambda e, ub_=ub_, am_=am_: e.tensor_tensor(out=am_[:, :], in0=SG[:, :], in1=banks[ub_][0][:, :], op=ALU.mult),
                  reads=[SG_b, banks[ub_][1]], writes=[amb])
                for jj in range(4):
                    T(lambda e, jj=jj, gb_=gb_, am_=am_: e.transpose(out=bank_bf(gb_)[:, jj * 128:(jj + 1) * 128], in_=am_[:, jj:512:4], identity=IDB[:, :]),
                      reads=[amb, IDB_b], writes=[banks[gb_][1]])
                A(lambda e, gb_=gb_, at_=at_: e.activation(out=at_[:, :, :].rearrange("p a t -> p (a t)"), in_=bank_bf(gb_)[:, 0:512], func=AF.Copy),
                  reads=[banks[gb_][1]], writes=[atb])

            def stepD(i):
                j = order[i]
                lane = j // LB
                at_, atb = ACTT[i % 2]
                wd_, wdb = WD[lane]
                yo_, yob = YO[i % 2]
                for hf in range(2):
                    for jj in range(4):
                        T(lambda e, hf=hf, jj=jj, at_=at_, wd_=wd_: e.matmul(out=banks[4 + hf][0][:, :], lhsT=at_[:, jj, :],
                                                                            rhs=wd_[:, jj, hf * 512:(hf + 1) * 512], start=(jj == 0), stop=(jj == 3)),
                          reads=[atb, wdb], writes=[banks[4 + hf][1]])
                A(lambda e, yo_=yo_: e.activation(out=yo_[:, 0:512], in_=banks[4][0][:, :], func=AF.Copy), reads=[banks[4][1]], writes=[yob])
                V(lambda e, yo_=yo_: e.tensor_copy(out=yo_[:, 512:1024], in_=banks[5][0][:, :]), reads=[banks[5][1]], writes=[yob])
                sc.dma("sync", lambda e, j=j, yo_=yo_: e.dma_start(out=ys_d[j * 128:(j + 1) * 128, :], in_=yo_[:, :]), reads=[yob], writes=[YS_b[j]])

            LOOK = NLANE - 1
            for i0 in range(LOOK):
                loads(i0)
            stepT(0)
            stepGU(0)
            stepT(1)
            for i in range(nsteps):
                if i + LOOK < nsteps:
                    loads(i + LOOK)
                if i + 1 < nsteps:
                    stepGU(i + 1)
                if i + 2 < nsteps:
                    stepT(i + 2)
                stepD(i)
            sc.drain("sync")
            sc.emit()

        pc_es = ExitStack()
        with pc_es:
            sC = lambda name, shape, dt: sb(name, shape, dt, stack=pc_es)
            FG, FG_b = sC("fgt", [128, 1024], F32)
            NCB = 6
            Y1 = [sC(f"y1{i}", [128, 1024], F32) for i in range(NCB)]
            Y2 = [sC(f"y2{i}", [128, 1024], F32) for i in range(NCB)]
            G1 = [sC(f"g1{i}", [128, 1024], BF16) for i in range(NCB)]
            G2B = [sC(f"g2b{i}", [128, 1024], BF16) for i in range(NCB)]
            X1 = [sC(f"x1{i}", [128, 1024], F32) for i in range(NCB)]
            ST3, _ = sC("st3", [128, 8], F32)
            st3b = [Buf() for _ in range(3)]
            sc.dma("sync", lambda e: e.dma_start(out=FG[:, :], in_=fg_d[:, :]), writes=[FG_b])
            G2, G2_b = sC("g2c", [128, 1024], F32)
            sc.dma("sync", lambda e: e.dma_start(out=G2[:, :], in_=g2s_d[:, :]), reads=[G2D_b], writes=[G2_b])
            def c_loads(t):
                p = t % NCB
                for k, (yy, yyb) in enumerate((G1[p], G2B[p])):
                    sc.dma("gpsimd", lambda e, k=k, t=t, yy=yy: e.indirect_dma_start(
                        out=yy[:, :], out_offset=None, in_=ys_d[:, :],
                        in_offset=bass.IndirectOffsetOnAxis(ap=DEST[:, k, t:t + 1], axis=0), bounds_check=sc.reg(e, R - 1), oob_is_err=False),
                        reads=[DEST_b] + YS_b, writes=[yyb])
                x1, x1b = X1[p]
                sc.dma("sync", lambda e, t=t, x1=x1: e.dma_start(out=x1[:, :], in_=x1_d[t * 128:(t + 1) * 128, :]), reads=[X1D_b[t]], writes=[x1b])

            st3 = [[Buf() for _ in range(3)] for _ in range(2)]

            def c_partA(t):
                p = t % NCB
                y1, y1b = Y1[p]
                y2, y2b = Y2[p]
                x1, x1b = X1[p]
                o = (t % 2) * 3
                sb3 = st3[t % 2]
                ga, gab = G1[p]
                gb2, gbb = G2B[p]
                A(lambda e, t=t, y1=y1, ga=ga: e.activation(out=y1[:, :], in_=ga[:, :], func=AF.Copy, scale=W12[:, 0, t:t + 1]), reads=[gab, W12_b], writes=[y1b])
                V(lambda e, t=t, y1=y1, gb2=gb2: e.scalar_tensor_tensor(out=y1[:, :], in0=gb2[:, :], scalar=W12[:, 1, t:t + 1], in1=y1[:, :],
                                                                        op0=ALU.mult, op1=ALU.add),
                  reads=[y1b, gbb, W12_b], writes=[y1b])
                V(lambda e, y1=y1: e.tensor_tensor(out=y1[:, :], in0=y1[:, :], in1=G2[:, :], op=ALU.mult), reads=[y1b, G2_b], writes=[y1b])
                V(lambda e, y1=y1, x1=x1: e.tensor_tensor(out=x1[:, :], in0=x1[:, :], in1=y1[:, :], op=ALU.add), reads=[y1b, x1b], writes=[x1b])
                V(lambda e, o=o: e.memset(ST3[:, o:o + 1], 0.0), writes=[sb3[0]])
                A(lambda e, x1=x1, y2=y2, o=o: e.activation(out=y2[:, :], in_=x1[:, :], func=AF.Square, accum_out=ST3[:, o:o + 1]),
                  reads=[x1b], writes=[y2b, sb3[0]])
                V(lambda e, o=o: e.tensor_scalar(ST3[:, o + 1:o + 2], ST3[:, o:o + 1], 1.0 / D, EPS, ALU.mult, ALU.add), reads=[sb3[0]], writes=[sb3[1]])
                P(lambda e, o=o: e.tensor_tensor(out=ST3[:, o + 2:o + 3], in0=ST3[:, o + 1:o + 2], in1=NEGH[:, 0:1], op=ALU.pow),
                  reads=[sb3[1], NEGH_b], writes=[sb3[2]])

            def c_partB(t):
                p = t % NCB
                y2, y2b = Y2[p]
                x1, x1b = X1[p]
                o = (t % 2) * 3
                sb3 = st3[t % 2]
                V(lambda e, x1=x1, y2=y2, o=o: e.scalar_tensor_tensor(out=y2[:, :], in0=x1[:, :], scalar=ST3[:, o + 2:o + 3], in1=FG[:, :],
                                                                      op0=ALU.mult, op1=ALU.mult),
                  reads=[x1b, sb3[2], FG_b], writes=[y2b])
                sc.dma("sync", lambda e, t=t, y2=y2: e.dma_start(out=out_d[t * 128:(t + 1) * 128, :], in_=y2[:, :]), reads=[y2b])

            for t0 in range(NCB - 1):
                c_loads(t0)
            c_partA(0)
            for t in range(NBLK):
                if t + NCB - 1 < NBLK:
                    c_loads(t + NCB - 1)
                if t + 1 < NBLK:
                    c_partA(t + 1)
                c_partB(t)
            sc.drain("sync")
            sc.emit()
    return nc, dbg_d


def _t5_thresholds():
    n = np.arange(0, 4200, dtype=np.int64)
    nf = np.maximum(n, 1).astype(np.float32)
    large = 16 + (np.log(nf / np.float32(16)) / np.float32(np.log(128 / 16)) * np.float32(16)).astype(np.int32)
    large = np.minimum(large, 31)
    bucket = np.where(n < 16, n, large)
    thr = np.zeros(32, np.float64)
    for k in range(1, 32):
        idx = np.nonzero(bucket >= k)[0]
        thr[k] = float(idx[0]) if len(idx) else 1e9
    return thr


def _prep_shared(inp):
    f = lambda a: np.ascontiguousarray(np.asarray(a, dtype=np.float32))
    sh = {}
    sh["zrows"] = np.zeros((1536, D), dtype=ml_dtypes.bfloat16)
    sh["relb"] = f(np.broadcast_to(np.asarray(inp["rel_bias"], np.float32).reshape(1, 256), (128, 256)))
    wada = np.asarray(inp["w_ada"], np.float32)[0].reshape(8, 128, 6, 1024)
    sh["w_ada"] = f(wada.transpose(1, 2, 0, 3))
    bada = np.asarray(inp["b_ada"], np.float32)[0]
    sh["b_ada_f"] = f(bada.reshape(6, 8, 128).transpose(2, 0, 1))
    sh["b_ada_r"] = f(np.broadcast_to(bada.reshape(1, 6, 1024), (128, 6, 1024)))
    sh["n1g"] = f(np.asarray(inp["norm1_g"], np.float32)[0].reshape(8, 128).T)
    sh["n2g"] = f(np.broadcast_to(np.asarray(inp["norm2_g"], np.float32)[0].reshape(1, 1024), (128, 1024)))
    sh["fg"] = f(np.broadcast_to(np.asarray(inp["final_g"], np.float32).reshape(1, 1024), (128, 1024)))
    w = np.asarray(inp["w_in"], np.float32)[0]
    wdev = np.concatenate([w[:, 0:512], w[:, 512:576], w[:, 512:576], w[:, 576:640], w[:, 576:640],
                           w[:, 640:768], w[:, 768:1280], w[:, 1280:1792], w[:, 1792:2816], w[:, 2816:3840]], axis=1)
    sh["w_in"] = f(wdev.reshape(8, 128, INW).transpose(1, 0, 2))
    sh["sinks"] = f(np.broadcast_to(np.asarray(inp["sinks"], np.float32)[0].reshape(1, 8), (128, 8)))
    sh["lng"] = f(np.broadcast_to(np.asarray(inp["gm_ln_g"], np.float32)[0].reshape(1, 512), (128, 512)))
    sh["lnb"] = f(np.broadcast_to(np.asarray(inp["gm_ln_b"], np.float32)[0].reshape(1, 512), (128, 512)))
    sh["wst"] = f(np.asarray(inp["gm_w_s"], np.float32)[0].transpose(2, 0, 1))
    sh["bs"] = f(np.asarray(inp["gm_b_s"], np.float32)[0].reshape(1, 512))
    sh["p_a"] = f(np.asarray(inp["p_a"], np.float32)[0].reshape(4, 128, 1024).transpose(1, 0, 2))
    sh["p_b"] = f(np.asarray(inp["p_b"], np.float32)[0].reshape(4, 128, 1024).transpose(1, 0, 2))
    sh["w_o"] = f(np.asarray(inp["w_o"], np.float32)[0].reshape(8, 128, 1024).transpose(1, 0, 2))
    wr = np.concatenate([np.asarray(inp["w_router_g"], np.float32)[0], np.asarray(inp["w_router_e"], np.float32)[0]], axis=1)
    sh["w_r"] = f(wr.reshape(8, 128, 36).transpose(1, 0, 2))
    sh["b_r"] = f(np.concatenate([np.asarray(inp["b_router_g"], np.float32)[0],
                                  np.asarray(inp["b_router_e"], np.float32)[0]]).reshape(1, 36))
    for nm in ("w_gate", "w_up", "w_down"):
        w2 = np.asarray(inp[nm], np.float32)[0].reshape(NE * 128, 2, 2048)
        sh[nm + "0"] = f(w2[:, 0, :])
        sh[nm + "1"] = f(w2[:, 1, :])
    return sh


def _prep_core(inp, b):
    m = {}
    m["x"] = np.ascontiguousarray(np.asarray(inp["x"], np.float32)[b])
    m["c"] = np.ascontiguousarray(np.asarray(inp["c"], np.float32)[b].reshape(8, 128).T)
    pos = np.asarray(inp["positions"], np.int32)[b]
    m["posq"] = np.ascontiguousarray(np.broadcast_to(pos[128:256].reshape(1, 128), (128, 128))).astype(np.int32)
    m["posk"] = np.ascontiguousarray(np.stack([pos[0:128], pos[128:256]], axis=1)).astype(np.int32)
    return m


def run(inputs, stage="full", dbg=False, n_cores=8):
    nc, dbg_d = build_nc(stage=stage, dbg=dbg)
    sh = _prep_shared(inputs)
    in_maps = []
    for b in range(n_cores):
        m = dict(sh)
        m.update(_prep_core(inputs, b))
        in_maps.append(m)
    res = run_bass_kernel_spmd(nc, in_maps, core_ids=list(range(n_cores)))
    return res


def kernel(**inputs):
    res = run(inputs)
    out = np.stack([np.asarray(r["out"], np.float32).reshape(S, D) for r in res.results], axis=0)
    return out
```

```python
import numpy as np
import ml_dtypes
from contextlib import ExitStack
import concourse.bass as bass
import concourse.mybir as mybir
from concourse.bass_utils import run_bass_kernel_spmd

F32 = mybir.dt.float32
BF16 = mybir.dt.bfloat16
I32 = mybir.dt.int32
AF = mybir.ActivationFunctionType
ALU = mybir.AluOpType
AX = mybir.AxisListType

D = 1024
S = 4096
NBLK = 32
ST = 512
NST = 8
BPS = 4
NE = 32
NB = 96
NLANE = 6
LB = NB // NLANE
R = NB * 128
INW = 3968
C_Q, C_K, C_V, C_GU, C_GV, C_GA, C_GB = 0, 512, 768, 896, 1408, 1920, 2944
EPS = 1e-6
GELU_FN = AF.Gelu_apprx_tanh


class Buf:
    __slots__ = ("w", "r", "ro")

    def __init__(self):
        self.w = []
        self.r = []
        self.ro = False


class Q:
    def __init__(self, name, sems, dma_sems):
        self.name = name
        self.spare = list(sems)
        self.sem = self.spare.pop()
        self.cnt = 0
        self.waited = {}
        self.prog = []
        self.dma = [[sm, 0] for sm in dma_sems]
        self.dma_i = 0


class Sched:
    def __init__(self, nc, es):
        self.nc = nc
        sems = [es.enter_context(nc.semaphore(f"s{i}")) for i in range(96)]
        it = iter(sems)
        take = lambda n: [next(it) for _ in range(n)]
        self.q = {
            "tensor": Q("tensor", take(4), []),
            "vector": Q("vector", take(3), []),
            "scalar": Q("scalar", take(3), take(4)),
            "gpsimd": Q("gpsimd", take(3), take(24)),
            "sync": Q("sync", take(2), take(24)),
        }
        self.all_dma = []

    def _wait(self, q, tok):
        sem, val = tok
        k = id(sem)
        if q.waited.get(k, 0) >= val:
            return
        q.waited[k] = val
        q.prog.append(("w", sem, val))

    def _deps(self, q, reads, writes, skip_self, join=False):
        for b in reads:
            for t in b.w:
                if not (skip_self and t[0] is q.sem):
                    self._wait(q, t)
        for b in writes:
            if not join:
                for t in b.w:
                    if not (skip_self and t[0] is q.sem):
                        self._wait(q, t)
            for t in b.r:
                if not (skip_self and t[0] is q.sem):
                    self._wait(q, t)

    def op(self, qn, fn, reads=(), writes=()):
        q = self.q[qn]
        if q.cnt >= 24000:
            q.sem = q.spare.pop()
            q.cnt = 0
        self._deps(q, reads, writes, qn == "tensor")
        q.cnt += 1
        tok = (q.sem, q.cnt)
        q.prog.append(("i", fn, q.sem, 1))
        for b in writes:
            b.w = [tok]
            b.r = []
        for b in reads:
            if not b.ro and b not in writes:
                b.r.append(tok)
        return tok

    def dma(self, qn, fn, reads=(), writes=(), join=False):
        q = self.q[qn]
        self._deps(q, reads, writes, False, join=join)
        slot = q.dma[q.dma_i % len(q.dma)]
        q.dma_i += 1
        if slot[1] > 0:
            self._wait(q, (slot[0], slot[1]))
        slot[1] += 16
        tok = (slot[0], slot[1])
        q.prog.append(("i", fn, slot[0], 16))
        for b in writes:
            if join:
                b.w = b.w + [tok]
            else:
                b.w = [tok]
                b.r = []
        for b in reads:
            if not b.ro and b not in writes:
                b.r.append(tok)
        self.all_dma.append(tok)
        return tok

    def drain(self, qn="sync"):
        q = self.q[qn]
        last = {}
        for sem, val in self.all_dma:
            last[id(sem)] = (sem, max(val, last.get(id(sem), (sem, 0))[1]))
        for tok in last.values():
            self._wait(q, tok)
        self.all_dma = []

    def reg(self, e, val):
        if val not in self.regcache:
            self.regcache[val] = e.to_reg(val)
        return self.regcache[val]

    def emit(self):
        self.regcache = {}
        with self.nc.Block() as blk:
            for name in ["tensor", "vector", "scalar", "gpsimd", "sync"]:
                q = self.q[name]
                items = q.prog
                q.prog = []

                def body(e, items=items):
                    for it in items:
                        if it[0] == "w":
                            e.wait_ge(it[1], it[2])
                        else:
                            it[1](e).then_inc(it[2], it[3])

                getattr(blk, name)(body)


def bc(ap, shape):
    return ap.broadcast_to(list(shape))


def build_nc(stage="full", dbg=False):
    nc = bass.Bass("TRN2", target_bir_lowering=False)

    def din(name, shape, dt=F32):
        return nc.dram_tensor(name, list(shape), dt, kind="ExternalInput").ap()

    x_d = din("x", [S, D])
    c_d = din("c", [128, 8])
    posq_d = din("posq", [128, 128], I32)
    posk_d = din("posk", [128, 2], I32)
    rb_d = din("relb", [128, 256])
    wada_d = din("w_ada", [128, 6, 8, 1024])
    badaf_d = din("b_ada_f", [128, 6, 8])
    badar_d = din("b_ada_r", [128, 6, 1024])
    n1g_d = din("n1g", [128, 8])
    n2g_d = din("n2g", [128, 1024])
    fg_d = din("fg", [128, 1024])
    win_d = din("w_in", [128, 8, INW])
    sinks_d = din("sinks", [128, 8])
    lng_d = din("lng", [128, 512])
    lnb_d = din("lnb", [128, 512])
    wst_d = din("wst", [128, 4, 128])
    bs_d = din("bs", [1, 512])
    pa_d = din("p_a", [128, 4, 1024])
    pb_d = din("p_b", [128, 4, 1024])
    wo_d = din("w_o", [128, 8, 1024])
    wr_d = din("w_r", [128, 8, 36])
    br_d = din("b_r", [1, 36])
    wg_d = [din(f"w_gate{i}", [NE * 128, 2048]) for i in range(2)]
    wu_d = [din(f"w_up{i}", [NE * 128, 2048]) for i in range(2)]
    wd_d = [din(f"w_down{i}", [NE * 128, 2048]) for i in range(2)]
    zr_d = din("zrows", [1536, D], BF16)
    out_d = nc.dram_tensor("out", [S, D], F32, kind="ExternalOutput").ap()
    h2_d = nc.dram_tensor("h2s", [S, D], BF16, kind="Internal").ap()
    x1_d = nc.dram_tensor("x1s", [S, D], F32, kind="Internal").ap()
    ys_d = nc.dram_tensor("yss", [R, D], BF16, kind="Internal").ap()
    st_d = nc.dram_tensor("sts", [R, 2], I32, kind="Internal").ap()
    g2s_d = nc.dram_tensor("g2s", [128, 1024], F32, kind="Internal").ap()
    xs_d = nc.dram_tensor("xss", [R, D], BF16, kind="Internal").ap()
    dbg_d = {}
    if dbg:
        dbg_d["lg"] = nc.dram_tensor("dbg_lg", [128, NBLK * 36], F32, kind="ExternalOutput").ap()
        dbg_d["x1"] = nc.dram_tensor("dbg_x1", [S, D], F32, kind="ExternalOutput").ap()
        dbg_d["rt"] = nc.dram_tensor("dbg_rt", [128, 512], F32, kind="ExternalOutput").ap()

    es = ExitStack()
    with es:
        sc = Sched(nc, es)
        T = lambda *a, **k: sc.op("tensor", *a, **k)
        V = lambda *a, **k: sc.op("vector", *a, **k)
        A = lambda *a, **k: sc.op("scalar", *a, **k)
        P = lambda *a, **k: sc.op("gpsimd", *a, **k)

        def sb(name, shape, dt, stack=es):
            t = stack.enter_context(nc.sbuf_tensor(name, list(shape), dt))
            return t, Buf()

        X1D_b = [Buf() for _ in range(NBLK)]
        H2D_b = [Buf() for _ in range(NBLK)]
        YS_b = [Buf() for _ in range(NB)]
        XS_b = [Buf() for _ in range(2 * NBLK)]
        XZ_b = [Buf() for _ in range(NST)]
        banks = []
        for i in range(8):
            t = es.enter_context(nc.psum_tensor(f"bank{i}", [128, 512], F32))
            banks.append((t, Buf()))

        def bank_bf(i):
            return banks[i][0][:, :].bitcast(BF16)

        IDB, IDB_b = sb("idb", [128, 128], BF16)
        IDF, IDF_b = sb("idf", [128, 128], F32)
        ONEF, ONEF_b = sb("onef", [128, 128], F32)
        G2D_b = Buf()
        LG, LG_b = sb("lg", [128, NBLK, 36], F32)
        NEGH, NEGH_b = sb("negh", [128, 8], F32)
        W12, W12_b = sb("w12", [128, 2, 32], F32)
        DEST, DEST_b = sb("dest", [128, 2, 32], I32)
        SLOT, SLOT_b = sb("slot", [128, NB], I32)
        IDXW, IDXW_b = sb("idxw", [128, NB], I32)

        V(lambda e: e.memset(ONEF[:, :], 1.0), writes=[ONEF_b])
        V(lambda e: e.memset(NEGH[:, :], -0.5), writes=[NEGH_b])
        P(lambda e: e.affine_select(out=IDF[:, :], in_=ONEF[:, :], pattern=[[-1, 128]],
                                    compare_op=ALU.is_equal, fill=0.0, base=0, channel_multiplier=1),
          reads=[ONEF_b], writes=[IDF_b])
        V(lambda e: e.tensor_copy(out=IDB[:, :], in_=IDF[:, :]), reads=[IDF_b], writes=[IDB_b])

        pa_es = ExitStack()
        with pa_es:
            def sa(name, shape, dt):
                return sb(name, shape, dt, stack=pa_es)

            WIN, WIN_b = sa("win", [128, 8, INW], BF16)
            PAW, PAW_b = sa("paw", [128, 4, 1024], BF16)
            PBW, PBW_b = sa("pbw", [128, 4, 1024], BF16)
            WOW, WOW_b = sa("wow", [128, 8, 1024], BF16)
            KT, KT_b = sa("kt", [128, 2, 2, 640], BF16)
            VA, VA_b = sa("va", [128, 5, 2, 65], BF16)
            BMH, BMH_b = sa("bmh", [128, 2, 8, 128], BF16)
            BML, BML_b = sa("bml", [128, 2, 8, 128], BF16)
            G1H, G1H_b = sa("g1h", [128, 1024], F32)
            A2, A2_b = sa("a2", [128, 1024], F32)
            SH2, SH2_b = sa("sh2", [128, 1024], F32)
            LNG, LNG_b = sa("lngt", [128, 512], F32)
            LNB, LNB_b = sa("lnbt", [128, 512], F32)
            WT, WT_b = sa("wt", [128, 4, 128], BF16)
            BS, BS_b = sa("bst", [1, 512], F32)
            WR, WR_b = sa("wr", [128, 8, 36], F32)
            BR, BR_b = sa("brt", [1, 36], F32)
            ES_, ES_b = sa("es", [128, 8], F32)
            A1, A1_b = sa("a1", [128, 8], F32)
            SH1, SH1_b = sa("sh1", [128, 8], F32)
            SM, SM_b = sa("small", [128, 64], F32)
            smb = [Buf() for _ in range(8)]
            p0_es = ExitStack()
            p0_es.__enter__()
            sa0 = lambda name, shape, dt: sb(name, shape, dt, stack=p0_es)


            thr = _t5_thresholds()
            RBT, RBT_b = sa0("rbt", [128, 32, 8], F32)
            PQ, PQ_b = sa0("pq", [128, 128], I32)
            PK, PK_b = sa0("pk", [128, 2], I32)
            PKF, PKF_b = sa0("pkf", [128, 2], F32)
            DD, DD_b = sa0("dd", [128, 2, 128], F32)
            IND, IND_b = sa0("ind", [128, 2, 128], F32)
            sc.dma("sync", lambda e: e.dma_start(out=RBT[:, :, :], in_=rb_d.rearrange("p (k h) -> p k h", h=8)), writes=[RBT_b])
            sc.dma("sync", lambda e: e.dma_start(out=PQ[:, :], in_=posq_d[:, :]), writes=[PQ_b])
            sc.dma("sync", lambda e: e.dma_start(out=PK[:, :], in_=posk_d[:, :]), writes=[PK_b])
            V(lambda e: e.tensor_copy(out=PKF[:, :], in_=PK[:, :]), reads=[PK_b], writes=[PKF_b])
            V(lambda e: e.tensor_copy(out=DD[:, 0, :], in_=PQ[:, :]), reads=[PQ_b], writes=[DD_b])
            V(lambda e: e.tensor_copy(out=DD[:, 1, :], in_=PQ[:, :]), reads=[PQ_b], writes=[DD_b])
            for hf in range(2):
                V(lambda e, hf=hf: e.tensor_scalar(DD[:, hf, :], DD[:, hf, :], PKF[:, hf:hf + 1], 0.0, ALU.subtract, ALU.max),
                  reads=[DD_b, PKF_b], writes=[DD_b])
            DLT, DLT_b = sa0("dlt", [128, 32, 8], F32)
            V(lambda e: e.tensor_copy(out=DLT[:, 0:1, :], in_=RBT[:, 0:1, :]), reads=[RBT_b], writes=[DLT_b])
            V(lambda e: e.tensor_tensor(out=DLT[:, 1:32, :], in0=RBT[:, 1:32, :], in1=RBT[:, 0:31, :], op=ALU.subtract),
              reads=[RBT_b], writes=[DLT_b])
            BA, _ = sa0("ba", [128, 2, 8, 128], F32)
            BAh = [Buf() for _ in range(8)]
            IND2, IND2_b = sa0("ind2", [128, 2, 128], F32)
            inds = [(IND, IND_b), (IND2, IND2_b)]
            for h in range(8):
                V(lambda e, h=h: e.tensor_copy(out=BA[:, :, h, :], in_=bc(DLT[:, 0, h:h + 1].unsqueeze(1), [128, 2, 128])),
                  reads=[DLT_b], writes=[BAh[h]])
            def t5_chunk(k0, k1):
                for k in range(k0, k1):
                    it_, itb = inds[k % 2]
                    V(lambda e, k=k, it_=it_: e.tensor_scalar(it_[:, :, :], DD[:, :, :], float(thr[k]), None, ALU.is_ge),
                      reads=[DD_b], writes=[itb])
                    for h in range(8):
                        V(lambda e, k=k, h=h, it_=it_: e.scalar_tensor_tensor(
                            out=BA[:, :, h, :], in0=it_[:, :, :], scalar=DLT[:, k, h:h + 1],
                            in1=BA[:, :, h, :], op0=ALU.mult, op1=ALU.add),
                            reads=[itb, DLT_b, BAh[h]], writes=[BAh[h]])
            t5_bounds = [1, 7, 12, 17, 22, 27, 32]
            def cast_load(dst_ap, src_ap, buf, join=False):
                sc.dma("gpsimd", lambda e: e.dma_start(out=dst_ap, in_=src_ap), writes=[buf], join=join)

            CL, CL_b = sa0("cl", [128, 8], F32)
            CS, CS_b = sa0("cs", [128, 8], BF16)
            CSR, CSR_b = sa0("csr", [128, 8, 128], BF16)
            WAD = [sa0(f"wad{i}", [128, 8, 1024], BF16) for i in range(2)]
            sc.dma("sync", lambda e: e.dma_start(out=CL[:, :], in_=c_d[:, :]), writes=[CL_b])
            A(lambda e: e.activation(out=CL[:, :], in_=CL[:, :], func=AF.Silu), reads=[CL_b], writes=[CL_b])
            V(lambda e: e.tensor_copy(out=CS[:, :], in_=CL[:, :]), reads=[CL_b], writes=[CS_b])
            V(lambda e: e.tensor_copy(out=CSR[:, :, :], in_=bc(CL[:, :].unsqueeze(2), [128, 8, 128])),
              reads=[CL_b], writes=[CSR_b])
            BF_, BF_b = sa0("badaf", [128, 6, 8], F32)
            sc.dma("sync", lambda e: e.dma_start(out=BF_[:, :, :], in_=badaf_d[:, :, :]), writes=[BF_b])
            N1G, N1G_b = sa0("n1gt", [128, 8], F32)
            sc.dma("sync", lambda e: e.dma_start(out=N1G[:, :], in_=n1g_d[:, :]), writes=[N1G_b])
            sc.dma("sync", lambda e: e.dma_start(out=G1H[:, :], in_=badar_d[:, 2, :]), writes=[G1H_b])
            sc.dma("sync", lambda e: e.dma_start(out=SH2[:, :], in_=badar_d[:, 3, :]), writes=[SH2_b])
            sc.dma("sync", lambda e: e.dma_start(out=A2[:, :], in_=badar_d[:, 4, :]), writes=[A2_b])
            G2, G2_b = sa0("g2t", [128, 1024], F32)
            sc.dma("sync", lambda e: e.dma_start(out=G2[:, :], in_=badar_d[:, 5, :]), writes=[G2_b])
            N2G, N2G_b = sa0("n2gt", [128, 1024], F32)
            sc.dma("sync", lambda e: e.dma_start(out=N2G[:, :], in_=n2g_d[:, :]), writes=[N2G_b])

            for v in range(6):
                t5_chunk(t5_bounds[v], t5_bounds[v + 1])
                wt_, wb_ = WAD[v % 2]
                for kk in range(4):
                    cast_load(wt_[:, 2 * kk:2 * kk + 2, :], wada_d[:, v, 2 * kk:2 * kk + 2, :], wb_, join=(kk > 0))
                if v < 2:
                    bk, bkb = banks[v]
                    for cc in range(8):
                        for kc in range(8):
                            T(lambda e, cc=cc, kc=kc, wt_=wt_, bk=bk: e.matmul(
                                out=bk[:, cc:cc + 1], lhsT=wt_[:, kc, cc * 128:(cc + 1) * 128],
                                rhs=CS[:, kc:kc + 1], start=(kc == 0), stop=(kc == 7)),
                              reads=[wb_, CS_b], writes=[bkb])
                    if v == 0:
                        V(lambda e, bk=bk: e.tensor_tensor(out=SH1[:, :], in0=bk[:, 0:8], in1=BF_[:, 0, :], op=ALU.add),
                          reads=[bkb, BF_b], writes=[SH1_b])
                    else:
                        V(lambda e, bk=bk: e.tensor_tensor(out=A1[:, :], in0=bk[:, 0:8], in1=BF_[:, 1, :], op=ALU.add),
                          reads=[bkb, BF_b], writes=[A1_b])
                        V(lambda e: e.scalar_tensor_tensor(out=A1[:, :], in0=A1[:, :], scalar=1.0, in1=N1G[:, :],
                                                           op0=ALU.add, op1=ALU.mult),
                          reads=[A1_b, N1G_b], writes=[A1_b])
                else:
                    dst, dstb = {2: (G1H, G1H_b), 3: (SH2, SH2_b), 4: (A2, A2_b), 5: (G2, G2_b)}[v]
                    for hf in range(2):
                        bk, bkb = banks[2 + hf]
                        for kc in range(8):
                            T(lambda e, kc=kc, hf=hf, wt_=wt_, bk=bk: e.matmul(
                                out=bk[:, :], lhsT=CSR[:, kc, :], rhs=wt_[:, kc, hf * 512:(hf + 1) * 512],
                                start=(kc == 0), stop=(kc == 7)),
                              reads=[wb_, CSR_b], writes=[bkb])
                        V(lambda e, hf=hf, bk=bk, dst=dst: e.tensor_tensor(
                            out=dst[:, hf * 512:(hf + 1) * 512], in0=bk[:, :], in1=dst[:, hf * 512:(hf + 1) * 512], op=ALU.add),
                          reads=[bkb, dstb], writes=[dstb])
                    if v == 2:
                        V(lambda e: e.tensor_scalar(G1H[:, :], G1H[:, :], 0.5, None, ALU.mult), reads=[G1H_b], writes=[G1H_b])
                    if v == 4:
                        V(lambda e: e.scalar_tensor_tensor(out=A2[:, :], in0=A2[:, :], scalar=1.0, in1=N2G[:, :],
                                                           op0=ALU.add, op1=ALU.mult),
                          reads=[A2_b, N2G_b], writes=[A2_b])

            for kc in range(8):
                for hh in range(2):
                    cast_load(WIN[:, kc, hh * 1984:(hh + 1) * 1984], win_d[:, kc, hh * 1984:(hh + 1) * 1984], WIN_b, join=(kc + hh > 0))
            for kc in range(4):
                cast_load(PAW[:, kc, :], pa_d[:, kc, :], PAW_b, join=(kc > 0))
                cast_load(PBW[:, kc, :], pb_d[:, kc, :], PBW_b, join=(kc > 0))
            for kc in range(8):
                cast_load(WOW[:, kc, :], wo_d[:, kc, :], WOW_b, join=(kc > 0))
            cast_load(WT[:, :, :], wst_d[:, :, :], WT_b)
            P(lambda e: e.affine_select(out=WT[:, :, :], in_=WT[:, :, :], pattern=[[0, 4], [1, 128]],
                                        compare_op=ALU.is_ge, fill=0.0, base=0, channel_multiplier=-1),
              reads=[WT_b], writes=[WT_b])
            for (dap, dstb, sap) in [(LNG[:, :], LNG_b, lng_d[:, :]), (LNB[:, :], LNB_b, lnb_d[:, :]), (BS[:, :], BS_b, bs_d[:, :]),
                                     (WR[:, :, :], WR_b, wr_d[:, :, :]), (BR[:, :], BR_b, br_d[:, :]), (ES_[:, :], ES_b, sinks_d[:, :])]:
                sc.dma("sync", lambda e, dap=dap, sap=sap: e.dma_start(out=dap, in_=sap), writes=[dstb])
            A(lambda e: e.activation(out=ES_[:, :], in_=ES_[:, :], func=AF.Exp), reads=[ES_b], writes=[ES_b])
            V(lambda e: e.memset(VA[:, :, :, :], 1.0), writes=[VA_b])
            V(lambda e: e.memset(KT[:, :, :, :], 0.0), writes=[KT_b])

            P(lambda e: e.affine_select(out=BA[:, 0, :, :], in_=BA[:, 0, :, :], pattern=[[0, 8], [-1, 128]],
                                        compare_op=ALU.is_gt, fill=-30000.0, base=0, channel_multiplier=1),
              reads=BAh, writes=BAh)
            P(lambda e: e.affine_select(out=BA[:, 1, :, :], in_=BA[:, 1, :, :], pattern=[[0, 8], [1, 128]],
                                        compare_op=ALU.is_ge, fill=-30000.0, base=0, channel_multiplier=-1),
              reads=BAh, writes=BAh)
            V(lambda e: e.tensor_copy(out=BMH[:, :, :, :], in_=BA[:, :, :, :]), reads=BAh, writes=[BMH_b])
            V(lambda e: e.tensor_tensor(out=BA[:, :, :, :], in0=BA[:, :, :, :], in1=BMH[:, :, :, :], op=ALU.subtract),
              reads=BAh + [BMH_b], writes=BAh)
            V(lambda e: e.tensor_copy(out=BML[:, :, :, :], in_=BA[:, :, :, :]), reads=BAh, writes=[BML_b])

            for b_ in (WIN_b, PAW_b, PBW_b, WOW_b):
                pass

            sc.dma("sync", lambda e: e.dma_start(out=g2s_d[:, :], in_=G2[:, :]), reads=[G2_b], writes=[G2D_b])
            sc.drain("sync")
            sc.emit()
            p0_es.__exit__(None, None, None)
            XT, _ = sa("xt", [128, 4, 1024], F32)
            xb = [Buf() for _ in range(4)]
            XN = [sa(f"xn{i}", [128, 1024], BF16) for i in range(2)]
            HT, _ = sa("ht", [128, 8, ST], BF16)
            HTk = [Buf() for _ in range(8)]
            QT, QT_b = sa("qt", [128, 4, ST], BF16)
            UT, UT_b = sa("ut", [128, 4, ST], BF16)
            YA, YA_b = sa("ya", [128, 512], BF16)
            YAT, YAT_b = sa("yat", [128, 4, ST], BF16)
            YBT, YBT_b = sa("ybt", [128, 4, ST], BF16)
            MT, MT_b = sa("mt", [128, 8, ST], BF16)
            RA, RA_b = sa("ra", [128, 512], F32)
            RB, RB_b = sa("rb", [128, 512], F32)
            RC, RC_b = sa("rc", [128, 512], F32)
            RD, RD_b = sa("rd", [128, 1024], F32)
            RE, RE_b = sa("re", [128, 1024], BF16)
            VNS = [sa(f"vn{i}", [128, 512], BF16) for i in range(2)]
            H2T, H2T_b = sa("h2t", [128, 8, 128], F32)
            H2Tb_b = Buf()
            LGT, LGT_b = sa("lgt", [128, 128], F32)
            PT_bf = RD[:, :].bitcast(BF16)
            n_st = NST if stage != "A1" else 1
            re_half = [Buf(), Buf()]
            pj = [0]
            pf = [0]

            def next_proj_bank():
                pj[0] ^= 1
                return banks[pj[0]]

            for s in range(n_st):
                for b in range(BPS):
                    n = s * BPS + b
                    sc.dma("sync", lambda e, b=b, n=n: e.dma_start(out=XT[:, b, :], in_=x_d[n * 128:(n + 1) * 128, :]),
                           writes=[xb[b]])
                sc.dma("scalar", lambda e, s=s: e.dma_start(out=xs_d[s * 1536:(s + 1) * 1536, :], in_=zr_d[:, :]), writes=[XZ_b[s]])
                SSQ = SM[:, 0:4]
                RSTD = SM[:, 4:8]
                V(lambda e: e.memset(SSQ, 0.0), writes=[smb[0]])
                for b in range(BPS):
                    xn_t, xn_b = XN[b % 2]
                    A(lambda e, b=b, xn_t=xn_t: e.activation(out=xn_t[:, :], in_=XT[:, b, :], func=AF.Square,
                                                             accum_out=SM[:, b:b + 1]),
                      reads=[xb[b]], writes=[xn_b, smb[0]])
                V(lambda e: e.tensor_scalar(SM[:, 8:12], SSQ, 1.0 / D, EPS, ALU.mult, ALU.add), reads=[smb[0]], writes=[smb[1]])
                P(lambda e: e.tensor_tensor(out=RSTD, in0=SM[:, 8:12], in1=NEGH[:, 0:4], op=ALU.pow),
                  reads=[smb[1], NEGH_b], writes=[smb[2]])
                for b in range(BPS):
                    xn_t, xn_b = XN[b % 2]
                    A(lambda e, b=b, xn_t=xn_t: e.activation(out=xn_t[:, :], in_=XT[:, b, :], func=AF.Copy,
                                                             scale=SM[:, 4 + b:5 + b]),
                      reads=[xb[b], smb[2]], writes=[xn_b])
                    for c in range(8):
                        bi = 2 + c // 2
                        T(lambda e, b=b, c=c, bi=bi, xn_t=xn_t: e.transpose(
                            out=bank_bf(bi)[:, (c % 2) * 512 + b * 128:(c % 2) * 512 + (b + 1) * 128],
                            in_=xn_t[:, c * 128:(c + 1) * 128], identity=IDB[:, :]),
                          reads=[xn_b, IDB_b], writes=[banks[bi][1]])
                for c in range(8):
                    bi = 2 + c // 2
                    V(lambda e, c=c, bi=bi: e.tensor_scalar(HT[:, c, :], bank_bf(bi)[:, (c % 2) * 512:(c % 2 + 1) * 512],
                                                            A1[:, c:c + 1], SH1[:, c:c + 1], ALU.mult, ALU.add),
                      reads=[banks[bi][1], A1_b, SH1_b], writes=[HTk[c]])

                def proj_fm(col, evac):
                    pf[0] = (pf[0] + 1) % 4
                    bk, bkb = banks[(0, 1, 6, 7)[pf[0]]]
                    for kc in range(8):
                        T(lambda e, kc=kc, bk=bk: e.matmul(out=bk[:, :], lhsT=WIN[:, kc, col:col + 128], rhs=HT[:, kc, :],
                                                           start=(kc == 0), stop=(kc == 7)),
                          reads=[WIN_b, HTk[kc]], writes=[bkb])
                    evac(bk, bkb)

                for i in range(4):
                    proj_fm(C_Q + i * 128, lambda bk, bkb, i=i: V(
                        lambda e: e.tensor_scalar(QT[:, i, :], bk[:, :], 0.125, None, ALU.mult), reads=[bkb], writes=[QT_b]))
                def k_evac(bk, bkb, i):
                    V(lambda e: e.tensor_copy(out=KT[0:64, i, 0, 128:640], in_=bk[0:64, :]), reads=[bkb], writes=[KT_b])
                    V(lambda e: e.tensor_copy(out=KT[64:128, i, 1, 128:640], in_=bk[64:128, :]), reads=[bkb], writes=[KT_b])

                for i in range(2):
                    proj_fm(C_K + i * 128, lambda bk, bkb, i=i: k_evac(bk, bkb, i))
                for i in range(4):
                    proj_fm(C_GU + i * 128, lambda bk, bkb, i=i: A(
                        lambda e: e.activation(out=UT[:, i, :], in_=bk[:, :], func=GELU_FN), reads=[bkb], writes=[UT_b]))

                def tm_front(b):
                    blk = slice(b * 128, (b + 1) * 128)
                    vn_t, vn_b = VNS[b % 2]
                    bk, bkb = next_proj_bank()
                    for kc in range(8):
                        T(lambda e, kc=kc, bk=bk, blk=blk: e.matmul(out=bk[:, 0:128], lhsT=HT[:, kc, blk],
                                                                    rhs=WIN[:, kc, C_V:C_V + 128], start=(kc == 0), stop=(kc == 7)),
                          reads=[WIN_b, HTk[kc]], writes=[bkb])
                    V(lambda e, b=b, bk=bk: e.tensor_copy(out=VA[:, 1 + b, :, 0:64],
                                                          in_=bk[:, 0:128].rearrange("p (k d) -> p k d", d=64)),
                      reads=[bkb], writes=[VA_b])
                    bk, bkb = next_proj_bank()
                    for kc in range(8):
                        T(lambda e, kc=kc, bk=bk, blk=blk: e.matmul(out=bk[:, :], lhsT=HT[:, kc, blk],
                                                                    rhs=WIN[:, kc, C_GV:C_GV + 512], start=(kc == 0), stop=(kc == 7)),
                          reads=[WIN_b, HTk[kc]], writes=[bkb])
                    A(lambda e, bk=bk: e.activation(out=RB[:, :], in_=bk[:, :], func=GELU_FN), reads=[bkb], writes=[RB_b])
                    V(lambda e: e.bn_stats(out=SM[:, 16:22], in_=RB[:, :]), reads=[RB_b], writes=[smb[3]])
                    V(lambda e: e.bn_aggr(out=SM[:, 22:24], in_=SM[:, 16:22]), reads=[smb[3]], writes=[smb[4]])
                    V(lambda e: e.tensor_scalar(SM[:, 24:25], SM[:, 23:24], EPS, None, ALU.add), reads=[smb[4]], writes=[smb[5]])
                    P(lambda e: e.tensor_tensor(out=SM[:, 25:26], in0=SM[:, 24:25], in1=NEGH[:, 0:1], op=ALU.pow),
                      reads=[smb[5], NEGH_b], writes=[smb[6]])
                    V(lambda e: e.tensor_scalar(RC[:, :], RB[:, :], SM[:, 22:23], SM[:, 25:26], ALU.subtract, ALU.mult),
                      reads=[RB_b, smb[4], smb[6]], writes=[RC_b])
                    P(lambda e: e.tensor_tensor(out=RC[:, :], in0=RC[:, :], in1=LNG[:, :], op=ALU.mult),
                      reads=[RC_b, LNG_b], writes=[RC_b])
                    P(lambda e, vn_t=vn_t: e.tensor_tensor(out=vn_t[:, :], in0=RC[:, :], in1=LNB[:, :], op=ALU.add),
                      reads=[RC_b, LNB_b], writes=[vn_b])

                def tm_spatial(b):
                    blk = slice(b * 128, (b + 1) * 128)
                    vn_t, vn_b = VNS[b % 2]
                    bk, bkb = next_proj_bank()
                    for g in range(4):
                        gs = slice(g * 128, (g + 1) * 128)
                        T(lambda e, bk=bk, gs=gs, g=g, vn_t=vn_t: e.matmul(out=bk[:, gs], lhsT=vn_t[:, gs], rhs=WT[:, g, :], start=True, stop=False),
                          reads=[vn_b, WT_b], writes=[bkb])
                        T(lambda e, bk=bk, gs=gs: e.matmul(out=bk[:, gs], lhsT=ONEF[0:1, 0:128], rhs=BS[0:1, gs], start=False, stop=True),
                          reads=[ONEF_b, BS_b], writes=[bkb])
                    V(lambda e, bk=bk, blk=blk: e.tensor_tensor(out=YBT[:, :, blk], in0=bk[:, :].rearrange("p (g t) -> p g t", t=128),
                                                                in1=UT[:, :, blk], op=ALU.mult),
                      reads=[bkb, UT_b], writes=[YBT_b])

                def at_info(b):
                    n = s * BPS + b
                    return n, slice(b * 128, (b + 1) * 128), ([1] if n == 0 else [0, 1])

                def at_S(b):
                    n, blk, halves = at_info(b)
                    for hf in halves:
                        kcols = slice((b + hf) * 128, (b + hf + 1) * 128)
                        for h in range(8):
                            par, kvh = h % 2, h // 4
                            bk, bkb = banks[2 + hf * 2 + h // 4]
                            oc = slice((h % 4) * 128, (h % 4 + 1) * 128)
                            T(lambda e, bk=bk, oc=oc, hf=hf, h=h: e.matmul(out=bk[:, oc], lhsT=IDB[:, :], rhs=BMH[:, hf, h, :], start=True, stop=False),
                              reads=[IDB_b, BMH_b], writes=[bkb])
                            T(lambda e, bk=bk, oc=oc, hf=hf, h=h: e.matmul(out=bk[:, oc], lhsT=IDB[:, :], rhs=BML[:, hf, h, :], start=False, stop=False),
                              reads=[IDB_b, BML_b], writes=[bkb])
                            T(lambda e, bk=bk, oc=oc, h=h, kvh=kvh, par=par, kcols=kcols, blk=blk: e.matmul(
                                out=bk[:, oc], lhsT=KT[:, kvh, par, kcols], rhs=QT[:, h // 2, blk], start=False, stop=True),
                              reads=[KT_b, QT_b], writes=[bkb])

                def at_softmax(b):
                    n, blk, halves = at_info(b)
                    for hf in halves:
                        for g2 in range(2):
                            bk, bkb = banks[2 + hf * 2 + g2]
                            pcol = (hf * 2 + g2) * 512
                            A(lambda e, bk=bk, pcol=pcol: e.activation(out=PT_bf[:, pcol:pcol + 512], in_=bk[:, :], func=AF.Exp),
                              reads=[bkb], writes=[RD_b])

                def at_PV(b):
                    n, blk, halves = at_info(b)
                    for h in range(8):
                        kvh = h // 4
                        bk, bkb = banks[6 + h // 4]
                        for hf in halves:
                            pcol = (hf * 2 + h // 4) * 512 + (h % 4) * 128
                            T(lambda e, bk=bk, h=h, kvh=kvh, hf=hf, pcol=pcol, b=b, halves=halves: e.matmul(
                                out=bk[:, (h % 4) * 65:(h % 4 + 1) * 65], lhsT=PT_bf[:, pcol:pcol + 128],
                                rhs=VA[:, b + hf, kvh, :], start=(hf == halves[0]), stop=(hf == 1)),
                              reads=[RD_b, VA_b], writes=[bkb])

                def at_norm(b):
                    n, blk, halves = at_info(b)
                    for grp in range(2):
                        bk, bkb = banks[6 + grp]
                        obv = bk[:, 0:260].rearrange("p (h d) -> p h d", d=65)
                        V(lambda e, obv=obv, grp=grp: e.tensor_tensor(out=SM[:, 32 + grp * 4:36 + grp * 4], in0=obv[:, :, 64],
                                                                      in1=ES_[:, grp * 4:(grp + 1) * 4], op=ALU.add),
                          reads=[bkb, ES_b], writes=[smb[7]])
                        V(lambda e, grp=grp: e.reciprocal(out=SM[:, 40 + grp * 4:44 + grp * 4], in_=SM[:, 32 + grp * 4:36 + grp * 4]),
                          reads=[smb[7]], writes=[smb[7]])
                        V(lambda e, obv=obv, grp=grp: e.tensor_tensor(
                            out=YA[:, grp * 256:(grp + 1) * 256].rearrange("p (h d) -> p h d", d=64), in0=obv[:, :, 0:64],
                            in1=bc(SM[:, 40 + grp * 4:44 + grp * 4].unsqueeze(2), [128, 4, 64]), op=ALU.mult),
                          reads=[bkb, smb[7]], writes=[YA_b])
                    bk, bkb = next_proj_bank()
                    bi = pj[0]
                    for c in range(4):
                        T(lambda e, c=c, bi=bi: e.transpose(out=bank_bf(bi)[:, c * 128:(c + 1) * 128], in_=YA[:, c * 128:(c + 1) * 128],
                                                            identity=IDB[:, :]),
                          reads=[YA_b, IDB_b], writes=[bkb])
                    V(lambda e, bi=bi, blk=blk: e.tensor_copy(out=YAT[:, :, blk],
                                                              in_=bank_bf(bi)[:, 0:512].rearrange("p (c t) -> p c t", t=128)),
                      reads=[bkb], writes=[YAT_b])

                tm_front(0)
                at_S(0)
                at_softmax(0)
                for b in range(BPS):
                    if b + 1 < BPS:
                        tm_front(b + 1)
                        at_S(b + 1)
                    tm_spatial(b)
                    at_PV(b)
                    if b + 1 < BPS:
                        at_softmax(b + 1)
                    at_norm(b)

                for cc in range(8):
                    cs_ = slice(cc * 128, (cc + 1) * 128)
                    bGA, bGB, bPA, bPB = (2, 3, 4, 5) if cc % 2 == 0 else (6, 7, 0, 1)
                    for kc in range(8):
                        T(lambda e, kc=kc, cc=cc, bGA=bGA: e.matmul(out=banks[bGA][0][:, :], lhsT=WIN[:, kc, C_GA + cc * 128:C_GA + (cc + 1) * 128],
                                                                    rhs=HT[:, kc, :], start=(kc == 0), stop=(kc == 7)),
                          reads=[WIN_b, HTk[kc]], writes=[banks[bGA][1]])
                    for kc in range(8):
                        T(lambda e, kc=kc, cc=cc, bGB=bGB: e.matmul(out=banks[bGB][0][:, :], lhsT=WIN[:, kc, C_GB + cc * 128:C_GB + (cc + 1) * 128],
                                                                    rhs=HT[:, kc, :], start=(kc == 0), stop=(kc == 7)),
                          reads=[WIN_b, HTk[kc]], writes=[banks[bGB][1]])
                    for kc in range(4):
                        T(lambda e, kc=kc, cs_=cs_, bPA=bPA: e.matmul(out=banks[bPA][0][:, :], lhsT=PAW[:, kc, cs_], rhs=YAT[:, kc, :],
                                                                      start=(kc == 0), stop=(kc == 3)),
                          reads=[PAW_b, YAT_b], writes=[banks[bPA][1]])
                    for kc in range(4):
                        T(lambda e, kc=kc, cs_=cs_, bPB=bPB: e.matmul(out=banks[bPB][0][:, :], lhsT=PBW[:, kc, cs_], rhs=YBT[:, kc, :],
                                                                      start=(kc == 0), stop=(kc == 3)),
                          reads=[PBW_b, YBT_b], writes=[banks[bPB][1]])
                    sa_t = RE[:, (cc % 2) * 512:(cc % 2) * 512 + 512]
                    sa_b = re_half[cc % 2]
                    A(lambda e, bGA=bGA, sa_t=sa_t: e.activation(out=sa_t, in_=banks[bGA][0][:, :], func=AF.Tanh, scale=0.5),
                      reads=[banks[bGA][1]], writes=[sa_b, RE_b])
                    V(lambda e, bPA=bPA, sa_t=sa_t: e.scalar_tensor_tensor(out=RA[:, :], in0=sa_t, scalar=1.0, in1=banks[bPA][0][:, :], op0=ALU.add, op1=ALU.mult),
                      reads=[sa_b, banks[bPA][1]], writes=[RA_b])
                    A(lambda e, bGB=bGB, sa_t=sa_t: e.activation(out=sa_t, in_=banks[bGB][0][:, :], func=AF.Tanh, scale=0.5),
                      reads=[banks[bGB][1]], writes=[sa_b, RE_b])
                    V(lambda e, bPB=bPB, sa_t=sa_t: e.scalar_tensor_tensor(out=RB[:, :], in0=sa_t, scalar=1.0, in1=banks[bPB][0][:, :], op0=ALU.add, op1=ALU.mult),
                      reads=[sa_b, banks[bPB][1]], writes=[RB_b])
                    P(lambda e, cc=cc: e.tensor_tensor(out=MT[:, cc, :], in0=RA[:, :], in1=RB[:, :], op=ALU.add),
                      reads=[RA_b, RB_b], writes=[MT_b])

                RD2 = QT[:, :, :].rearrange("p a t -> p (a t)").bitcast(F32)
                RE2 = XN[0][0]
                h2bufs = [(RD, RD_b, RE, RE_b, [RE_b, re_half[0], re_half[1]]), (RD2, QT_b, RE2, XN[0][1], [XN[0][1]])]

                def wo_WO(b):
                    blk = slice(b * 128, (b + 1) * 128)
                    for hf in range(2):
                        hs = slice(hf * 512, (hf + 1) * 512)
                        bk, bkb = next_proj_bank()
                        for kc in range(8):
                            T(lambda e, kc=kc, bk=bk, blk=blk, hs=hs: e.matmul(out=bk[:, :], lhsT=MT[:, kc, blk], rhs=WOW[:, kc, hs],
                                                                               start=(kc == 0), stop=(kc == 7)),
                              reads=[MT_b, WOW_b], writes=[bkb])
                        V(lambda e, bk=bk, hs=hs: e.tensor_tensor(out=RC[:, :], in0=bk[:, :], in1=G1H[:, hs], op=ALU.mult),
                          reads=[bkb, G1H_b], writes=[RC_b])
                        V(lambda e, b=b, hs=hs: e.tensor_tensor(out=XT[:, b, hs], in0=XT[:, b, hs], in1=RC[:, :], op=ALU.add),
                          reads=[RC_b, xb[b]], writes=[xb[b]])

                def wo_N2(b):
                    n = s * BPS + b
                    rd, rdb, re_, reb, rew = h2bufs[b % 2]
                    sc.dma("sync", lambda e, b=b, n=n: e.dma_start(out=x1_d[n * 128:(n + 1) * 128, :], in_=XT[:, b, :]), reads=[xb[b]], writes=[X1D_b[n]])
                    if dbg:
                        sc.dma("sync", lambda e, b=b, n=n: e.dma_start(out=dbg_d["x1"][n * 128:(n + 1) * 128, :], in_=XT[:, b, :]),
                               reads=[xb[b]])
                    V(lambda e: e.memset(SM[:, 48:49], 0.0), writes=[smb[0]])
                    A(lambda e, b=b, rd=rd: e.activation(out=rd[:, :], in_=XT[:, b, :], func=AF.Square, accum_out=SM[:, 48:49]),
                      reads=[xb[b]], writes=[rdb, smb[0]])
                    V(lambda e: e.tensor_scalar(SM[:, 49:50], SM[:, 48:49], 1.0 / D, EPS, ALU.mult, ALU.add), reads=[smb[0]], writes=[smb[1]])
                    P(lambda e: e.tensor_tensor(out=SM[:, 50:51], in0=SM[:, 49:50], in1=NEGH[:, 0:1], op=ALU.pow),
                      reads=[smb[1], NEGH_b], writes=[smb[2]])
                    V(lambda e, b=b, rd=rd: e.scalar_tensor_tensor(out=rd[:, :], in0=XT[:, b, :], scalar=SM[:, 50:51], in1=A2[:, :],
                                                                   op0=ALU.mult, op1=ALU.mult),
                      reads=[xb[b], smb[2], A2_b], writes=[rdb])
                    V(lambda e, rd=rd: e.tensor_tensor(out=rd[:, :], in0=rd[:, :], in1=SH2[:, :], op=ALU.add), reads=[rdb, SH2_b], writes=[rdb])
                    A(lambda e, rd=rd, re_=re_: e.activation(out=re_[:, :], in_=rd[:, :], func=AF.Copy), reads=[rdb], writes=rew)
                    sc.dma("sync", lambda e, n=n, re_=re_: e.dma_start(out=h2_d[n * 128:(n + 1) * 128, :], in_=re_[:, :]), reads=[reb], writes=[H2D_b[n]])

                def wo_TR(b):
                    n = s * BPS + b
                    rd, rdb, re_, reb, rew = h2bufs[b % 2]
                    for c in range(8):
                        bi = 6 + c // 4
                        T(lambda e, c=c, bi=bi, rd=rd: e.transpose(out=banks[bi][0][:, (c % 4) * 128:(c % 4 + 1) * 128],
                                                                   in_=rd[:, c * 128:(c + 1) * 128], identity=IDF[:, :]),
                          reads=[rdb, IDF_b], writes=[banks[bi][1]])
                    A(lambda e: e.activation(out=H2T[:, 0:4, :], in_=banks[6][0][:, :].rearrange("p (c t) -> p c t", t=128), func=AF.Copy),
                      reads=[banks[6][1]], writes=[H2T_b])
                    V(lambda e: e.tensor_copy(out=H2T[:, 4:8, :], in_=banks[7][0][:, :].rearrange("p (c t) -> p c t", t=128)),
                      reads=[banks[7][1]], writes=[H2Tb_b])
                    bk, bkb = next_proj_bank()
                    for c in range(8):
                        T(lambda e, c=c, bk=bk: e.matmul(out=bk[0:36, 0:128], lhsT=WR[:, c, :], rhs=H2T[:, c, :], start=(c == 0), stop=False),
                          reads=[H2T_b if c < 4 else H2Tb_b, WR_b], writes=[bkb])
                    T(lambda e, bk=bk: e.matmul(out=bk[0:36, 0:128], lhsT=BR[0:1, 0:36], rhs=ONEF[0:1, 0:128], start=False, stop=True),
                      reads=[ONEF_b, BR_b], writes=[bkb])
                    A(lambda e, bk=bk: e.activation(out=LGT[0:36, :], in_=bk[0:36, 0:128], func=AF.Copy), reads=[bkb], writes=[LGT_b])

                def wo_LG(b):
                    n = s * BPS + b
                    bk, bkb = next_proj_bank()
                    T(lambda e, bk=bk: e.transpose(out=bk[:, 0:36], in_=LGT[0:36, :], identity=IDF[0:36, 0:36]),
                      reads=[LGT_b, IDF_b], writes=[bkb])
                    V(lambda e, bk=bk, n=n: e.tensor_copy(out=LG[:, n, :], in_=bk[:, 0:36]), reads=[bkb], writes=[LG_b])

                wo_WO(0)
                wo_N2(0)
                for b in range(BPS):
                    if b + 1 < BPS:
                        wo_WO(b + 1)
                    if b > 0:
                        wo_LG(b - 1)
                    wo_TR(b)
                    if b + 1 < BPS:
                        wo_N2(b + 1)
                wo_LG(BPS - 1)
                P(lambda e: e.tensor_copy(out=KT[:, :, :, 0:128], in_=KT[:, :, :, 512:640]), reads=[KT_b], writes=[KT_b])
                P(lambda e: e.tensor_copy(out=VA[:, 0, :, :], in_=VA[:, 4, :, :]), reads=[VA_b], writes=[VA_b])

            if dbg:
                sc.dma("sync", lambda e: e.dma_start(out=dbg_d["lg"][:, :], in_=LG[:, :, :].rearrange("p t k -> p (t k)")), reads=[LG_b])
            sc.drain("sync")
            sc.emit()

        if stage in ("A", "A1"):
            return nc, dbg_d

        pr_es = ExitStack()
        with pr_es:
            sr = lambda name, shape, dt: sb(name, shape, dt, stack=pr_es)
            GM, GM_b = sr("gm", [128, 32], F32)
            OHG, OHG_b = sr("ohg", [128, 32, 4], F32)
            T4, T4_b = sr("t4", [128, 32, 4], F32)
            PEN, PEN_b = sr("pen", [128, 32, 4], F32)
            PG, PG_b = sr("pg", [128, 32], F32)
            LEM, LEM_b = sr("lem", [128, 32, 32], F32)
            LEM2, LEM2_b = sr("lem2", [128, 32, 32], F32)
            OH1, OH1_b = sr("oh1", [128, 32, 32], F32)
            OH2, OH2_b = sr("oh2", [128, 32, 32], F32)
            M1, M1_b = sr("m1", [128, 32], F32)
            M2, M2_b = sr("m2", [128, 32], F32)
            DL, DL_b = sr("dl", [128, 32], F32)
            MM, MM_b = sr("mm", [128, 32, 32], F32)
            US, US_b = sr("us", [128, 128], F32)
            TOT, TOT_b = sr("tot", [128, 32, 32], F32)
            CA, CA_b = sr("ca", [128, 32, 32], F32)
            CB, CB_b = sr("cb", [128, 32, 32], F32)
            RANK, RANK_b = sr("rank", [128, 32, 32], F32)
            THRI, THRI_b = sr("thri", [128, 32], I32)
            THRF, THRF_b = sr("thrf", [128, 32], F32)
            NBK, NBK_b = sr("nbk", [128, 32], F32)
            C1, C1_b = sr("c1", [128, 32], F32)
            C2, C2_b = sr("c2", [128, 32], F32)
            PST, PST_b = sr("pst", [128, 32], F32)
            JVI, JVI_b = sr("jvi", [128, NB], I32)
            JVF, JVF_b = sr("jvf", [128, NB], F32)
            CMP2, CMP2_b = sr("cmp2", [128, NB, 32], F32)
            EJ, EJ_b = sr("ej", [128, NB], F32)
            DF, DF_b = sr("df", [128, 2, 32], F32)
            TOKID, TOKID_b = sr("tokid", [128, 32, 2], I32)
            ZERO, ZERO_b = sr("zero", [128, 256], I32)
            SLJ, SLJ_b = sr("slj", [128, 256], I32)
            SLJF, SLJF_b = sr("sljf", [128, 128], F32)
            PIDI, PIDI_b = sr("pidi", [128, 1], I32)
            PIDF, PIDF_b = sr("pidf", [128, 1], F32)
            IWF, IWF_b = sr("iwf", [128, NB], F32)

            lgv = LG[:, :, 0:4]
            lev = LG[:, :, 4:36]
            V(lambda e: e.tensor_reduce(out=GM[:, :], in_=lgv, axis=AX.X, op=ALU.max), reads=[LG_b], writes=[GM_b])
            V(lambda e: e.tensor_tensor(out=OHG[:, :, :], in0=lgv, in1=bc(GM[:, :].unsqueeze(2), [128, 32, 4]), op=ALU.is_equal),
              reads=[LG_b, GM_b], writes=[OHG_b])
            V(lambda e: e.tensor_tensor(out=T4[:, :, :], in0=lgv, in1=bc(GM[:, :].unsqueeze(2), [128, 32, 4]), op=ALU.subtract),
              reads=[LG_b, GM_b], writes=[T4_b])
            A(lambda e: e.activation(out=T4[:, :, :], in_=T4[:, :, :], func=AF.Exp), reads=[T4_b], writes=[T4_b])
            V(lambda e: e.tensor_reduce(out=PG[:, :], in_=T4[:, :, :], axis=AX.X, op=ALU.add), reads=[T4_b], writes=[PG_b])
            V(lambda e: e.reciprocal(out=PG[:, :], in_=PG[:, :]), reads=[PG_b], writes=[PG_b])
            V(lambda e: e.tensor_scalar(PEN[:, :, :], OHG[:, :, :], 1e30, -1e30, ALU.mult, ALU.add), reads=[OHG_b], writes=[PEN_b])
            V(lambda e: e.tensor_tensor(out=LEM[:, :, :].rearrange("p t (g i) -> p t g i", i=8),
                                        in0=lev.rearrange("p t (g i) -> p t g i", i=8),
                                        in1=bc(PEN[:, :, :].unsqueeze(3), [128, 32, 4, 8]), op=ALU.add),
              reads=[LG_b, PEN_b], writes=[LEM_b])
            V(lambda e: e.tensor_reduce(out=M1[:, :], in_=LEM[:, :, :], axis=AX.X, op=ALU.max), reads=[LEM_b], writes=[M1_b])
            V(lambda e: e.tensor_tensor(out=OH1[:, :, :], in0=LEM[:, :, :], in1=bc(M1[:, :].unsqueeze(2), [128, 32, 32]), op=ALU.is_equal),
              reads=[LEM_b, M1_b], writes=[OH1_b])
            V(lambda e: e.scalar_tensor_tensor(out=LEM2[:, :, :], in0=OH1[:, :, :], scalar=-1e30, in1=LEM[:, :, :],
                                               op0=ALU.mult, op1=ALU.add),
              reads=[OH1_b, LEM_b], writes=[LEM2_b])
            V(lambda e: e.tensor_reduce(out=M2[:, :], in_=LEM2[:, :, :], axis=AX.X, op=ALU.max), reads=[LEM2_b], writes=[M2_b])
            V(lambda e: e.tensor_tensor(out=OH2[:, :, :], in0=LEM2[:, :, :], in1=bc(M2[:, :].unsqueeze(2), [128, 32, 32]), op=ALU.is_equal),
              reads=[LEM2_b, M2_b], writes=[OH2_b])
            V(lambda e: e.tensor_tensor(out=DL[:, :], in0=M2[:, :], in1=M1[:, :], op=ALU.subtract), reads=[M1_b, M2_b], writes=[DL_b])
            A(lambda e: e.activation(out=DL[:, :], in_=DL[:, :], func=AF.Exp), reads=[DL_b], writes=[DL_b])
            V(lambda e: e.tensor_scalar(DL[:, :], DL[:, :], 1.0, None, ALU.add), reads=[DL_b], writes=[DL_b])
            V(lambda e: e.reciprocal(out=DL[:, :], in_=DL[:, :]), reads=[DL_b], writes=[DL_b])
            V(lambda e: e.tensor_tensor(out=W12[:, 0, :], in0=PG[:, :], in1=DL[:, :], op=ALU.mult), reads=[PG_b, DL_b], writes=[W12_b])
            V(lambda e: e.tensor_tensor(out=W12[:, 1, :], in0=PG[:, :], in1=W12[:, 0, :], op=ALU.subtract), reads=[PG_b, W12_b], writes=[W12_b])
            V(lambda e: e.tensor_tensor(out=MM[:, :, :], in0=OH1[:, :, :], in1=OH2[:, :, :], op=ALU.add), reads=[OH1_b, OH2_b], writes=[MM_b])
            P(lambda e: e.affine_select(out=US[:, :], in_=ONEF[:, :], pattern=[[1, 128]], compare_op=ALU.is_gt, fill=0.0,
                                        base=0, channel_multiplier=-1), reads=[ONEF_b], writes=[US_b])
            MMf = MM[:, :, :].rearrange("p t e -> p (t e)")
            for hf in range(2):
                T(lambda e, hf=hf: e.matmul(out=banks[hf][0][:, :], lhsT=US[:, :], rhs=MMf[:, hf * 512:(hf + 1) * 512], start=True, stop=True),
                  reads=[US_b, MM_b], writes=[banks[hf][1]])
                T(lambda e, hf=hf: e.matmul(out=banks[2 + hf][0][:, :], lhsT=ONEF[:, :], rhs=MMf[:, hf * 512:(hf + 1) * 512], start=True, stop=True),
                  reads=[ONEF_b, MM_b], writes=[banks[2 + hf][1]])
            TOTf = TOT[:, :, :].rearrange("p t e -> p (t e)")
            for hf in range(2):
                V(lambda e, hf=hf: e.tensor_copy(out=TOTf[:, hf * 512:(hf + 1) * 512], in_=banks[2 + hf][0][:, :]),
                  reads=[banks[2 + hf][1]], writes=[TOT_b])
            chain = [(TOT, TOT_b), (CA, CA_b), (CB, CB_b), (CA, CA_b), (CB, CB_b), (CA, CA_b)]
            for i, sh_ in enumerate([1, 2, 4, 8, 16]):
                (src, srcb), (dst, dstb) = chain[i], chain[i + 1]
                V(lambda e, src=src, dst=dst, sh_=sh_: e.tensor_copy(out=dst[:, 0:sh_, :], in_=src[:, 0:sh_, :]), reads=[srcb], writes=[dstb])
                V(lambda e, src=src, dst=dst, sh_=sh_: e.tensor_tensor(out=dst[:, sh_:32, :], in0=src[:, sh_:32, :], in1=src[:, 0:32 - sh_, :], op=ALU.add),
                  reads=[srcb], writes=[dstb])
            V(lambda e: e.tensor_tensor(out=CB[:, :, :], in0=CA[:, :, :], in1=TOT[:, :, :], op=ALU.subtract), reads=[CA_b, TOT_b], writes=[CB_b])
            CBf = CB[:, :, :].rearrange("p t e -> p (t e)")
            RANKf = RANK[:, :, :].rearrange("p t e -> p (t e)")
            for hf in range(2):
                V(lambda e, hf=hf: e.tensor_tensor(out=RANKf[:, hf * 512:(hf + 1) * 512], in0=banks[hf][0][:, :],
                                                   in1=CBf[:, hf * 512:(hf + 1) * 512], op=ALU.add),
                  reads=[banks[hf][1], CB_b], writes=[RANK_b])
            CNT = CA[:, 31, :]
            P(lambda e: e.iota(THRI[:, :], pattern=[[128, 32]], base=1, channel_multiplier=0), writes=[THRI_b])
            V(lambda e: e.tensor_copy(out=THRF[:, :], in_=THRI[:, :]), reads=[THRI_b], writes=[THRF_b])
            V(lambda e: e.tensor_tensor(out=LEM[:, :, :], in0=bc(CNT.unsqueeze(2), [128, 32, 32]),
                                        in1=bc(THRF[:, :].unsqueeze(1), [128, 32, 32]), op=ALU.is_ge),
              reads=[CA_b, THRF_b], writes=[LEM_b])
            V(lambda e: e.tensor_reduce(out=NBK[:, :], in_=LEM[:, :, :], axis=AX.X, op=ALU.add), reads=[LEM_b], writes=[NBK_b])
            chain2 = [(NBK, NBK_b), (C1, C1_b), (C2, C2_b), (C1, C1_b), (C2, C2_b), (C1, C1_b)]
            for i, sh_ in enumerate([1, 2, 4, 8, 16]):
                (src, srcb), (dst, dstb) = chain2[i], chain2[i + 1]
                V(lambda e, src=src, dst=dst, sh_=sh_: e.tensor_copy(out=dst[:, 0:sh_], in_=src[:, 0:sh_]), reads=[srcb], writes=[dstb])
                V(lambda e, src=src, dst=dst, sh_=sh_: e.tensor_tensor(out=dst[:, sh_:32], in0=src[:, sh_:32], in1=src[:, 0:32 - sh_], op=ALU.add),
                  reads=[srcb], writes=[dstb])
            V(lambda e: e.tensor_tensor(out=PST[:, :], in0=C1[:, :], in1=NBK[:, :], op=ALU.subtract), reads=[C1_b, NBK_b], writes=[PST_b])
            P(lambda e: e.iota(JVI[:, :], pattern=[[1, NB]], base=0, channel_multiplier=0), writes=[JVI_b])
            V(lambda e: e.tensor_copy(out=JVF[:, :], in_=JVI[:, :]), reads=[JVI_b], writes=[JVF_b])
            V(lambda e: e.tensor_tensor(out=CMP2[:, :, :], in0=bc(C1[:, :].unsqueeze(1), [128, NB, 32]),
                                        in1=bc(JVF[:, :].unsqueeze(2), [128, NB, 32]), op=ALU.is_le),
              reads=[C1_b, JVF_b], writes=[CMP2_b])
            V(lambda e: e.tensor_reduce(out=EJ[:, :], in_=CMP2[:, :, :], axis=AX.X, op=ALU.add), reads=[CMP2_b], writes=[EJ_b])
            V(lambda e: e.tensor_scalar(EJ[:, :], EJ[:, :], 31.0, None, ALU.min), reads=[EJ_b], writes=[EJ_b])
            V(lambda e: e.scalar_tensor_tensor(out=LEM2[:, :, :], in0=bc(PST[:, :].unsqueeze(1), [128, 32, 32]), scalar=128.0,
                                               in1=RANK[:, :, :], op0=ALU.mult, op1=ALU.add),
              reads=[PST_b, RANK_b], writes=[LEM2_b])
            for k, (OH, OHb) in enumerate([(OH1, OH1_b), (OH2, OH2_b)]):
                V(lambda e, OH=OH: e.tensor_tensor(out=LEM[:, :, :], in0=OH[:, :, :], in1=LEM2[:, :, :], op=ALU.mult),
                  reads=[OHb, LEM2_b], writes=[LEM_b])
                V(lambda e, k=k: e.tensor_reduce(out=DF[:, k, :], in_=LEM[:, :, :], axis=AX.X, op=ALU.add), reads=[LEM_b], writes=[DF_b])
            V(lambda e: e.tensor_copy(out=DEST[:, :, :], in_=DF[:, :, :]), reads=[DF_b], writes=[DEST_b])
            HX = [sr(f"hx{i}", [128, 1024], BF16) for i in range(4)]
            for t in range(NBLK):
                hx, hxb = HX[t % 4]
                sc.dma("sync", lambda e, t=t, hx=hx: e.dma_start(out=hx[:, :], in_=h2_d[t * 128:(t + 1) * 128, :]), reads=[H2D_b[t]], writes=[hxb])
                for k in range(2):
                    sc.dma("gpsimd", lambda e, k=k, t=t, hx=hx: e.indirect_dma_start(
                        out=xs_d[:, :], out_offset=bass.IndirectOffsetOnAxis(ap=DEST[:, k, t:t + 1], axis=0),
                        in_=hx[:, :], in_offset=None, bounds_check=sc.reg(e, R - 1), oob_is_err=False),
                        reads=[DEST_b, hxb] + XZ_b, writes=[XS_b[2 * t + k]])
            P(lambda e: e.iota(PIDI[:, :], pattern=[[0, 1]], base=0, channel_multiplier=1), writes=[PIDI_b])
            V(lambda e: e.tensor_copy(out=PIDF[:, :], in_=PIDI[:, :]), reads=[PIDI_b], writes=[PIDF_b])
            V(lambda e: e.tensor_scalar(IWF[:, :], EJ[:, :], 128.0, PIDF[:, 0:1], ALU.mult, ALU.add), reads=[EJ_b, PIDF_b], writes=[IWF_b])
            EQ, EQ_b = sr("eq", [128, NB], F32)
            V(lambda e: e.memset(EQ[:, :], 0.0), writes=[EQ_b])
            V(lambda e: e.tensor_tensor(out=EQ[:, 1:NB], in0=EJ[:, 1:NB], in1=EJ[:, 0:NB - 1], op=ALU.is_equal), reads=[EJ_b], writes=[EQ_b])
            V(lambda e: e.memset(EQ[:, :].rearrange("p (l k) -> p l k", k=LB)[:, :, 0:1], 0.0), writes=[EQ_b])
            V(lambda e: e.scalar_tensor_tensor(out=IWF[:, :], in0=EQ[:, :], scalar=1.0e6, in1=IWF[:, :], op0=ALU.mult, op1=ALU.add),
              reads=[EQ_b, IWF_b], writes=[IWF_b])
            V(lambda e: e.tensor_copy(out=IDXW[:, :], in_=IWF[:, :]), reads=[IWF_b], writes=[IDXW_b])
            if dbg:
                sc.dma("sync", lambda e: e.dma_start(out=dbg_d["rt"][:, 0:NB], in_=IWF[:, :]), reads=[IWF_b])
                sc.dma("sync", lambda e: e.dma_start(out=dbg_d["rt"][:, 256:320], in_=DF[:, :, :].rearrange("p k t -> p (k t)")), reads=[DF_b])
                sc.dma("sync", lambda e: e.dma_start(out=dbg_d["rt"][:, 320:384], in_=W12[:, :, :].rearrange("p k t -> p (k t)")), reads=[W12_b])
                sc.dma("sync", lambda e: e.dma_start(out=dbg_d["rt"][:, 384:416], in_=CA[:, 31, :]), reads=[CA_b])
            sc.drain("sync")
            sc.emit()

        if stage == "R":
            return nc, dbg_d

        pb_es = ExitStack()
        with pb_es:
            sB = lambda name, shape, dt: sb(name, shape, dt, stack=pb_es)
            WG = [sB(f"wg{i}", [128, 8, 512], BF16) for i in range(NLANE)]
            WU = [sB(f"wu{i}", [128, 8, 512], BF16) for i in range(NLANE)]
            WD = [sB(f"wd{i}", [128, 4, 1024], BF16) for i in range(NLANE)]
            NXG = 8
            XG = [sB(f"xg{i}", [128, 1024], BF16) for i in range(NXG)]
            XTt = [sB(f"xtt{i}", [128, 8, 128], BF16) for i in range(2)]
            SG, SG_b = sB("sg", [128, 512], F32)
            ACTT = [sB(f"actt{i}", [128, 4, 128], BF16) for i in range(2)]
            ACTM = [sB(f"actm{i}", [128, 512], BF16) for i in range(2)]
            YO = [sB(f"yo{i}", [128, 1024], BF16) for i in range(2)]
            order = [l * LB + k for k in range(LB) for l in range(NLANE)]
            nsteps = len(order)

            def loads(i):
                j = order[i]
                lane = j // LB
                xg, xgb = XG[i % NXG]
                sc.dma("sync", lambda e, j=j, xg=xg: e.dma_start(out=xg[:, :], in_=xs_d[j * 128:(j + 1) * 128, :]),
                       reads=XS_b, writes=[xgb])
                for (Wt, wdl) in ((WG, wg_d), (WU, wu_d), (WD, wd_d)):
                    wt_, wtb = Wt[lane]
                    wflat = wt_[:, :, :].rearrange("p a b -> p (a b)")
                    for hh in range(2):
                        sc.dma("gpsimd", lambda e, j=j, hh=hh, wflat=wflat, wdl=wdl: e.indirect_dma_start(
                            out=wflat[:, hh * 2048:(hh + 1) * 2048], out_offset=None, in_=wdl[hh][:, :],
                            in_offset=bass.IndirectOffsetOnAxis(ap=IDXW[:, j:j + 1], axis=0), bounds_check=sc.reg(e, NE * 128 - 1), oob_is_err=False),
                            reads=[IDXW_b], writes=[wtb], join=(hh > 0))

            def stepT(i):
                xg, xgb = XG[i % NXG]
                tb = i % 2
                xt_, xtb = XTt[i % 2]
                for c in range(8):
                    T(lambda e, c=c, tb=tb, xg=xg: e.transpose(out=bank_bf(tb)[:, c * 128:(c + 1) * 128], in_=xg[:, c:1024:8], identity=IDB[:, :]),
                      reads=[xgb, IDB_b], writes=[banks[tb][1]])
                V(lambda e, tb=tb, xt_=xt_: e.tensor_copy(out=xt_[:, :, :].rearrange("p c t -> p (c t)"), in_=bank_bf(tb)[:, 0:1024]),
                  reads=[banks[tb][1]], writes=[xtb])

            def stepGU(i):
                j = order[i]
                lane = j // LB
                xt_, xtb = XTt[i % 2]
                gb_, ub_ = (2, 3) if i % 2 == 0 else (6, 7)
                for (Wt, bi) in ((WG, gb_), (WU, ub_)):
                    wt_, wtb = Wt[lane]
                    for c in range(8):
                        T(lambda e, wt_=wt_, bi=bi, c=c, xt_=xt_: e.matmul(
                            out=banks[bi][0][:, :], lhsT=xt_[:, c, :], rhs=wt_[:, c, :], start=(c == 0), stop=(c == 7)),
                          reads=[wtb, xtb], writes=[banks[bi][1]])
                at_, atb = ACTT[i % 2]
                am_, amb = ACTM[i % 2]
                A(lambda e, gb_=gb_: e.activation(out=SG[:, :], in_=banks[gb_][0][:, :], func=AF.Silu), reads=[banks[gb_][1]], writes=[SG_b])
                V(lambda e, ub_=ub_, am_=am_: e.tensor_tensor(out=am_[:, :], in0=SG[:, :], in1=banks[ub_][0][:, :], op=ALU.mult),
                  reads=[SG_b, banks[ub_][1]], writes=[amb])
                for jj in range(4):
                    T(lambda e, jj=jj, gb_=gb_, am_=am_: e.transpose(out=bank_bf(gb_)[:, jj * 128:(jj + 1) * 128], in_=am_[:, jj:512:4], identity=IDB[:, :]),
                      reads=[amb, IDB_b], writes=[banks[gb_][1]])
                A(lambda e, gb_=gb_, at_=at_: e.activation(out=at_[:, :, :].rearrange("p a t -> p (a t)"), in_=bank_bf(gb_)[:, 0:512], func=AF.Copy),
                  reads=[banks[gb_][1]], writes=[atb])

            def stepD(i):
                j = order[i]
                lane = j // LB
                at_, atb = ACTT[i % 2]
                wd_, wdb = WD[lane]
                yo_, yob = YO[i % 2]
                for hf in range(2):
                    for jj in range(4):
                        T(lambda e, hf=hf, jj=jj, at_=at_, wd_=wd_: e.matmul(out=banks[4 + hf][0][:, :], lhsT=at_[:, jj, :],
                                                                            rhs=wd_[:, jj, hf * 512:(hf + 1) * 512], start=(jj == 0), stop=(jj == 3)),
                          reads=[atb, wdb], writes=[banks[4 + hf][1]])
                A(lambda e, yo_=yo_: e.activation(out=yo_[:, 0:512], in_=banks[4][0][:, :], func=AF.Copy), reads=[banks[4][1]], writes=[yob])
                V(lambda e, yo_=yo_: e.tensor_copy(out=yo_[:, 512:1024], in_=banks[5][0][:, :]), reads=[banks[5][1]], writes=[yob])
                sc.dma("sync", lambda e, j=j, yo_=yo_: e.dma_start(out=ys_d[j * 128:(j + 1) * 128, :], in_=yo_[:, :]), reads=[yob], writes=[YS_b[j]])

            LOOK = NLANE - 1
            for i0 in range(LOOK):
                loads(i0)
            stepT(0)
            stepGU(0)
            stepT(1)
            for i in range(nsteps):
                if i + LOOK < nsteps:
                    loads(i + LOOK)
                if i + 1 < nsteps:
                    stepGU(i + 1)
                if i + 2 < nsteps:
                    stepT(i + 2)
                stepD(i)
            sc.drain("sync")
            sc.emit()

        pc_es = ExitStack()
        with pc_es:
            sC = lambda name, shape, dt: sb(name, shape, dt, stack=pc_es)
            FG, FG_b = sC("fgt", [128, 1024], F32)
            NCB = 4
            Y1 = [sC(f"y1{i}", [128, 1024], F32) for i in range(NCB)]
            Y2 = [sC(f"y2{i}", [128, 1024], F32) for i in range(NCB)]
            G1 = [sC(f"g1{i}", [128, 1024], BF16) for i in range(NCB)]
            G2B = [sC(f"g2b{i}", [128, 1024], BF16) for i in range(NCB)]
            X1 = [sC(f"x1{i}", [128, 1024], F32) for i in range(NCB)]
            ST3, _ = sC("st3", [128, 8], F32)
            st3b = [Buf() for _ in range(3)]
            sc.dma("sync", lambda e: e.dma_start(out=FG[:, :], in_=fg_d[:, :]), writes=[FG_b])
            G2, G2_b = sC("g2c", [128, 1024], F32)
            sc.dma("sync", lambda e: e.dma_start(out=G2[:, :], in_=g2s_d[:, :]), reads=[G2D_b], writes=[G2_b])
            def c_loads(t):
                p = t % NCB
                for k, (yy, yyb) in enumerate((G1[p], G2B[p])):
                    sc.dma("gpsimd", lambda e, k=k, t=t, yy=yy: e.indirect_dma_start(
                        out=yy[:, :], out_offset=None, in_=ys_d[:, :],
                        in_offset=bass.IndirectOffsetOnAxis(ap=DEST[:, k, t:t + 1], axis=0), bounds_check=sc.reg(e, R - 1), oob_is_err=False),
                        reads=[DEST_b] + YS_b, writes=[yyb])
                x1, x1b = X1[p]
                sc.dma("sync", lambda e, t=t, x1=x1: e.dma_start(out=x1[:, :], in_=x1_d[t * 128:(t + 1) * 128, :]), reads=[X1D_b[t]], writes=[x1b])

            st3 = [[Buf() for _ in range(3)] for _ in range(2)]

            def c_partA(t):
                p = t % NCB
                y1, y1b = Y1[p]
                y2, y2b = Y2[p]
                x1, x1b = X1[p]
                o = (t % 2) * 3
                sb3 = st3[t % 2]
                ga, gab = G1[p]
                gb2, gbb = G2B[p]
                A(lambda e, t=t, y1=y1, ga=ga: e.activation(out=y1[:, :], in_=ga[:, :], func=AF.Copy, scale=W12[:, 0, t:t + 1]), reads=[gab, W12_b], writes=[y1b])
                V(lambda e, t=t, y1=y1, gb2=gb2: e.scalar_tensor_tensor(out=y1[:, :], in0=gb2[:, :], scalar=W12[:, 1, t:t + 1], in1=y1[:, :],
                                                                        op0=ALU.mult, op1=ALU.add),
                  reads=[y1b, gbb, W12_b], writes=[y1b])
                V(lambda e, y1=y1: e.tensor_tensor(out=y1[:, :], in0=y1[:, :], in1=G2[:, :], op=ALU.mult), reads=[y1b, G2_b], writes=[y1b])
                V(lambda e, y1=y1, x1=x1: e.tensor_tensor(out=x1[:, :], in0=x1[:, :], in1=y1[:, :], op=ALU.add), reads=[y1b, x1b], writes=[x1b])
                V(lambda e, o=o: e.memset(ST3[:, o:o + 1], 0.0), writes=[sb3[0]])
                A(lambda e, x1=x1, y2=y2, o=o: e.activation(out=y2[:, :], in_=x1[:, :], func=AF.Square, accum_out=ST3[:, o:o + 1]),
                  reads=[x1b], writes=[y2b, sb3[0]])
                V(lambda e, o=o: e.tensor_scalar(ST3[:, o + 1:o + 2], ST3[:, o:o + 1], 1.0 / D, EPS, ALU.mult, ALU.add), reads=[sb3[0]], writes=[sb3[1]])
                P(lambda e, o=o: e.tensor_tensor(out=ST3[:, o + 2:o + 3], in0=ST3[:, o + 1:o + 2], in1=NEGH[:, 0:1], op=ALU.pow),
                  reads=[sb3[1], NEGH_b], writes=[sb3[2]])

            def c_partB(t):
                p = t % NCB
                y2, y2b = Y2[p]
                x1, x1b = X1[p]
                o = (t % 2) * 3
                sb3 = st3[t % 2]
                V(lambda e, x1=x1, y2=y2, o=o: e.scalar_tensor_tensor(out=y2[:, :], in0=x1[:, :], scalar=ST3[:, o + 2:o + 3], in1=FG[:, :],
                                                                      op0=ALU.mult, op1=ALU.mult),
                  reads=[x1b, sb3[2], FG_b], writes=[y2b])
                sc.dma("sync", lambda e, t=t, y2=y2: e.dma_start(out=out_d[t * 128:(t + 1) * 128, :], in_=y2[:, :]), reads=[y2b])

            for t0 in range(NCB - 1):
                c_loads(t0)
            c_partA(0)
            for t in range(NBLK):
                if t + NCB - 1 < NBLK:
                    c_loads(t + NCB - 1)
                if t + 1 < NBLK:
                    c_partA(t + 1)
                c_partB(t)
            sc.drain("sync")
            sc.emit()
    return nc, dbg_d


def _t5_thresholds():
    n = np.arange(0, 4200, dtype=np.int64)
    nf = np.maximum(n, 1).astype(np.float32)
    large = 16 + (np.log(nf / np.float32(16)) / np.float32(np.log(128 / 16)) * np.float32(16)).astype(np.int32)
    large = np.minimum(large, 31)
    bucket = np.where(n < 16, n, large)
    thr = np.zeros(32, np.float64)
    for k in range(1, 32):
        idx = np.nonzero(bucket >= k)[0]
        thr[k] = float(idx[0]) if len(idx) else 1e9
    return thr


def _prep_shared(inp):
    f = lambda a: np.ascontiguousarray(np.asarray(a, dtype=np.float32))
    sh = {}
    sh["zrows"] = np.zeros((1536, D), dtype=ml_dtypes.bfloat16)
    sh["relb"] = f(np.broadcast_to(np.asarray(inp["rel_bias"], np.float32).reshape(1, 256), (128, 256)))
    wada = np.asarray(inp["w_ada"], np.float32)[0].reshape(8, 128, 6, 1024)
    sh["w_ada"] = f(wada.transpose(1, 2, 0, 3))
    bada = np.asarray(inp["b_ada"], np.float32)[0]
    sh["b_ada_f"] = f(bada.reshape(6, 8, 128).transpose(2, 0, 1))
    sh["b_ada_r"] = f(np.broadcast_to(bada.reshape(1, 6, 1024), (128, 6, 1024)))
    sh["n1g"] = f(np.asarray(inp["norm1_g"], np.float32)[0].reshape(8, 128).T)
    sh["n2g"] = f(np.broadcast_to(np.asarray(inp["norm2_g"], np.float32)[0].reshape(1, 1024), (128, 1024)))
    sh["fg"] = f(np.broadcast_to(np.asarray(inp["final_g"], np.float32).reshape(1, 1024), (128, 1024)))
    w = np.asarray(inp["w_in"], np.float32)[0]
    wdev = np.concatenate([w[:, 0:512], w[:, 512:576], w[:, 512:576], w[:, 576:640], w[:, 576:640],
                           w[:, 640:768], w[:, 768:1280], w[:, 1280:1792], w[:, 1792:2816], w[:, 2816:3840]], axis=1)
    sh["w_in"] = f(wdev.reshape(8, 128, INW).transpose(1, 0, 2))
    sh["sinks"] = f(np.broadcast_to(np.asarray(inp["sinks"], np.float32)[0].reshape(1, 8), (128, 8)))
    sh["lng"] = f(np.broadcast_to(np.asarray(inp["gm_ln_g"], np.float32)[0].reshape(1, 512), (128, 512)))
    sh["lnb"] = f(np.broadcast_to(np.asarray(inp["gm_ln_b"], np.float32)[0].reshape(1, 512), (128, 512)))
    sh["wst"] = f(np.asarray(inp["gm_w_s"], np.float32)[0].transpose(2, 0, 1))
    sh["bs"] = f(np.asarray(inp["gm_b_s"], np.float32)[0].reshape(1, 512))
    sh["p_a"] = f(np.asarray(inp["p_a"], np.float32)[0].reshape(4, 128, 1024).transpose(1, 0, 2))
    sh["p_b"] = f(np.asarray(inp["p_b"], np.float32)[0].reshape(4, 128, 1024).transpose(1, 0, 2))
    sh["w_o"] = f(np.asarray(inp["w_o"], np.float32)[0].reshape(8, 128, 1024).transpose(1, 0, 2))
    wr = np.concatenate([np.asarray(inp["w_router_g"], np.float32)[0], np.asarray(inp["w_router_e"], np.float32)[0]], axis=1)
    sh["w_r"] = f(wr.reshape(8, 128, 36).transpose(1, 0, 2))
    sh["b_r"] = f(np.concatenate([np.asarray(inp["b_router_g"], np.float32)[0],
                                  np.asarray(inp["b_router_e"], np.float32)[0]]).reshape(1, 36))
    for nm in ("w_gate", "w_up", "w_down"):
        w2 = np.asarray(inp[nm], np.float32)[0].reshape(NE * 128, 2, 2048)
        sh[nm + "0"] = f(w2[:, 0, :])
        sh[nm + "1"] = f(w2[:, 1, :])
    return sh


def _prep_core(inp, b):
    m = {}
    m["x"] = np.ascontiguousarray(np.asarray(inp["x"], np.float32)[b])
    m["c"] = np.ascontiguousarray(np.asarray(inp["c"], np.float32)[b].reshape(8, 128).T)
    pos = np.asarray(inp["positions"], np.int32)[b]
    m["posq"] = np.ascontiguousarray(np.broadcast_to(pos[128:256].reshape(1, 128), (128, 128))).astype(np.int32)
    m["posk"] = np.ascontiguousarray(np.stack([pos[0:128], pos[128:256]], axis=1)).astype(np.int32)
    return m


def run(inputs, stage="full", dbg=False, n_cores=8):
    nc, dbg_d = build_nc(stage=stage, dbg=dbg)
    sh = _prep_shared(inputs)
    in_maps = []
    for b in range(n_cores):
        m = dict(sh)
        m.update(_prep_core(inputs, b))
        in_maps.append(m)
    res = run_bass_kernel_spmd(nc, in_maps, core_ids=list(range(n_cores)))
    return res


def kernel(**inputs):
    res = run(inputs)
    out = np.stack([np.asarray(r["out"], np.float32).reshape(S, D) for r in res.results], axis=0)
    return out
```

```python
import numpy as np
import ml_dtypes
from contextlib import ExitStack
import concourse.bass as bass
import concourse.mybir as mybir
from concourse.bass_utils import run_bass_kernel_spmd

F32 = mybir.dt.float32
BF16 = mybir.dt.bfloat16
I32 = mybir.dt.int32
AF = mybir.ActivationFunctionType
ALU = mybir.AluOpType
AX = mybir.AxisListType

D = 1024
S = 4096
NBLK = 32
ST = 512
NST = 8
BPS = 4
NE = 32
NB = 96
NLANE = 6
LB = NB // NLANE
R = NB * 128
INW = 3968
C_Q, C_K, C_V, C_GU, C_GV, C_GA, C_GB = 0, 512, 768, 896, 1408, 1920, 2944
EPS = 1e-6
GELU_FN = AF.Gelu_apprx_tanh


class Buf:
    __slots__ = ("w", "r", "ro")

    def __init__(self):
        self.w = []
        self.r = []
        self.ro = False


class Q:
    def __init__(self, name, sems, dma_sems):
        self.name = name
        self.spare = list(sems)
        self.sem = self.spare.pop()
        self.cnt = 0
        self.waited = {}
        self.prog = []
        self.dma = [[sm, 0] for sm in dma_sems]
        self.dma_i = 0


class Sched:
    def __init__(self, nc, es):
        self.nc = nc
        sems = [es.enter_context(nc.semaphore(f"s{i}")) for i in range(96)]
        it = iter(sems)
        take = lambda n: [next(it) for _ in range(n)]
        self.q = {
            "tensor": Q("tensor", take(4), []),
            "vector": Q("vector", take(3), []),
            "scalar": Q("scalar", take(3), take(4)),
            "gpsimd": Q("gpsimd", take(3), take(24)),
            "sync": Q("sync", take(2), take(24)),
        }
        self.all_dma = []

    def _wait(self, q, tok):
        sem, val = tok
        k = id(sem)
        if q.waited.get(k, 0) >= val:
            return
        q.waited[k] = val
        q.prog.append(("w", sem, val))

    def _deps(self, q, reads, writes, skip_self, join=False):
        need = {}

        def add(t):
            if skip_self and t[0] is q.sem:
                return
            k = id(t[0])
            if k not in need or need[k][1] < t[1]:
                need[k] = t

        for b in reads:
            for t in b.w:
                add(t)
        for b in writes:
            if not join:
                for t in b.w:
                    add(t)
            for t in b.r:
                add(t)
        for t in need.values():
            self._wait(q, t)

    def op(self, qn, fn, reads=(), writes=()):
        q = self.q[qn]
        if q.cnt >= 24000:
            q.sem = q.spare.pop()
            q.cnt = 0
        self._deps(q, reads, writes, qn == "tensor")
        q.cnt += 1
        tok = (q.sem, q.cnt)
        q.prog.append(("i", fn, q.sem, 1))
        for b in writes:
            b.w = [tok]
            b.r = []
        for b in reads:
            if not b.ro and b not in writes:
                b.r.append(tok)
        return tok

    def dma(self, qn, fn, reads=(), writes=(), join=False):
        q = self.q[qn]
        self._deps(q, reads, writes, False, join=join)
        slot = q.dma[q.dma_i % len(q.dma)]
        q.dma_i += 1
        if slot[1] > 0:
            self._wait(q, (slot[0], slot[1]))
        slot[1] += 16
        tok = (slot[0], slot[1])
        q.prog.append(("i", fn, slot[0], 16))
        for b in writes:
            if join:
                b.w = b.w + [tok]
            else:
                b.w = [tok]
                b.r = []
        for b in reads:
            if not b.ro and b not in writes:
                b.r.append(tok)
        self.all_dma.append(tok)
        return tok

    def drain(self, qn="sync"):
        q = self.q[qn]
        last = {}
        for sem, val in self.all_dma:
            last[id(sem)] = (sem, max(val, last.get(id(sem), (sem, 0))[1]))
        for tok in last.values():
            self._wait(q, tok)
        self.all_dma = []

    def reg(self, e, val):
        if val not in self.regcache:
            self.regcache[val] = e.to_reg(val)
        return self.regcache[val]

    def emit(self):
        self.regcache = {}
        with self.nc.Block() as blk:
            for name in ["tensor", "vector", "scalar", "gpsimd", "sync"]:
                q = self.q[name]
                items = q.prog
                q.prog = []

                def body(e, items=items):
                    for it in items:
                        if it[0] == "w":
                            e.wait_ge(it[1], it[2])
                        else:
                            it[1](e).then_inc(it[2], it[3])

                getattr(blk, name)(body)


def bc(ap, shape):
    return ap.broadcast_to(list(shape))


def build_nc(stage="full", dbg=False):
    nc = bass.Bass("TRN2", target_bir_lowering=False)

    def din(name, shape, dt=F32):
        return nc.dram_tensor(name, list(shape), dt, kind="ExternalInput").ap()

    x_d = din("x", [S, D])
    c_d = din("c", [128, 8])
    posq_d = din("posq", [128, 128], I32)
    posk_d = din("posk", [128, 2], I32)
    rb_d = din("relb", [128, 256])
    wada_d = din("w_ada", [128, 6, 8, 1024])
    badaf_d = din("b_ada_f", [128, 6, 8])
    badar_d = din("b_ada_r", [128, 6, 1024])
    n1g_d = din("n1g", [128, 8])
    n2g_d = din("n2g", [128, 1024])
    fg_d = din("fg", [128, 1024])
    win_d = din("w_in", [128, 8, INW])
    sinks_d = din("sinks", [128, 8])
    lng_d = din("lng", [128, 512])
    lnb_d = din("lnb", [128, 512])
    wst_d = din("wst", [128, 4, 128])
    bs_d = din("bs", [1, 512])
    pa_d = din("p_a", [128, 4, 1024])
    pb_d = din("p_b", [128, 4, 1024])
    wo_d = din("w_o", [128, 8, 1024])
    wr_d = din("w_r", [128, 8, 36])
    br_d = din("b_r", [1, 36])
    wg_d = [din(f"w_gate{i}", [NE * 128, 2048]) for i in range(2)]
    wu_d = [din(f"w_up{i}", [NE * 128, 2048]) for i in range(2)]
    wd_d = [din(f"w_down{i}", [NE * 128, 2048]) for i in range(2)]
    zr_d = din("zrows", [1536, D], BF16)
    out_d = nc.dram_tensor("out", [S, D], F32, kind="ExternalOutput").ap()
    h2_d = nc.dram_tensor("h2s", [S, D], BF16, kind="Internal").ap()
    x1_d = nc.dram_tensor("x1s", [S, D], F32, kind="Internal").ap()
    ys_d = nc.dram_tensor("yss", [R, D], BF16, kind="Internal").ap()
    st_d = nc.dram_tensor("sts", [R, 2], I32, kind="Internal").ap()
    g2s_d = nc.dram_tensor("g2s", [128, 1024], F32, kind="Internal").ap()
    xs_d = nc.dram_tensor("xss", [R, D], BF16, kind="Internal").ap()
    dbg_d = {}
    if dbg:
        dbg_d["lg"] = nc.dram_tensor("dbg_lg", [128, NBLK * 36], F32, kind="ExternalOutput").ap()
        dbg_d["x1"] = nc.dram_tensor("dbg_x1", [S, D], F32, kind="ExternalOutput").ap()
        dbg_d["rt"] = nc.dram_tensor("dbg_rt", [128, 512], F32, kind="ExternalOutput").ap()

    es = ExitStack()
    with es:
        sc = Sched(nc, es)
        T = lambda *a, **k: sc.op("tensor", *a, **k)
        V = lambda *a, **k: sc.op("vector", *a, **k)
        A = lambda *a, **k: sc.op("scalar", *a, **k)
        P = lambda *a, **k: sc.op("gpsimd", *a, **k)

        def sb(name, shape, dt, stack=es):
            t = stack.enter_context(nc.sbuf_tensor(name, list(shape), dt))
            return t, Buf()

        X1D_b = [Buf() for _ in range(NBLK)]
        H2D_b = [Buf() for _ in range(NBLK)]
        YS_b = [Buf() for _ in range(NB)]
        XS_b = [Buf() for _ in range(2 * NBLK)]
        XZ_b = [Buf() for _ in range(NST)]
        banks = []
        for i in range(8):
            t = es.enter_context(nc.psum_tensor(f"bank{i}", [128, 512], F32))
            banks.append((t, Buf()))

        def bank_bf(i):
            return banks[i][0][:, :].bitcast(BF16)

        IDB, IDB_b = sb("idb", [128, 128], BF16)
        IDF, IDF_b = sb("idf", [128, 128], F32)
        ONEF, ONEF_b = sb("onef", [128, 128], F32)
        G2D_b = Buf()
        LG, LG_b = sb("lg", [128, NBLK, 36], F32)
        NEGH, NEGH_b = sb("negh", [128, 8], F32)
        W12, W12_b = sb("w12", [128, 2, 32], F32)
        DEST, DEST_b = sb("dest", [128, 2, 32], I32)
        SLOT, SLOT_b = sb("slot", [128, NB], I32)
        IDXW, IDXW_b = sb("idxw", [128, NB], I32)

        V(lambda e: e.memset(ONEF[:, :], 1.0), writes=[ONEF_b])
        V(lambda e: e.memset(NEGH[:, :], -0.5), writes=[NEGH_b])
        P(lambda e: e.affine_select(out=IDF[:, :], in_=ONEF[:, :], pattern=[[-1, 128]],
                                    compare_op=ALU.is_equal, fill=0.0, base=0, channel_multiplier=1),
          reads=[ONEF_b], writes=[IDF_b])
        V(lambda e: e.tensor_copy(out=IDB[:, :], in_=IDF[:, :]), reads=[IDF_b], writes=[IDB_b])

        pa_es = ExitStack()
        with pa_es:
            def sa(name, shape, dt):
                return sb(name, shape, dt, stack=pa_es)

            WIN, WIN_b = sa("win", [128, 8, INW], BF16)
            PAW, PAW_b = sa("paw", [128, 4, 1024], BF16)
            PBW, PBW_b = sa("pbw", [128, 4, 1024], BF16)
            WOW, WOW_b = sa("wow", [128, 8, 1024], BF16)
            KT, KT_b = sa("kt", [128, 2, 2, 640], BF16)
            VA, VA_b = sa("va", [128, 5, 2, 65], BF16)
            BMH, BMH_b = sa("bmh", [128, 2, 8, 128], BF16)
            BML, BML_b = sa("bml", [128, 2, 8, 128], BF16)
            G1H, G1H_b = sa("g1h", [128, 1024], F32)
            A2, A2_b = sa("a2", [128, 1024], F32)
            SH2, SH2_b = sa("sh2", [128, 1024], F32)
            LNG, LNG_b = sa("lngt", [128, 512], F32)
            LNB, LNB_b = sa("lnbt", [128, 512], F32)
            WT, WT_b = sa("wt", [128, 4, 128], BF16)
            BS, BS_b = sa("bst", [1, 512], F32)
            WR, WR_b = sa("wr", [128, 8, 36], F32)
            BR, BR_b = sa("brt", [1, 36], F32)
            ES_, ES_b = sa("es", [128, 8], F32)
            A1, A1_b = sa("a1", [128, 8], F32)
            SH1, SH1_b = sa("sh1", [128, 8], F32)
            SM, SM_b = sa("small", [128, 64], F32)
            smb = [Buf() for _ in range(8)]
            p0_es = ExitStack()
            p0_es.__enter__()
            sa0 = lambda name, shape, dt: sb(name, shape, dt, stack=p0_es)


            thr = _t5_thresholds()
            RBT, RBT_b = sa0("rbt", [128, 32, 8], F32)
            PQ, PQ_b = sa0("pq", [128, 128], I32)
            PK, PK_b = sa0("pk", [128, 2], I32)
            PKF, PKF_b = sa0("pkf", [128, 2], F32)
            DD, DD_b = sa0("dd", [128, 2, 128], F32)
            IND, IND_b = sa0("ind", [128, 2, 128], F32)
            sc.dma("sync", lambda e: e.dma_start(out=RBT[:, :, :], in_=rb_d.rearrange("p (k h) -> p k h", h=8)), writes=[RBT_b])
            sc.dma("sync", lambda e: e.dma_start(out=PQ[:, :], in_=posq_d[:, :]), writes=[PQ_b])
            sc.dma("sync", lambda e: e.dma_start(out=PK[:, :], in_=posk_d[:, :]), writes=[PK_b])
            V(lambda e: e.tensor_copy(out=PKF[:, :], in_=PK[:, :]), reads=[PK_b], writes=[PKF_b])
            V(lambda e: e.tensor_copy(out=DD[:, 0, :], in_=PQ[:, :]), reads=[PQ_b], writes=[DD_b])
            V(lambda e: e.tensor_copy(out=DD[:, 1, :], in_=PQ[:, :]), reads=[PQ_b], writes=[DD_b])
            for hf in range(2):
                V(lambda e, hf=hf: e.tensor_scalar(DD[:, hf, :], DD[:, hf, :], PKF[:, hf:hf + 1], 0.0, ALU.subtract, ALU.max),
                  reads=[DD_b, PKF_b], writes=[DD_b])
            DLT, DLT_b = sa0("dlt", [128, 32, 8], F32)
            V(lambda e: e.tensor_copy(out=DLT[:, 0:1, :], in_=RBT[:, 0:1, :]), reads=[RBT_b], writes=[DLT_b])
            V(lambda e: e.tensor_tensor(out=DLT[:, 1:32, :], in0=RBT[:, 1:32, :], in1=RBT[:, 0:31, :], op=ALU.subtract),
              reads=[RBT_b], writes=[DLT_b])
            BA, _ = sa0("ba", [128, 2, 8, 128], F32)
            BAh = [Buf() for _ in range(8)]
            IND2, IND2_b = sa0("ind2", [128, 2, 128], F32)
            inds = [(IND, IND_b), (IND2, IND2_b)]
            for h in range(8):
                V(lambda e, h=h: e.tensor_copy(out=BA[:, :, h, :], in_=bc(DLT[:, 0, h:h + 1].unsqueeze(1), [128, 2, 128])),
                  reads=[DLT_b], writes=[BAh[h]])
            def t5_chunk(k0, k1):
                for k in range(k0, k1):
                    it_, itb = inds[k % 2]
                    V(lambda e, k=k, it_=it_: e.tensor_scalar(it_[:, :, :], DD[:, :, :], float(thr[k]), None, ALU.is_ge),
                      reads=[DD_b], writes=[itb])
                    for h in range(8):
                        V(lambda e, k=k, h=h, it_=it_: e.scalar_tensor_tensor(
                            out=BA[:, :, h, :], in0=it_[:, :, :], scalar=DLT[:, k, h:h + 1],
                            in1=BA[:, :, h, :], op0=ALU.mult, op1=ALU.add),
                            reads=[itb, DLT_b, BAh[h]], writes=[BAh[h]])
            t5_bounds = [1, 7, 12, 17, 22, 27, 32]
            def cast_load(dst_ap, src_ap, buf, join=False):
                sc.dma("gpsimd", lambda e: e.dma_start(out=dst_ap, in_=src_ap), writes=[buf], join=join)

            CL, CL_b = sa0("cl", [128, 8], F32)
            CS, CS_b = sa0("cs", [128, 8], BF16)
            CSR, CSR_b = sa0("csr", [128, 8, 128], BF16)
            WAD = [sa0(f"wad{i}", [128, 8, 1024], BF16) for i in range(2)]
            sc.dma("sync", lambda e: e.dma_start(out=CL[:, :], in_=c_d[:, :]), writes=[CL_b])
            A(lambda e: e.activation(out=CL[:, :], in_=CL[:, :], func=AF.Silu), reads=[CL_b], writes=[CL_b])
            V(lambda e: e.tensor_copy(out=CS[:, :], in_=CL[:, :]), reads=[CL_b], writes=[CS_b])
            V(lambda e: e.tensor_copy(out=CSR[:, :, :], in_=bc(CL[:, :].unsqueeze(2), [128, 8, 128])),
              reads=[CL_b], writes=[CSR_b])
            BF_, BF_b = sa0("badaf", [128, 6, 8], F32)
            sc.dma("sync", lambda e: e.dma_start(out=BF_[:, :, :], in_=badaf_d[:, :, :]), writes=[BF_b])
            N1G, N1G_b = sa0("n1gt", [128, 8], F32)
            sc.dma("sync", lambda e: e.dma_start(out=N1G[:, :], in_=n1g_d[:, :]), writes=[N1G_b])
            sc.dma("sync", lambda e: e.dma_start(out=G1H[:, :], in_=badar_d[:, 2, :]), writes=[G1H_b])
            sc.dma("sync", lambda e: e.dma_start(out=SH2[:, :], in_=badar_d[:, 3, :]), writes=[SH2_b])
            sc.dma("sync", lambda e: e.dma_start(out=A2[:, :], in_=badar_d[:, 4, :]), writes=[A2_b])
            G2, G2_b = sa0("g2t", [128, 1024], F32)
            sc.dma("sync", lambda e: e.dma_start(out=G2[:, :], in_=badar_d[:, 5, :]), writes=[G2_b])
            N2G, N2G_b = sa0("n2gt", [128, 1024], F32)
            sc.dma("sync", lambda e: e.dma_start(out=N2G[:, :], in_=n2g_d[:, :]), writes=[N2G_b])

            for v in range(6):
                t5_chunk(t5_bounds[v], t5_bounds[v + 1])
                wt_, wb_ = WAD[v % 2]
                for kk in range(4):
                    cast_load(wt_[:, 2 * kk:2 * kk + 2, :], wada_d[:, v, 2 * kk:2 * kk + 2, :], wb_, join=(kk > 0))
                if v < 2:
                    bk, bkb = banks[v]
                    for cc in range(8):
                        for kc in range(8):
                            T(lambda e, cc=cc, kc=kc, wt_=wt_, bk=bk: e.matmul(
                                out=bk[:, cc:cc + 1], lhsT=wt_[:, kc, cc * 128:(cc + 1) * 128],
                                rhs=CS[:, kc:kc + 1], start=(kc == 0), stop=(kc == 7)),
                              reads=[wb_, CS_b], writes=[bkb])
                    if v == 0:
                        V(lambda e, bk=bk: e.tensor_tensor(out=SH1[:, :], in0=bk[:, 0:8], in1=BF_[:, 0, :], op=ALU.add),
                          reads=[bkb, BF_b], writes=[SH1_b])
                    else:
                        V(lambda e, bk=bk: e.tensor_tensor(out=A1[:, :], in0=bk[:, 0:8], in1=BF_[:, 1, :], op=ALU.add),
                          reads=[bkb, BF_b], writes=[A1_b])
                        V(lambda e: e.scalar_tensor_tensor(out=A1[:, :], in0=A1[:, :], scalar=1.0, in1=N1G[:, :],
                                                           op0=ALU.add, op1=ALU.mult),
                          reads=[A1_b, N1G_b], writes=[A1_b])
                else:
                    dst, dstb = {2: (G1H, G1H_b), 3: (SH2, SH2_b), 4: (A2, A2_b), 5: (G2, G2_b)}[v]
                    for hf in range(2):
                        bk, bkb = banks[2 + hf]
                        for kc in range(8):
                            T(lambda e, kc=kc, hf=hf, wt_=wt_, bk=bk: e.matmul(
                                out=bk[:, :], lhsT=CSR[:, kc, :], rhs=wt_[:, kc, hf * 512:(hf + 1) * 512],
                                start=(kc == 0), stop=(kc == 7)),
                              reads=[wb_, CSR_b], writes=[bkb])
                        V(lambda e, hf=hf, bk=bk, dst=dst: e.tensor_tensor(
                            out=dst[:, hf * 512:(hf + 1) * 512], in0=bk[:, :], in1=dst[:, hf * 512:(hf + 1) * 512], op=ALU.add),
                          reads=[bkb, dstb], writes=[dstb])
                    if v == 2:
                        V(lambda e: e.tensor_scalar(G1H[:, :], G1H[:, :], 0.5, None, ALU.mult), reads=[G1H_b], writes=[G1H_b])
                    if v == 4:
                        V(lambda e: e.scalar_tensor_tensor(out=A2[:, :], in0=A2[:, :], scalar=1.0, in1=N2G[:, :],
                                                           op0=ALU.add, op1=ALU.mult),
                          reads=[A2_b, N2G_b], writes=[A2_b])

            for kc in range(8):
                for hh in range(2):
                    cast_load(WIN[:, kc, hh * 1984:(hh + 1) * 1984], win_d[:, kc, hh * 1984:(hh + 1) * 1984], WIN_b, join=(kc + hh > 0))
            for kc in range(4):
                cast_load(PAW[:, kc, :], pa_d[:, kc, :], PAW_b, join=(kc > 0))
                cast_load(PBW[:, kc, :], pb_d[:, kc, :], PBW_b, join=(kc > 0))
            for kc in range(8):
                cast_load(WOW[:, kc, :], wo_d[:, kc, :], WOW_b, join=(kc > 0))
            cast_load(WT[:, :, :], wst_d[:, :, :], WT_b)
            P(lambda e: e.affine_select(out=WT[:, :, :], in_=WT[:, :, :], pattern=[[0, 4], [1, 128]],
                                        compare_op=ALU.is_ge, fill=0.0, base=0, channel_multiplier=-1),
              reads=[WT_b], writes=[WT_b])
            for (dap, dstb, sap) in [(LNG[:, :], LNG_b, lng_d[:, :]), (LNB[:, :], LNB_b, lnb_d[:, :]), (BS[:, :], BS_b, bs_d[:, :]),
                                     (WR[:, :, :], WR_b, wr_d[:, :, :]), (BR[:, :], BR_b, br_d[:, :]), (ES_[:, :], ES_b, sinks_d[:, :])]:
                sc.dma("sync", lambda e, dap=dap, sap=sap: e.dma_start(out=dap, in_=sap), writes=[dstb])
            A(lambda e: e.activation(out=ES_[:, :], in_=ES_[:, :], func=AF.Exp), reads=[ES_b], writes=[ES_b])
            V(lambda e: e.memset(VA[:, :, :, :], 1.0), writes=[VA_b])
            V(lambda e: e.memset(KT[:, :, :, :], 0.0), writes=[KT_b])

            P(lambda e: e.affine_select(out=BA[:, 0, :, :], in_=BA[:, 0, :, :], pattern=[[0, 8], [-1, 128]],
                                        compare_op=ALU.is_gt, fill=-30000.0, base=0, channel_multiplier=1),
              reads=BAh, writes=BAh)
            P(lambda e: e.affine_select(out=BA[:, 1, :, :], in_=BA[:, 1, :, :], pattern=[[0, 8], [1, 128]],
                                        compare_op=ALU.is_ge, fill=-30000.0, base=0, channel_multiplier=-1),
              reads=BAh, writes=BAh)
            V(lambda e: e.tensor_copy(out=BMH[:, :, :, :], in_=BA[:, :, :, :]), reads=BAh, writes=[BMH_b])
            V(lambda e: e.tensor_tensor(out=BA[:, :, :, :], in0=BA[:, :, :, :], in1=BMH[:, :, :, :], op=ALU.subtract),
              reads=BAh + [BMH_b], writes=BAh)
            V(lambda e: e.tensor_copy(out=BML[:, :, :, :], in_=BA[:, :, :, :]), reads=BAh, writes=[BML_b])

            for b_ in (WIN_b, PAW_b, PBW_b, WOW_b):
                pass

            sc.dma("sync", lambda e: e.dma_start(out=g2s_d[:, :], in_=G2[:, :]), reads=[G2_b], writes=[G2D_b])
            sc.drain("sync")
            sc.emit()
            p0_es.__exit__(None, None, None)
            XT, _ = sa("xt", [128, 4, 1024], F32)
            xb = [Buf() for _ in range(4)]
            XN = [sa(f"xn{i}", [128, 1024], BF16) for i in range(2)]
            HT, _ = sa("ht", [128, 8, ST], BF16)
            HTk = [Buf() for _ in range(8)]
            QT, QT_b = sa("qt", [128, 4, ST], BF16)
            UT, UT_b = sa("ut", [128, 4, ST], BF16)
            YA, YA_b = sa("ya", [128, 512], BF16)
            YAT, YAT_b = sa("yat", [128, 4, ST], BF16)
            YBT, YBT_b = sa("ybt", [128, 4, ST], BF16)
            MT, MT_b = sa("mt", [128, 8, ST], BF16)
            RA, RA_b = sa("ra", [128, 512], F32)
            RB, RB_b = sa("rb", [128, 512], F32)
            RC, RC_b = sa("rc", [128, 512], F32)
            RD, RD_b = sa("rd", [128, 1024], F32)
            RE, RE_b = sa("re", [128, 1024], BF16)
            VNS = [sa(f"vn{i}", [128, 512], BF16) for i in range(2)]
            H2T, H2T_b = sa("h2t", [128, 8, 128], F32)
            H2Tb_b = Buf()
            LGT, LGT_b = sa("lgt", [128, 128], F32)
            PT_bf = RD[:, :].bitcast(BF16)
            n_st = NST if stage != "A1" else 1
            re_half = [Buf(), Buf()]
            pj = [0]
            pf = [0]

            def next_proj_bank():
                pj[0] ^= 1
                return banks[pj[0]]

            for s in range(n_st):
                for b in range(BPS):
                    n = s * BPS + b
                    sc.dma("sync", lambda e, b=b, n=n: e.dma_start(out=XT[:, b, :], in_=x_d[n * 128:(n + 1) * 128, :]),
                           writes=[xb[b]])
                sc.dma("scalar", lambda e, s=s: e.dma_start(out=xs_d[s * 1536:(s + 1) * 1536, :], in_=zr_d[:, :]), writes=[XZ_b[s]])
                SSQ = SM[:, 0:4]
                RSTD = SM[:, 4:8]
                V(lambda e: e.memset(SSQ, 0.0), writes=[smb[0]])
                for b in range(BPS):
                    xn_t, xn_b = XN[b % 2]
                    A(lambda e, b=b, xn_t=xn_t: e.activation(out=xn_t[:, :], in_=XT[:, b, :], func=AF.Square,
                                                             accum_out=SM[:, b:b + 1]),
                      reads=[xb[b]], writes=[xn_b, smb[0]])
                V(lambda e: e.tensor_scalar(SM[:, 8:12], SSQ, 1.0 / D, EPS, ALU.mult, ALU.add), reads=[smb[0]], writes=[smb[1]])
                P(lambda e: e.tensor_tensor(out=RSTD, in0=SM[:, 8:12], in1=NEGH[:, 0:4], op=ALU.pow),
                  reads=[smb[1], NEGH_b], writes=[smb[2]])
                for b in range(BPS):
                    xn_t, xn_b = XN[b % 2]
                    A(lambda e, b=b, xn_t=xn_t: e.activation(out=xn_t[:, :], in_=XT[:, b, :], func=AF.Copy,
                                                             scale=SM[:, 4 + b:5 + b]),
                      reads=[xb[b], smb[2]], writes=[xn_b])
                    for c in range(8):
                        bi = 2 + c // 2
                        T(lambda e, b=b, c=c, bi=bi, xn_t=xn_t: e.transpose(
                            out=bank_bf(bi)[:, (c % 2) * 512 + b * 128:(c % 2) * 512 + (b + 1) * 128],
                            in_=xn_t[:, c * 128:(c + 1) * 128], identity=IDB[:, :]),
                          reads=[xn_b, IDB_b], writes=[banks[bi][1]])
                for c in range(8):
                    bi = 2 + c // 2
                    V(lambda e, c=c, bi=bi: e.tensor_scalar(HT[:, c, :], bank_bf(bi)[:, (c % 2) * 512:(c % 2 + 1) * 512],
                                                            A1[:, c:c + 1], SH1[:, c:c + 1], ALU.mult, ALU.add),
                      reads=[banks[bi][1], A1_b, SH1_b], writes=[HTk[c]])

                def proj_fm(col, evac):
                    pf[0] = (pf[0] + 1) % 4
                    bk, bkb = banks[(0, 1, 6, 7)[pf[0]]]
                    for kc in range(8):
                        T(lambda e, kc=kc, bk=bk: e.matmul(out=bk[:, :], lhsT=WIN[:, kc, col:col + 128], rhs=HT[:, kc, :],
                                                           start=(kc == 0), stop=(kc == 7)),
                          reads=[WIN_b, HTk[kc]], writes=[bkb])
                    evac(bk, bkb)

                for i in range(4):
                    proj_fm(C_Q + i * 128, lambda bk, bkb, i=i: V(
                        lambda e: e.tensor_scalar(QT[:, i, :], bk[:, :], 0.125, None, ALU.mult), reads=[bkb], writes=[QT_b]))
                def k_evac(bk, bkb, i):
                    V(lambda e: e.tensor_copy(out=KT[0:64, i, 0, 128:640], in_=bk[0:64, :]), reads=[bkb], writes=[KT_b])
                    V(lambda e: e.tensor_copy(out=KT[64:128, i, 1, 128:640], in_=bk[64:128, :]), reads=[bkb], writes=[KT_b])

                for i in range(2):
                    proj_fm(C_K + i * 128, lambda bk, bkb, i=i: k_evac(bk, bkb, i))
                for i in range(4):
                    proj_fm(C_GU + i * 128, lambda bk, bkb, i=i: A(
                        lambda e: e.activation(out=UT[:, i, :], in_=bk[:, :], func=GELU_FN), reads=[bkb], writes=[UT_b]))

                def tm_front(b):
                    blk = slice(b * 128, (b + 1) * 128)
                    vn_t, vn_b = VNS[b % 2]
                    bk, bkb = next_proj_bank()
                    for kc in range(8):
                        T(lambda e, kc=kc, bk=bk, blk=blk: e.matmul(out=bk[:, 0:128], lhsT=HT[:, kc, blk],
                                                                    rhs=WIN[:, kc, C_V:C_V + 128], start=(kc == 0), stop=(kc == 7)),
                          reads=[WIN_b, HTk[kc]], writes=[bkb])
                    V(lambda e, b=b, bk=bk: e.tensor_copy(out=VA[:, 1 + b, :, 0:64],
                                                          in_=bk[:, 0:128].rearrange("p (k d) -> p k d", d=64)),
                      reads=[bkb], writes=[VA_b])
                    bk, bkb = next_proj_bank()
                    for kc in range(8):
                        T(lambda e, kc=kc, bk=bk, blk=blk: e.matmul(out=bk[:, :], lhsT=HT[:, kc, blk],
                                                                    rhs=WIN[:, kc, C_GV:C_GV + 512], start=(kc == 0), stop=(kc == 7)),
                          reads=[WIN_b, HTk[kc]], writes=[bkb])
                    A(lambda e, bk=bk: e.activation(out=RB[:, :], in_=bk[:, :], func=GELU_FN), reads=[bkb], writes=[RB_b])
                    V(lambda e: e.bn_stats(out=SM[:, 16:22], in_=RB[:, :]), reads=[RB_b], writes=[smb[3]])
                    V(lambda e: e.bn_aggr(out=SM[:, 22:24], in_=SM[:, 16:22]), reads=[smb[3]], writes=[smb[4]])
                    V(lambda e: e.tensor_scalar(SM[:, 24:25], SM[:, 23:24], EPS, None, ALU.add), reads=[smb[4]], writes=[smb[5]])
                    P(lambda e: e.tensor_tensor(out=SM[:, 25:26], in0=SM[:, 24:25], in1=NEGH[:, 0:1], op=ALU.pow),
                      reads=[smb[5], NEGH_b], writes=[smb[6]])
                    V(lambda e: e.tensor_scalar(RC[:, :], RB[:, :], SM[:, 22:23], SM[:, 25:26], ALU.subtract, ALU.mult),
                      reads=[RB_b, smb[4], smb[6]], writes=[RC_b])
                    P(lambda e: e.tensor_tensor(out=RC[:, :], in0=RC[:, :], in1=LNG[:, :], op=ALU.mult),
                      reads=[RC_b, LNG_b], writes=[RC_b])
                    P(lambda e, vn_t=vn_t: e.tensor_tensor(out=vn_t[:, :], in0=RC[:, :], in1=LNB[:, :], op=ALU.add),
                      reads=[RC_b, LNB_b], writes=[vn_b])

                def tm_spatial(b):
                    blk = slice(b * 128, (b + 1) * 128)
                    vn_t, vn_b = VNS[b % 2]
                    bk, bkb = next_proj_bank()
                    for g in range(4):
                        gs = slice(g * 128, (g + 1) * 128)
                        T(lambda e, bk=bk, gs=gs, g=g, vn_t=vn_t: e.matmul(out=bk[:, gs], lhsT=vn_t[:, gs], rhs=WT[:, g, :], start=True, stop=False),
                          reads=[vn_b, WT_b], writes=[bkb])
                        T(lambda e, bk=bk, gs=gs: e.matmul(out=bk[:, gs], lhsT=ONEF[0:1, 0:128], rhs=BS[0:1, gs], start=False, stop=True),
                          reads=[ONEF_b, BS_b], writes=[bkb])
                    V(lambda e, bk=bk, blk=blk: e.tensor_tensor(out=YBT[:, :, blk], in0=bk[:, :].rearrange("p (g t) -> p g t", t=128),
                                                                in1=UT[:, :, blk], op=ALU.mult),
                      reads=[bkb, UT_b], writes=[YBT_b])

                def at_info(b):
                    n = s * BPS + b
                    return n, slice(b * 128, (b + 1) * 128), ([1] if n == 0 else [0, 1])

                def at_S(b):
                    n, blk, halves = at_info(b)
                    for hf in halves:
                        kcols = slice((b + hf) * 128, (b + hf + 1) * 128)
                        for h in range(8):
                            par, kvh = h % 2, h // 4
                            bk, bkb = banks[2 + hf * 2 + h // 4]
                            oc = slice((h % 4) * 128, (h % 4 + 1) * 128)
                            T(lambda e, bk=bk, oc=oc, hf=hf, h=h: e.matmul(out=bk[:, oc], lhsT=IDB[:, :], rhs=BMH[:, hf, h, :], start=True, stop=False),
                              reads=[IDB_b, BMH_b], writes=[bkb])
                            T(lambda e, bk=bk, oc=oc, hf=hf, h=h: e.matmul(out=bk[:, oc], lhsT=IDB[:, :], rhs=BML[:, hf, h, :], start=False, stop=False),
                              reads=[IDB_b, BML_b], writes=[bkb])
                            T(lambda e, bk=bk, oc=oc, h=h, kvh=kvh, par=par, kcols=kcols, blk=blk: e.matmul(
                                out=bk[:, oc], lhsT=KT[:, kvh, par, kcols], rhs=QT[:, h // 2, blk], start=False, stop=True),
                              reads=[KT_b, QT_b], writes=[bkb])

                def at_softmax(b):
                    n, blk, halves = at_info(b)
                    for hf in halves:
                        for g2 in range(2):
                            bk, bkb = banks[2 + hf * 2 + g2]
                            pcol = (hf * 2 + g2) * 512
                            A(lambda e, bk=bk, pcol=pcol: e.activation(out=PT_bf[:, pcol:pcol + 512], in_=bk[:, :], func=AF.Exp),
                              reads=[bkb], writes=[RD_b])

                def at_PV(b):
                    n, blk, halves = at_info(b)
                    for h in range(8):
                        kvh = h // 4
                        bk, bkb = banks[6 + h // 4]
                        for hf in halves:
                            pcol = (hf * 2 + h // 4) * 512 + (h % 4) * 128
                            T(lambda e, bk=bk, h=h, kvh=kvh, hf=hf, pcol=pcol, b=b, halves=halves: e.matmul(
                                out=bk[:, (h % 4) * 65:(h % 4 + 1) * 65], lhsT=PT_bf[:, pcol:pcol + 128],
                                rhs=VA[:, b + hf, kvh, :], start=(hf == halves[0]), stop=(hf == 1)),
                              reads=[RD_b, VA_b], writes=[bkb])

                def at_norm(b):
                    n, blk, halves = at_info(b)
                    for grp in range(2):
                        bk, bkb = banks[6 + grp]
                        obv = bk[:, 0:260].rearrange("p (h d) -> p h d", d=65)
                        V(lambda e, obv=obv, grp=grp: e.tensor_tensor(out=SM[:, 32 + grp * 4:36 + grp * 4], in0=obv[:, :, 64],
                                                                      in1=ES_[:, grp * 4:(grp + 1) * 4], op=ALU.add),
                          reads=[bkb, ES_b], writes=[smb[7]])
                        V(lambda e, grp=grp: e.reciprocal(out=SM[:, 40 + grp * 4:44 + grp * 4], in_=SM[:, 32 + grp * 4:36 + grp * 4]),
                          reads=[smb[7]], writes=[smb[7]])
                        V(lambda e, obv=obv, grp=grp: e.tensor_tensor(
                            out=YA[:, grp * 256:(grp + 1) * 256].rearrange("p (h d) -> p h d", d=64), in0=obv[:, :, 0:64],
                            in1=bc(SM[:, 40 + grp * 4:44 + grp * 4].unsqueeze(2), [128, 4, 64]), op=ALU.mult),
                          reads=[bkb, smb[7]], writes=[YA_b])
                    bk, bkb = next_proj_bank()
                    bi = pj[0]
                    for c in range(4):
                        T(lambda e, c=c, bi=bi: e.transpose(out=bank_bf(bi)[:, c * 128:(c + 1) * 128], in_=YA[:, c * 128:(c + 1) * 128],
                                                            identity=IDB[:, :]),
                          reads=[YA_b, IDB_b], writes=[bkb])
                    V(lambda e, bi=bi, blk=blk: e.tensor_copy(out=YAT[:, :, blk],
                                                              in_=bank_bf(bi)[:, 0:512].rearrange("p (c t) -> p c t", t=128)),
                      reads=[bkb], writes=[YAT_b])

                tm_front(0)
                at_S(0)
                at_softmax(0)
                for b in range(BPS):
                    if b + 1 < BPS:
                        tm_front(b + 1)
                        at_S(b + 1)
                    tm_spatial(b)
                    at_PV(b)
                    if b + 1 < BPS:
                        at_softmax(b + 1)
                    at_norm(b)

                for cc in range(8):
                    cs_ = slice(cc * 128, (cc + 1) * 128)
                    bGA, bGB, bPA, bPB = (2, 3, 4, 5) if cc % 2 == 0 else (6, 7, 0, 1)
                    for kc in range(8):
                        T(lambda e, kc=kc, cc=cc, bGA=bGA: e.matmul(out=banks[bGA][0][:, :], lhsT=WIN[:, kc, C_GA + cc * 128:C_GA + (cc + 1) * 128],
                                                                    rhs=HT[:, kc, :], start=(kc == 0), stop=(kc == 7)),
                          reads=[WIN_b, HTk[kc]], writes=[banks[bGA][1]])
                    for kc in range(8):
                        T(lambda e, kc=kc, cc=cc, bGB=bGB: e.matmul(out=banks[bGB][0][:, :], lhsT=WIN[:, kc, C_GB + cc * 128:C_GB + (cc + 1) * 128],
                                                                    rhs=HT[:, kc, :], start=(kc == 0), stop=(kc == 7)),
                          reads=[WIN_b, HTk[kc]], writes=[banks[bGB][1]])
                    for kc in range(4):
                        T(lambda e, kc=kc, cs_=cs_, bPA=bPA: e.matmul(out=banks[bPA][0][:, :], lhsT=PAW[:, kc, cs_], rhs=YAT[:, kc, :],
                                                                      start=(kc == 0), stop=(kc == 3)),
                          reads=[PAW_b, YAT_b], writes=[banks[bPA][1]])
                    for kc in range(4):
                        T(lambda e, kc=kc, cs_=cs_, bPB=bPB: e.matmul(out=banks[bPB][0][:, :], lhsT=PBW[:, kc, cs_], rhs=YBT[:, kc, :],
                                                                      start=(kc == 0), stop=(kc == 3)),
                          reads=[PBW_b, YBT_b], writes=[banks[bPB][1]])
                    sa_t = RE[:, (cc % 2) * 512:(cc % 2) * 512 + 512]
                    sa_b = re_half[cc % 2]
                    A(lambda e, bGA=bGA, sa_t=sa_t: e.activation(out=sa_t, in_=banks[bGA][0][:, :], func=AF.Tanh, scale=0.5),
                      reads=[banks[bGA][1]], writes=[sa_b, RE_b])
                    V(lambda e, bPA=bPA, sa_t=sa_t: e.scalar_tensor_tensor(out=RA[:, :], in0=sa_t, scalar=1.0, in1=banks[bPA][0][:, :], op0=ALU.add, op1=ALU.mult),
                      reads=[sa_b, banks[bPA][1]], writes=[RA_b])
                    A(lambda e, bGB=bGB, sa_t=sa_t: e.activation(out=sa_t, in_=banks[bGB][0][:, :], func=AF.Tanh, scale=0.5),
                      reads=[banks[bGB][1]], writes=[sa_b, RE_b])
                    V(lambda e, bPB=bPB, sa_t=sa_t: e.scalar_tensor_tensor(out=RB[:, :], in0=sa_t, scalar=1.0, in1=banks[bPB][0][:, :], op0=ALU.add, op1=ALU.mult),
                      reads=[sa_b, banks[bPB][1]], writes=[RB_b])
                    P(lambda e, cc=cc: e.tensor_tensor(out=MT[:, cc, :], in0=RA[:, :], in1=RB[:, :], op=ALU.add),
                      reads=[RA_b, RB_b], writes=[MT_b])

                RD2 = QT[:, :, :].rearrange("p a t -> p (a t)").bitcast(F32)
                RE2 = XN[0][0]
                h2bufs = [(RD, RD_b, RE, RE_b, [RE_b, re_half[0], re_half[1]]), (RD2, QT_b, RE2, XN[0][1], [XN[0][1]])]

                def wo_WO(b):
                    blk = slice(b * 128, (b + 1) * 128)
                    for hf in range(2):
                        hs = slice(hf * 512, (hf + 1) * 512)
                        bk, bkb = next_proj_bank()
                        for kc in range(8):
                            T(lambda e, kc=kc, bk=bk, blk=blk, hs=hs: e.matmul(out=bk[:, :], lhsT=MT[:, kc, blk], rhs=WOW[:, kc, hs],
                                                                               start=(kc == 0), stop=(kc == 7)),
                              reads=[MT_b, WOW_b], writes=[bkb])
                        V(lambda e, bk=bk, hs=hs: e.tensor_tensor(out=RC[:, :], in0=bk[:, :], in1=G1H[:, hs], op=ALU.mult),
                          reads=[bkb, G1H_b], writes=[RC_b])
                        V(lambda e, b=b, hs=hs: e.tensor_tensor(out=XT[:, b, hs], in0=XT[:, b, hs], in1=RC[:, :], op=ALU.add),
                          reads=[RC_b, xb[b]], writes=[xb[b]])

                def wo_N2(b):
                    n = s * BPS + b
                    rd, rdb, re_, reb, rew = h2bufs[b % 2]
                    sc.dma("sync", lambda e, b=b, n=n: e.dma_start(out=x1_d[n * 128:(n + 1) * 128, :], in_=XT[:, b, :]), reads=[xb[b]], writes=[X1D_b[n]])
                    if dbg:
                        sc.dma("sync", lambda e, b=b, n=n: e.dma_start(out=dbg_d["x1"][n * 128:(n + 1) * 128, :], in_=XT[:, b, :]),
                               reads=[xb[b]])
                    V(lambda e: e.memset(SM[:, 48:49], 0.0), writes=[smb[0]])
                    A(lambda e, b=b, rd=rd: e.activation(out=rd[:, :], in_=XT[:, b, :], func=AF.Square, accum_out=SM[:, 48:49]),
                      reads=[xb[b]], writes=[rdb, smb[0]])
                    V(lambda e: e.tensor_scalar(SM[:, 49:50], SM[:, 48:49], 1.0 / D, EPS, ALU.mult, ALU.add), reads=[smb[0]], writes=[smb[1]])
                    P(lambda e: e.tensor_tensor(out=SM[:, 50:51], in0=SM[:, 49:50], in1=NEGH[:, 0:1], op=ALU.pow),
                      reads=[smb[1], NEGH_b], writes=[smb[2]])
                    V(lambda e, b=b, rd=rd: e.scalar_tensor_tensor(out=rd[:, :], in0=XT[:, b, :], scalar=SM[:, 50:51], in1=A2[:, :],
                                                                   op0=ALU.mult, op1=ALU.mult),
                      reads=[xb[b], smb[2], A2_b], writes=[rdb])
                    V(lambda e, rd=rd: e.tensor_tensor(out=rd[:, :], in0=rd[:, :], in1=SH2[:, :], op=ALU.add), reads=[rdb, SH2_b], writes=[rdb])
                    A(lambda e, rd=rd, re_=re_: e.activation(out=re_[:, :], in_=rd[:, :], func=AF.Copy), reads=[rdb], writes=rew)
                    sc.dma("sync", lambda e, n=n, re_=re_: e.dma_start(out=h2_d[n * 128:(n + 1) * 128, :], in_=re_[:, :]), reads=[reb], writes=[H2D_b[n]])

                def wo_TR(b):
                    n = s * BPS + b
                    rd, rdb, re_, reb, rew = h2bufs[b % 2]
                    for c in range(8):
                        bi = 6 + c // 4
                        T(lambda e, c=c, bi=bi, rd=rd: e.transpose(out=banks[bi][0][:, (c % 4) * 128:(c % 4 + 1) * 128],
                                                                   in_=rd[:, c * 128:(c + 1) * 128], identity=IDF[:, :]),
                          reads=[rdb, IDF_b], writes=[banks[bi][1]])
                    A(lambda e: e.activation(out=H2T[:, 0:4, :], in_=banks[6][0][:, :].rearrange("p (c t) -> p c t", t=128), func=AF.Copy),
                      reads=[banks[6][1]], writes=[H2T_b])
                    V(lambda e: e.tensor_copy(out=H2T[:, 4:8, :], in_=banks[7][0][:, :].rearrange("p (c t) -> p c t", t=128)),
                      reads=[banks[7][1]], writes=[H2Tb_b])
                    bk, bkb = next_proj_bank()
                    for c in range(8):
                        T(lambda e, c=c, bk=bk: e.matmul(out=bk[0:36, 0:128], lhsT=WR[:, c, :], rhs=H2T[:, c, :], start=(c == 0), stop=False),
                          reads=[H2T_b if c < 4 else H2Tb_b, WR_b], writes=[bkb])
                    T(lambda e, bk=bk: e.matmul(out=bk[0:36, 0:128], lhsT=BR[0:1, 0:36], rhs=ONEF[0:1, 0:128], start=False, stop=True),
                      reads=[ONEF_b, BR_b], writes=[bkb])
                    A(lambda e, bk=bk: e.activation(out=LGT[0:36, :], in_=bk[0:36, 0:128], func=AF.Copy), reads=[bkb], writes=[LGT_b])

                def wo_LG(b):
                    n = s * BPS + b
                    bk, bkb = next_proj_bank()
                    T(lambda e, bk=bk: e.transpose(out=bk[:, 0:36], in_=LGT[0:36, :], identity=IDF[0:36, 0:36]),
                      reads=[LGT_b, IDF_b], writes=[bkb])
                    V(lambda e, bk=bk, n=n: e.tensor_copy(out=LG[:, n, :], in_=bk[:, 0:36]), reads=[bkb], writes=[LG_b])

                wo_WO(0)
                wo_N2(0)
                for b in range(BPS):
                    if b + 1 < BPS:
                        wo_WO(b + 1)
                    if b > 0:
                        wo_LG(b - 1)
                    wo_TR(b)
                    if b + 1 < BPS:
                        wo_N2(b + 1)
                wo_LG(BPS - 1)
                P(lambda e: e.tensor_copy(out=KT[:, :, :, 0:128], in_=KT[:, :, :, 512:640]), reads=[KT_b], writes=[KT_b])
                P(lambda e: e.tensor_copy(out=VA[:, 0, :, :], in_=VA[:, 4, :, :]), reads=[VA_b], writes=[VA_b])

            if dbg:
                sc.dma("sync", lambda e: e.dma_start(out=dbg_d["lg"][:, :], in_=LG[:, :, :].rearrange("p t k -> p (t k)")), reads=[LG_b])
            sc.drain("sync")
            sc.emit()

        if stage in ("A", "A1"):
            return nc, dbg_d

        pr_es = ExitStack()
        with pr_es:
            sr = lambda name, shape, dt: sb(name, shape, dt, stack=pr_es)
            GM, GM_b = sr("gm", [128, 32], F32)
            OHG, OHG_b = sr("ohg", [128, 32, 4], F32)
            T4, T4_b = sr("t4", [128, 32, 4], F32)
            PEN, PEN_b = sr("pen", [128, 32, 4], F32)
            PG, PG_b = sr("pg", [128, 32], F32)
            LEM, LEM_b = sr("lem", [128, 32, 32], F32)
            LEM2, LEM2_b = sr("lem2", [128, 32, 32], F32)
            OH1, OH1_b = sr("oh1", [128, 32, 32], F32)
            OH2, OH2_b = sr("oh2", [128, 32, 32], F32)
            M1, M1_b = sr("m1", [128, 32], F32)
            M2, M2_b = sr("m2", [128, 32], F32)
            DL, DL_b = sr("dl", [128, 32], F32)
            MM, MM_b = sr("mm", [128, 32, 32], F32)
            US, US_b = sr("us", [128, 128], F32)
            TOT, TOT_b = sr("tot", [128, 32, 32], F32)
            CA, CA_b = sr("ca", [128, 32, 32], F32)
            CB, CB_b = sr("cb", [128, 32, 32], F32)
            RANK, RANK_b = sr("rank", [128, 32, 32], F32)
            THRI, THRI_b = sr("thri", [128, 32], I32)
            THRF, THRF_b = sr("thrf", [128, 32], F32)
            NBK, NBK_b = sr("nbk", [128, 32], F32)
            C1, C1_b = sr("c1", [128, 32], F32)
            C2, C2_b = sr("c2", [128, 32], F32)
            PST, PST_b = sr("pst", [128, 32], F32)
            JVI, JVI_b = sr("jvi", [128, NB], I32)
            JVF, JVF_b = sr("jvf", [128, NB], F32)
            CMP2, CMP2_b = sr("cmp2", [128, NB, 32], F32)
            EJ, EJ_b = sr("ej", [128, NB], F32)
            DF, DF_b = sr("df", [128, 2, 32], F32)
            TOKID, TOKID_b = sr("tokid", [128, 32, 2], I32)
            ZERO, ZERO_b = sr("zero", [128, 256], I32)
            SLJ, SLJ_b = sr("slj", [128, 256], I32)
            SLJF, SLJF_b = sr("sljf", [128, 128], F32)
            PIDI, PIDI_b = sr("pidi", [128, 1], I32)
            PIDF, PIDF_b = sr("pidf", [128, 1], F32)
            IWF, IWF_b = sr("iwf", [128, NB], F32)

            lgv = LG[:, :, 0:4]
            lev = LG[:, :, 4:36]
            V(lambda e: e.tensor_reduce(out=GM[:, :], in_=lgv, axis=AX.X, op=ALU.max), reads=[LG_b], writes=[GM_b])
            V(lambda e: e.tensor_tensor(out=OHG[:, :, :], in0=lgv, in1=bc(GM[:, :].unsqueeze(2), [128, 32, 4]), op=ALU.is_equal),
              reads=[LG_b, GM_b], writes=[OHG_b])
            V(lambda e: e.tensor_tensor(out=T4[:, :, :], in0=lgv, in1=bc(GM[:, :].unsqueeze(2), [128, 32, 4]), op=ALU.subtract),
              reads=[LG_b, GM_b], writes=[T4_b])
            A(lambda e: e.activation(out=T4[:, :, :], in_=T4[:, :, :], func=AF.Exp), reads=[T4_b], writes=[T4_b])
            V(lambda e: e.tensor_reduce(out=PG[:, :], in_=T4[:, :, :], axis=AX.X, op=ALU.add), reads=[T4_b], writes=[PG_b])
            V(lambda e: e.reciprocal(out=PG[:, :], in_=PG[:, :]), reads=[PG_b], writes=[PG_b])
            V(lambda e: e.tensor_scalar(PEN[:, :, :], OHG[:, :, :], 1e30, -1e30, ALU.mult, ALU.add), reads=[OHG_b], writes=[PEN_b])
            V(lambda e: e.tensor_tensor(out=LEM[:, :, :].rearrange("p t (g i) -> p t g i", i=8),
                                        in0=lev.rearrange("p t (g i) -> p t g i", i=8),
                                        in1=bc(PEN[:, :, :].unsqueeze(3), [128, 32, 4, 8]), op=ALU.add),
              reads=[LG_b, PEN_b], writes=[LEM_b])
            V(lambda e: e.tensor_reduce(out=M1[:, :], in_=LEM[:, :, :], axis=AX.X, op=ALU.max), reads=[LEM_b], writes=[M1_b])
            V(lambda e: e.tensor_tensor(out=OH1[:, :, :], in0=LEM[:, :, :], in1=bc(M1[:, :].unsqueeze(2), [128, 32, 32]), op=ALU.is_equal),
              reads=[LEM_b, M1_b], writes=[OH1_b])
            V(lambda e: e.scalar_tensor_tensor(out=LEM2[:, :, :], in0=OH1[:, :, :], scalar=-1e30, in1=LEM[:, :, :],
                                               op0=ALU.mult, op1=ALU.add),
              reads=[OH1_b, LEM_b], writes=[LEM2_b])
            V(lambda e: e.tensor_reduce(out=M2[:, :], in_=LEM2[:, :, :], axis=AX.X, op=ALU.max), reads=[LEM2_b], writes=[M2_b])
            V(lambda e: e.tensor_tensor(out=OH2[:, :, :], in0=LEM2[:, :, :], in1=bc(M2[:, :].unsqueeze(2), [128, 32, 32]), op=ALU.is_equal),
              reads=[LEM2_b, M2_b], writes=[OH2_b])
            V(lambda e: e.tensor_tensor(out=DL[:, :], in0=M2[:, :], in1=M1[:, :], op=ALU.subtract), reads=[M1_b, M2_b], writes=[DL_b])
            A(lambda e: e.activation(out=DL[:, :], in_=DL[:, :], func=AF.Exp), reads=[DL_b], writes=[DL_b])
            V(lambda e: e.tensor_scalar(DL[:, :], DL[:, :], 1.0, None, ALU.add), reads=[DL_b], writes=[DL_b])
            V(lambda e: e.reciprocal(out=DL[:, :], in_=DL[:, :]), reads=[DL_b], writes=[DL_b])
            V(lambda e: e.tensor_tensor(out=W12[:, 0, :], in0=PG[:, :], in1=DL[:, :], op=ALU.mult), reads=[PG_b, DL_b], writes=[W12_b])
            V(lambda e: e.tensor_tensor(out=W12[:, 1, :], in0=PG[:, :], in1=W12[:, 0, :], op=ALU.subtract), reads=[PG_b, W12_b], writes=[W12_b])
            V(lambda e: e.tensor_tensor(out=MM[:, :, :], in0=OH1[:, :, :], in1=OH2[:, :, :], op=ALU.add), reads=[OH1_b, OH2_b], writes=[MM_b])
            P(lambda e: e.affine_select(out=US[:, :], in_=ONEF[:, :], pattern=[[1, 128]], compare_op=ALU.is_gt, fill=0.0,
                                        base=0, channel_multiplier=-1), reads=[ONEF_b], writes=[US_b])
            MMf = MM[:, :, :].rearrange("p t e -> p (t e)")
            for hf in range(2):
                T(lambda e, hf=hf: e.matmul(out=banks[hf][0][:, :], lhsT=US[:, :], rhs=MMf[:, hf * 512:(hf + 1) * 512], start=True, stop=True),
                  reads=[US_b, MM_b], writes=[banks[hf][1]])
                T(lambda e, hf=hf: e.matmul(out=banks[2 + hf][0][:, :], lhsT=ONEF[:, :], rhs=MMf[:, hf * 512:(hf + 1) * 512], start=True, stop=True),
                  reads=[ONEF_b, MM_b], writes=[banks[2 + hf][1]])
            TOTf = TOT[:, :, :].rearrange("p t e -> p (t e)")
            for hf in range(2):
                V(lambda e, hf=hf: e.tensor_copy(out=TOTf[:, hf * 512:(hf + 1) * 512], in_=banks[2 + hf][0][:, :]),
                  reads=[banks[2 + hf][1]], writes=[TOT_b])
            chain = [(TOT, TOT_b), (CA, CA_b), (CB, CB_b), (CA, CA_b), (CB, CB_b), (CA, CA_b)]
            for i, sh_ in enumerate([1, 2, 4, 8, 16]):
                (src, srcb), (dst, dstb) = chain[i], chain[i + 1]
                V(lambda e, src=src, dst=dst, sh_=sh_: e.tensor_copy(out=dst[:, 0:sh_, :], in_=src[:, 0:sh_, :]), reads=[srcb], writes=[dstb])
                V(lambda e, src=src, dst=dst, sh_=sh_: e.tensor_tensor(out=dst[:, sh_:32, :], in0=src[:, sh_:32, :], in1=src[:, 0:32 - sh_, :], op=ALU.add),
                  reads=[srcb], writes=[dstb])
            V(lambda e: e.tensor_tensor(out=CB[:, :, :], in0=CA[:, :, :], in1=TOT[:, :, :], op=ALU.subtract), reads=[CA_b, TOT_b], writes=[CB_b])
            CBf = CB[:, :, :].rearrange("p t e -> p (t e)")
            RANKf = RANK[:, :, :].rearrange("p t e -> p (t e)")
            for hf in range(2):
                V(lambda e, hf=hf: e.tensor_tensor(out=RANKf[:, hf * 512:(hf + 1) * 512], in0=banks[hf][0][:, :],
                                                   in1=CBf[:, hf * 512:(hf + 1) * 512], op=ALU.add),
                  reads=[banks[hf][1], CB_b], writes=[RANK_b])
            CNT = CA[:, 31, :]
            P(lambda e: e.iota(THRI[:, :], pattern=[[128, 32]], base=1, channel_multiplier=0), writes=[THRI_b])
            V(lambda e: e.tensor_copy(out=THRF[:, :], in_=THRI[:, :]), reads=[THRI_b], writes=[THRF_b])
            V(lambda e: e.tensor_tensor(out=LEM[:, :, :], in0=bc(CNT.unsqueeze(2), [128, 32, 32]),
                                        in1=bc(THRF[:, :].unsqueeze(1), [128, 32, 32]), op=ALU.is_ge),
              reads=[CA_b, THRF_b], writes=[LEM_b])
            V(lambda e: e.tensor_reduce(out=NBK[:, :], in_=LEM[:, :, :], axis=AX.X, op=ALU.add), reads=[LEM_b], writes=[NBK_b])
            chain2 = [(NBK, NBK_b), (C1, C1_b), (C2, C2_b), (C1, C1_b), (C2, C2_b), (C1, C1_b)]
            for i, sh_ in enumerate([1, 2, 4, 8, 16]):
                (src, srcb), (dst, dstb) = chain2[i], chain2[i + 1]
                V(lambda e, src=src, dst=dst, sh_=sh_: e.tensor_copy(out=dst[:, 0:sh_], in_=src[:, 0:sh_]), reads=[srcb], writes=[dstb])
                V(lambda e, src=src, dst=dst, sh_=sh_: e.tensor_tensor(out=dst[:, sh_:32], in0=src[:, sh_:32], in1=src[:, 0:32 - sh_], op=ALU.add),
                  reads=[srcb], writes=[dstb])
            V(lambda e: e.tensor_tensor(out=PST[:, :], in0=C1[:, :], in1=NBK[:, :], op=ALU.subtract), reads=[C1_b, NBK_b], writes=[PST_b])
            P(lambda e: e.iota(JVI[:, :], pattern=[[1, NB]], base=0, channel_multiplier=0), writes=[JVI_b])
            V(lambda e: e.tensor_copy(out=JVF[:, :], in_=JVI[:, :]), reads=[JVI_b], writes=[JVF_b])
            V(lambda e: e.tensor_tensor(out=CMP2[:, :, :], in0=bc(C1[:, :].unsqueeze(1), [128, NB, 32]),
                                        in1=bc(JVF[:, :].unsqueeze(2), [128, NB, 32]), op=ALU.is_le),
              reads=[C1_b, JVF_b], writes=[CMP2_b])
            V(lambda e: e.tensor_reduce(out=EJ[:, :], in_=CMP2[:, :, :], axis=AX.X, op=ALU.add), reads=[CMP2_b], writes=[EJ_b])
            V(lambda e: e.tensor_scalar(EJ[:, :], EJ[:, :], 31.0, None, ALU.min), reads=[EJ_b], writes=[EJ_b])
            V(lambda e: e.scalar_tensor_tensor(out=LEM2[:, :, :], in0=bc(PST[:, :].unsqueeze(1), [128, 32, 32]), scalar=128.0,
                                               in1=RANK[:, :, :], op0=ALU.mult, op1=ALU.add),
              reads=[PST_b, RANK_b], writes=[LEM2_b])
            for k, (OH, OHb) in enumerate([(OH1, OH1_b), (OH2, OH2_b)]):
                V(lambda e, OH=OH: e.tensor_tensor(out=LEM[:, :, :], in0=OH[:, :, :], in1=LEM2[:, :, :], op=ALU.mult),
                  reads=[OHb, LEM2_b], writes=[LEM_b])
                V(lambda e, k=k: e.tensor_reduce(out=DF[:, k, :], in_=LEM[:, :, :], axis=AX.X, op=ALU.add), reads=[LEM_b], writes=[DF_b])
            V(lambda e: e.tensor_copy(out=DEST[:, :, :], in_=DF[:, :, :]), reads=[DF_b], writes=[DEST_b])
            HX = [sr(f"hx{i}", [128, 1024], BF16) for i in range(4)]
            for t in range(NBLK):
                hx, hxb = HX[t % 4]
                sc.dma("sync", lambda e, t=t, hx=hx: e.dma_start(out=hx[:, :], in_=h2_d[t * 128:(t + 1) * 128, :]), reads=[H2D_b[t]], writes=[hxb])
                for k in range(2):
                    sc.dma("gpsimd", lambda e, k=k, t=t, hx=hx: e.indirect_dma_start(
                        out=xs_d[:, :], out_offset=bass.IndirectOffsetOnAxis(ap=DEST[:, k, t:t + 1], axis=0),
                        in_=hx[:, :], in_offset=None, bounds_check=sc.reg(e, R - 1), oob_is_err=False),
                        reads=[DEST_b, hxb] + XZ_b, writes=[XS_b[2 * t + k]])
            P(lambda e: e.iota(PIDI[:, :], pattern=[[0, 1]], base=0, channel_multiplier=1), writes=[PIDI_b])
            V(lambda e: e.tensor_copy(out=PIDF[:, :], in_=PIDI[:, :]), reads=[PIDI_b], writes=[PIDF_b])
            V(lambda e: e.tensor_scalar(IWF[:, :], EJ[:, :], 128.0, PIDF[:, 0:1], ALU.mult, ALU.add), reads=[EJ_b, PIDF_b], writes=[IWF_b])
            EQ, EQ_b = sr("eq", [128, NB], F32)
            V(lambda e: e.memset(EQ[:, :], 0.0), writes=[EQ_b])
            V(lambda e: e.tensor_tensor(out=EQ[:, 1:NB], in0=EJ[:, 1:NB], in1=EJ[:, 0:NB - 1], op=ALU.is_equal), reads=[EJ_b], writes=[EQ_b])
            V(lambda e: e.memset(EQ[:, :].rearrange("p (l k) -> p l k", k=LB)[:, :, 0:1], 0.0), writes=[EQ_b])
            V(lambda e: e.scalar_tensor_tensor(out=IWF[:, :], in0=EQ[:, :], scalar=1.0e6, in1=IWF[:, :], op0=ALU.mult, op1=ALU.add),
              reads=[EQ_b, IWF_b], writes=[IWF_b])
            V(lambda e: e.tensor_copy(out=IDXW[:, :], in_=IWF[:, :]), reads=[IWF_b], writes=[IDXW_b])
            if dbg:
                sc.dma("sync", lambda e: e.dma_start(out=dbg_d["rt"][:, 0:NB], in_=IWF[:, :]), reads=[IWF_b])
                sc.dma("sync", lambda e: e.dma_start(out=dbg_d["rt"][:, 256:320], in_=DF[:, :, :].rearrange("p k t -> p (k t)")), reads=[DF_b])
                sc.dma("sync", lambda e: e.dma_start(out=dbg_d["rt"][:, 320:384], in_=W12[:, :, :].rearrange("p k t -> p (k t)")), reads=[W12_b])
                sc.dma("sync", lambda e: e.dma_start(out=dbg_d["rt"][:, 384:416], in_=CA[:, 31, :]), reads=[CA_b])
            sc.drain("sync")
            sc.emit()

        if stage == "R":
            return nc, dbg_d

        pb_es = ExitStack()
        with pb_es:
            sB = lambda name, shape, dt: sb(name, shape, dt, stack=pb_es)
            WG = [sB(f"wg{i}", [128, 8, 512], BF16) for i in range(NLANE)]
            WU = [sB(f"wu{i}", [128, 8, 512], BF16) for i in range(NLANE)]
            WD = [sB(f"wd{i}", [128, 4, 1024], BF16) for i in range(NLANE)]
            NXG = 8
            XG = [sB(f"xg{i}", [128, 1024], BF16) for i in range(NXG)]
            XTt = [sB(f"xtt{i}", [128, 8, 128], BF16) for i in range(2)]
            SG, SG_b = sB("sg", [128, 512], F32)
            ACTT = [sB(f"actt{i}", [128, 4, 128], BF16) for i in range(2)]
            ACTM = [sB(f"actm{i}", [128, 512], BF16) for i in range(2)]
            YO = [sB(f"yo{i}", [128, 1024], BF16) for i in range(2)]
            order = [l * LB + k for k in range(LB) for l in range(NLANE)]
            nsteps = len(order)

            def loads(i):
                j = order[i]
                lane = j // LB
                xg, xgb = XG[i % NXG]
                sc.dma("sync", lambda e, j=j, xg=xg: e.dma_start(out=xg[:, :], in_=xs_d[j * 128:(j + 1) * 128, :]),
                       reads=XS_b, writes=[xgb])
                for (Wt, wdl) in ((WG, wg_d), (WU, wu_d), (WD, wd_d)):
                    wt_, wtb = Wt[lane]
                    wflat = wt_[:, :, :].rearrange("p a b -> p (a b)")
                    for hh in range(2):
                        sc.dma("gpsimd", lambda e, j=j, hh=hh, wflat=wflat, wdl=wdl: e.indirect_dma_start(
                            out=wflat[:, hh * 2048:(hh + 1) * 2048], out_offset=None, in_=wdl[hh][:, :],
                            in_offset=bass.IndirectOffsetOnAxis(ap=IDXW[:, j:j + 1], axis=0), bounds_check=sc.reg(e, NE * 128 - 1), oob_is_err=False),
                            reads=[IDXW_b], writes=[wtb], join=(hh > 0))

            def stepT(i):
                xg, xgb = XG[i % NXG]
                tb = i % 2
                xt_, xtb = XTt[i % 2]
                for c in range(8):
                    T(lambda e, c=c, tb=tb, xg=xg: e.transpose(out=bank_bf(tb)[:, c * 128:(c + 1) * 128], in_=xg[:, c:1024:8], identity=IDB[:, :]),
                      reads=[xgb, IDB_b], writes=[banks[tb][1]])
                V(lambda e, tb=tb, xt_=xt_: e.tensor_copy(out=xt_[:, :, :].rearrange("p c t -> p (c t)"), in_=bank_bf(tb)[:, 0:1024]),
                  reads=[banks[tb][1]], writes=[xtb])

            def stepGU(i):
                j = order[i]
                lane = j // LB
                xt_, xtb = XTt[i % 2]
                gb_, ub_ = (2, 3) if i % 2 == 0 else (6, 7)
                for (Wt, bi) in ((WG, gb_), (WU, ub_)):
                    wt_, wtb = Wt[lane]
                    for c in range(8):
                        T(lambda e, wt_=wt_, bi=bi, c=c, xt_=xt_: e.matmul(
                            out=banks[bi][0][:, :], lhsT=xt_[:, c, :], rhs=wt_[:, c, :], start=(c == 0), stop=(c == 7)),
                          reads=[wtb, xtb], writes=[banks[bi][1]])
                at_, atb = ACTT[i % 2]
                am_, amb = ACTM[i % 2]
                A(lambda e, gb_=gb_: e.activation(out=SG[:, :], in_=banks[gb_][0][:, :], func=AF.Silu), reads=[banks[gb_][1]], writes=[SG_b])
                V(lambda e, ub_=ub_, am_=am_: e.tensor_tensor(out=am_[:, :], in0=SG[:, :], in1=banks[ub_][0][:, :], op=ALU.mult),
                  reads=[SG_b, banks[ub_][1]], writes=[amb])
                for jj in range(4):
                    T(lambda e, jj=jj, gb_=gb_, am_=am_: e.transpose(out=bank_bf(gb_)[:, jj * 128:(jj + 1) * 128], in_=am_[:, jj:512:4], identity=IDB[:, :]),
                      reads=[amb, IDB_b], writes=[banks[gb_][1]])
                A(lambda e, gb_=gb_, at_=at_: e.activation(out=at_[:, :, :].rearrange("p a t -> p (a t)"), in_=bank_bf(gb_)[:, 0:512], func=AF.Copy),
                  reads=[banks[gb_][1]], writes=[atb])

            def stepD(i):
                j = order[i]
                lane = j // LB
                at_, atb = ACTT[i % 2]
                wd_, wdb = WD[lane]
                yo_, yob = YO[i % 2]
                for hf in range(2):
                    for jj in range(4):
                        T(lambda e, hf=hf, jj=jj, at_=at_, wd_=wd_: e.matmul(out=banks[4 + hf][0][:, :], lhsT=at_[:, jj, :],
                                                                            rhs=wd_[:, jj, hf * 512:(hf + 1) * 512], start=(jj == 0), stop=(jj == 3)),
                          reads=[atb, wdb], writes=[banks[4 + hf][1]])
                A(lambda e, yo_=yo_: e.activation(out=yo_[:, 0:512], in_=banks[4][0][:, :], func=AF.Copy), reads=[banks[4][1]], writes=[yob])
                V(lambda e, yo_=yo_: e.tensor_copy(out=yo_[:, 512:1024], in_=banks[5][0][:, :]), reads=[banks[5][1]], writes=[yob])
                sc.dma("sync", lambda e, j=j, yo_=yo_: e.dma_start(out=ys_d[j * 128:(j + 1) * 128, :], in_=yo_[:, :]), reads=[yob], writes=[YS_b[j]])

            LOOK = NLANE - 1
            for i0 in range(LOOK):
                loads(i0)
            stepT(0)
            stepGU(0)
            stepT(1)
            for i in range(nsteps):
                if i + LOOK < nsteps:
                    loads(i + LOOK)
                if i + 1 < nsteps:
                    stepGU(i + 1)
                if i + 2 < nsteps:
                    stepT(i + 2)
                stepD(i)
            sc.drain("sync")
            sc.emit()

        pc_es = ExitStack()
        with pc_es:
            sC = lambda name, shape, dt: sb(name, shape, dt, stack=pc_es)
            FG, FG_b = sC("fgt", [128, 1024], F32)
            NCB = 4
            Y1 = [sC(f"y1{i}", [128, 1024], F32) for i in range(NCB)]
            Y2 = [sC(f"y2{i}", [128, 1024], F32) for i in range(NCB)]
            G1 = [sC(f"g1{i}", [128, 1024], BF16) for i in range(NCB)]
            G2B = [sC(f"g2b{i}", [128, 1024], BF16) for i in range(NCB)]
            X1 = [sC(f"x1{i}", [128, 1024], F32) for i in range(NCB)]
            ST3, _ = sC("st3", [128, 8], F32)
            st3b = [Buf() for _ in range(3)]
            sc.dma("sync", lambda e: e.dma_start(out=FG[:, :], in_=fg_d[:, :]), writes=[FG_b])
            G2, G2_b = sC("g2c", [128, 1024], F32)
            sc.dma("sync", lambda e: e.dma_start(out=G2[:, :], in_=g2s_d[:, :]), reads=[G2D_b], writes=[G2_b])
            def c_loads(t):
                p = t % NCB
                for k, (yy, yyb) in enumerate((G1[p], G2B[p])):
                    sc.dma("gpsimd", lambda e, k=k, t=t, yy=yy: e.indirect_dma_start(
                        out=yy[:, :], out_offset=None, in_=ys_d[:, :],
                        in_offset=bass.IndirectOffsetOnAxis(ap=DEST[:, k, t:t + 1], axis=0), bounds_check=sc.reg(e, R - 1), oob_is_err=False),
                        reads=[DEST_b] + YS_b, writes=[yyb])
                x1, x1b = X1[p]
                sc.dma("sync", lambda e, t=t, x1=x1: e.dma_start(out=x1[:, :], in_=x1_d[t * 128:(t + 1) * 128, :]), reads=[X1D_b[t]], writes=[x1b])

            st3 = [[Buf() for _ in range(3)] for _ in range(2)]

            def c_partA(t):
                p = t % NCB
                y1, y1b = Y1[p]
                y2, y2b = Y2[p]
                x1, x1b = X1[p]
                o = (t % 2) * 3
                sb3 = st3[t % 2]
                ga, gab = G1[p]
                gb2, gbb = G2B[p]
                A(lambda e, t=t, y1=y1, ga=ga: e.activation(out=y1[:, :], in_=ga[:, :], func=AF.Copy, scale=W12[:, 0, t:t + 1]), reads=[gab, W12_b], writes=[y1b])
                V(lambda e, t=t, y1=y1, gb2=gb2: e.scalar_tensor_tensor(out=y1[:, :], in0=gb2[:, :], scalar=W12[:, 1, t:t + 1], in1=y1[:, :],
                                                                        op0=ALU.mult, op1=ALU.add),
                  reads=[y1b, gbb, W12_b], writes=[y1b])
                V(lambda e, y1=y1: e.tensor_tensor(out=y1[:, :], in0=y1[:, :], in1=G2[:, :], op=ALU.mult), reads=[y1b, G2_b], writes=[y1b])
                V(lambda e, y1=y1, x1=x1: e.tensor_tensor(out=x1[:, :], in0=x1[:, :], in1=y1[:, :], op=ALU.add), reads=[y1b, x1b], writes=[x1b])
                V(lambda e, o=o: e.memset(ST3[:, o:o + 1], 0.0), writes=[sb3[0]])
                A(lambda e, x1=x1, y2=y2, o=o: e.activation(out=y2[:, :], in_=x1[:, :], func=AF.Square, accum_out=ST3[:, o:o + 1]),
                  reads=[x1b], writes=[y2b, sb3[0]])
                V(lambda e, o=o: e.tensor_scalar(ST3[:, o + 1:o + 2], ST3[:, o:o + 1], 1.0 / D, EPS, ALU.mult, ALU.add), reads=[sb3[0]], writes=[sb3[1]])
                P(lambda e, o=o: e.tensor_tensor(out=ST3[:, o + 2:o + 3], in0=ST3[:, o + 1:o + 2], in1=NEGH[:, 0:1], op=ALU.pow),
                  reads=[sb3[1], NEGH_b], writes=[sb3[2]])

            def c_partB(t):
                p = t % NCB
                y2, y2b = Y2[p]
                x1, x1b = X1[p]
                o = (t % 2) * 3
                sb3 = st3[t % 2]
                V(lambda e, x1=x1, y2=y2, o=o: e.scalar_tensor_tensor(out=y2[:, :], in0=x1[:, :], scalar=ST3[:, o + 2:o + 3], in1=FG[:, :],
                                                                      op0=ALU.mult, op1=ALU.mult),
                  reads=[x1b, sb3[2], FG_b], writes=[y2b])
                sc.dma("sync", lambda e, t=t, y2=y2: e.dma_start(out=out_d[t * 128:(t + 1) * 128, :], in_=y2[:, :]), reads=[y2b])

            for t0 in range(NCB - 1):
                c_loads(t0)
            c_partA(0)
            for t in range(NBLK):
                if t + NCB - 1 < NBLK:
                    c_loads(t + NCB - 1)
                if t + 1 < NBLK:
                    c_partA(t + 1)
                c_partB(t)
            sc.drain("sync")
            sc.emit()
    return nc, dbg_d


def _t5_thresholds():
    n = np.arange(0, 4200, dtype=np.int64)
    nf = np.maximum(n, 1).astype(np.float32)
    large = 16 + (np.log(nf / np.float32(16)) / np.float32(np.log(128 / 16)) * np.float32(16)).astype(np.int32)
    large = np.minimum(large, 31)
    bucket = np.where(n < 16, n, large)
    thr = np.zeros(32, np.float64)
    for k in range(1, 32):
        idx = np.nonzero(bucket >= k)[0]
        thr[k] = float(idx[0]) if len(idx) else 1e9
    return thr


def _prep_shared(inp):
    f = lambda a: np.ascontiguousarray(np.asarray(a, dtype=np.float32))
    sh = {}
    sh["zrows"] = np.zeros((1536, D), dtype=ml_dtypes.bfloat16)
    sh["relb"] = f(np.broadcast_to(np.asarray(inp["rel_bias"], np.float32).reshape(1, 256), (128, 256)))
    wada = np.asarray(inp["w_ada"], np.float32)[0].reshape(8, 128, 6, 1024)
    sh["w_ada"] = f(wada.transpose(1, 2, 0, 3))
    bada = np.asarray(inp["b_ada"], np.float32)[0]
    sh["b_ada_f"] = f(bada.reshape(6, 8, 128).transpose(2, 0, 1))
    sh["b_ada_r"] = f(np.broadcast_to(bada.reshape(1, 6, 1024), (128, 6, 1024)))
    sh["n1g"] = f(np.asarray(inp["norm1_g"], np.float32)[0].reshape(8, 128).T)
    sh["n2g"] = f(np.broadcast_to(np.asarray(inp["norm2_g"], np.float32)[0].reshape(1, 1024), (128, 1024)))
    sh["fg"] = f(np.broadcast_to(np.asarray(inp["final_g"], np.float32).reshape(1, 1024), (128, 1024)))
    w = np.asarray(inp["w_in"], np.float32)[0]
    wdev = np.concatenate([w[:, 0:512], w[:, 512:576], w[:, 512:576], w[:, 576:640], w[:, 576:640],
                           w[:, 640:768], w[:, 768:1280], w[:, 1280:1792], w[:, 1792:2816], w[:, 2816:3840]], axis=1)
    sh["w_in"] = f(wdev.reshape(8, 128, INW).transpose(1, 0, 2))
    sh["sinks"] = f(np.broadcast_to(np.asarray(inp["sinks"], np.float32)[0].reshape(1, 8), (128, 8)))
    sh["lng"] = f(np.broadcast_to(np.asarray(inp["gm_ln_g"], np.float32)[0].reshape(1, 512), (128, 512)))
    sh["lnb"] = f(np.broadcast_to(np.asarray(inp["gm_ln_b"], np.float32)[0].reshape(1, 512), (128, 512)))
    sh["wst"] = f(np.asarray(inp["gm_w_s"], np.float32)[0].transpose(2, 0, 1))
    sh["bs"] = f(np.asarray(inp["gm_b_s"], np.float32)[0].reshape(1, 512))
    sh["p_a"] = f(np.asarray(inp["p_a"], np.float32)[0].reshape(4, 128, 1024).transpose(1, 0, 2))
    sh["p_b"] = f(np.asarray(inp["p_b"], np.float32)[0].reshape(4, 128, 1024).transpose(1, 0, 2))
    sh["w_o"] = f(np.asarray(inp["w_o"], np.float32)[0].reshape(8, 128, 1024).transpose(1, 0, 2))
    wr = np.concatenate([np.asarray(inp["w_router_g"], np.float32)[0], np.asarray(inp["w_router_e"], np.float32)[0]], axis=1)
    sh["w_r"] = f(wr.reshape(8, 128, 36).transpose(1, 0, 2))
    sh["b_r"] = f(np.concatenate([np.asarray(inp["b_router_g"], np.float32)[0],
                                  np.asarray(inp["b_router_e"], np.float32)[0]]).reshape(1, 36))
    for nm in ("w_gate", "w_up", "w_down"):
        w2 = np.asarray(inp[nm], np.float32)[0].reshape(NE * 128, 2, 2048)
        sh[nm + "0"] = f(w2[:, 0, :])
        sh[nm + "1"] = f(w2[:, 1, :])
    return sh


def _prep_core(inp, b):
    m = {}
    m["x"] = np.ascontiguousarray(np.asarray(inp["x"], np.float32)[b])
    m["c"] = np.ascontiguousarray(np.asarray(inp["c"], np.float32)[b].reshape(8, 128).T)
    pos = np.asarray(inp["positions"], np.int32)[b]
    m["posq"] = np.ascontiguousarray(np.broadcast_to(pos[128:256].reshape(1, 128), (128, 128))).astype(np.int32)
    m["posk"] = np.ascontiguousarray(np.stack([pos[0:128], pos[128:256]], axis=1)).astype(np.int32)
    return m


def run(inputs, stage="full", dbg=False, n_cores=8):
    nc, dbg_d = build_nc(stage=stage, dbg=dbg)
    sh = _prep_shared(inputs)
    in_maps = []
    for b in range(n_cores):
        m = dict(sh)
        m.update(_prep_core(inputs, b))
        in_maps.append(m)
    res = run_bass_kernel_spmd(nc, in_maps, core_ids=list(range(n_cores)))
    return res


def kernel(**inputs):
    res = run(inputs)
    out = np.stack([np.asarray(r["out"], np.float32).reshape(S, D) for r in res.results], axis=0)
    return out
```
